# Optimizing a Trainium2 kernel written in Bass

```python
import math
import jax, jax.numpy as jnp
from jax import lax
import numpy as np

D_MODEL = 2048
BATCH = 2
SEQ = 4096
DEPTH = 2

HEAD_DIM = 128
DILATED_GROUPS = ((128, 1), (512, 4), (2048, 16))
HEADS_PER_GROUP = 4
N_DIL_GROUPS = len(DILATED_GROUPS)
N_SELF_HEADS = N_DIL_GROUPS * HEADS_PER_GROUP
N_RET_HEADS = 12
SELF_WIDTH = N_SELF_HEADS * HEAD_DIM
MEM_HEADS = 4
MEM_LEN = 256
MEM_WIDTH = MEM_HEADS * HEAD_DIM
MIX_WIDTH = SELF_WIDTH + MEM_WIDTH
RET_CHUNK = 128
N_EXPERTS = 32
TOP_K = 4
D_EXPERT = D_MODEL
SWIGLU_ALPHA = 1.702
SWIGLU_LIMIT = 7.0
MOE_BLOCK = 128
N_MIXERS = 2
DEEPNORM_ALPHA = (2 * DEPTH) ** 0.25
DEEPNORM_BETA = (8 * DEPTH) ** -0.25
LN_EPS = 1e-5
NEG_INF = -1e30

kernel_name = "hybrid_dilated_retention_moe_deepnorm"


def _alibi_slopes(n):
    def pow2(m):
        start = 2.0 ** (-8.0 / m)
        return [start ** (i + 1) for i in range(m)]
    if math.log2(n).is_integer():
        s = pow2(n)
    else:
        c = 2 ** math.floor(math.log2(n))
        s = pow2(c) + pow2(2 * c)[0::2][: n - c]
    return sorted(s, reverse=True)


def _layer_norm(x, g, b):
    xf = x.astype(jnp.float32)
    mu = xf.mean(-1, keepdims=True)
    var = jnp.square(xf - mu).mean(-1, keepdims=True)
    return ((xf - mu) * lax.rsqrt(var + LN_EPS) * g + b).astype(x.dtype)


def _dilated_window_group(q, k, v, slopes, window, dilation):
    B, S, h, hd = q.shape
    steps = window // dilation
    L = S // dilation
    nb = -(-L // steps)
    Lp = nb * steps

    def to_sub(t):
        t = t.reshape(B, L, dilation, h, hd).transpose(0, 2, 3, 1, 4)
        t = jnp.pad(t, ((0, 0), (0, 0), (0, 0), (0, Lp - L), (0, 0)))
        return t.reshape(B, dilation, h, nb, steps, hd)

    def with_prev(t):
        prev = jnp.pad(t[:, :, :, :-1], ((0, 0), (0, 0), (0, 0), (1, 0), (0, 0), (0, 0)))
        return jnp.concatenate([prev, t], axis=4)

    qb, kb, vb = to_sub(q), to_sub(k), to_sub(v)
    kc, vc = with_prev(kb), with_prev(vb)
    s = jnp.einsum('brhnqd,brhnkd->brhnqk', qb, kc,
                   preferred_element_type=jnp.float32) * (hd ** -0.5)
    qi = jnp.arange(steps)[:, None]
    kj = jnp.arange(2 * steps)[None, :]
    dist = steps + qi - kj
    key_pos = jnp.arange(nb)[:, None, None] * steps + kj[None] - steps
    valid = (dist >= 0) & (dist <= steps) & (key_pos >= 0)
    bias = -slopes[:, None, None, None] * (dist * dilation).astype(jnp.float32)[None, None]
    s = jnp.where(valid, s + bias, NEG_INF)
    lse = jax.nn.logsumexp(s, axis=-1)
    p = jnp.exp(s - lse[..., None])
    o = jnp.einsum('brhnqk,brhnkd->brhnqd', p.astype(v.dtype), vc)
    o = o.reshape(B, dilation, h, Lp, hd)[:, :, :, :L].transpose(0, 3, 1, 2, 4).reshape(B, S, h, hd)
    lse = lse.reshape(B, dilation, h, Lp)[..., :L].transpose(0, 3, 1, 2).reshape(B, S, h)
    return o, lse


def _dilated_attention(q, k, v, slopes):
    B, S, _ = q.shape
    shp = (B, S, N_DIL_GROUPS, HEADS_PER_GROUP, HEAD_DIM)
    qh, kh, vh = q.reshape(shp), k.reshape(shp), v.reshape(shp)
    outs, lses = [], []
    for g, (window, dil) in enumerate(DILATED_GROUPS):
        o, l = _dilated_window_group(qh[:, :, g], kh[:, :, g], vh[:, :, g],
                                     slopes[g * HEADS_PER_GROUP:(g + 1) * HEADS_PER_GROUP], window, dil)
        outs.append(o)
        lses.append(l)
    w = jax.nn.softmax(jnp.stack(lses, axis=2), axis=2)
    o = jnp.stack(outs, axis=2) * w[..., None].astype(q.dtype)
    return o.reshape(B, S, SELF_WIDTH)


def _retention(q, k, v, g, gn_gain, log_gamma):
    B, S, _ = q.shape
    H, hd, C = N_RET_HEADS, HEAD_DIM, RET_CHUNK
    n = S // C

    def heads(t):
        return t.astype(jnp.float32).reshape(B, n, C, H, hd)

    qc, kc, vc = heads(q), heads(k) * (hd ** -0.5), heads(v)
    idx = jnp.arange(C, dtype=jnp.float32)
    diff = idx[:, None] - idx[None, :]
    decay = jnp.where(diff >= 0, jnp.exp(jnp.maximum(diff, 0.0)[None] * log_gamma[:, None, None]), 0.0)
    scores = jnp.einsum('bnqhd,bnkhd->bnhqk', qc, kc) * decay
    intra = jnp.einsum('bnhqk,bnkhd->bnqhd', scores, vc)
    k_decay = jnp.exp((C - 1 - idx)[:, None] * log_gamma[None, :])
    chunk_kv = jnp.einsum('bnkhd,kh,bnkhe->nbhde', kc, k_decay, vc)
    chunk_decay = jnp.exp(C * log_gamma)[:, None, None]

    def step(state, kv):
        return state * chunk_decay + kv, state

    _, prev = lax.scan(step, jnp.zeros((B, H, hd, hd), jnp.float32), chunk_kv)
    q_decay = jnp.exp((idx + 1.0)[:, None] * log_gamma[None, :])
    cross = jnp.einsum('bnqhd,qh,nbhde->bnqhe', qc, q_decay, prev)
    r = (intra + cross).reshape(B, S, H, hd)
    mu = r.mean(-1, keepdims=True)
    var = jnp.square(r - mu).mean(-1, keepdims=True)
    r = ((r - mu) * lax.rsqrt(var + LN_EPS)).reshape(B, S, H * hd) * gn_gain.astype(jnp.float32)
    return (jax.nn.silu(g.astype(jnp.float32)) * r).astype(q.dtype)


def _memory_attention(q, mem_k, mem_v):
    B, S, _ = q.shape
    M = mem_k.shape[1]
    qh = q.reshape(B, S, MEM_HEADS, HEAD_DIM)
    kh = mem_k.reshape(B, M, MEM_HEADS, HEAD_DIM)
    vh = mem_v.reshape(B, M, MEM_HEADS, HEAD_DIM)
    s = jnp.einsum('bshd,bmhd->bhsm', qh, kh, preferred_element_type=jnp.float32) * (HEAD_DIM ** -0.5)
    p = jax.nn.softmax(s, axis=-1).astype(q.dtype)
    return jnp.einsum('bhsm,bmhd->bshd', p, vh).reshape(B, S, MEM_WIDTH)


def _moe(h, layer, router_w, router_b, w_in, b_in, w_out, b_out):
    B, S, D = h.shape
    N = B * S
    xf = h.reshape(N, D)
    logits = (xf @ router_w[layer] + router_b[layer]).astype(jnp.float32)
    top_v, top_e = lax.top_k(logits, TOP_K)
    gates = jax.nn.softmax(top_v, axis=-1)
    e_flat = top_e.reshape(-1)
    g_flat = gates.reshape(-1)
    n_assign = N * TOP_K
    order = jnp.argsort(e_flat)
    e_sorted = e_flat[order]
    tok_sorted = (order // TOP_K).astype(jnp.int32)
    counts = jnp.bincount(e_flat, length=N_EXPERTS)
    starts = jnp.cumsum(counts) - counts
    padded = (counts + MOE_BLOCK - 1) // MOE_BLOCK * MOE_BLOCK
    pad_ends = jnp.cumsum(padded)
    pad_starts = pad_ends - padded
    dest = (pad_starts[e_sorted] + jnp.arange(n_assign) - starts[e_sorted]).astype(jnp.int32)
    n_blocks = (n_assign + N_EXPERTS * (MOE_BLOCK - 1) + MOE_BLOCK - 1) // MOE_BLOCK
    row_tok = jnp.full((n_blocks * MOE_BLOCK,), N, jnp.int32).at[dest].set(tok_sorted)
    block_e = jnp.minimum(jnp.searchsorted(pad_ends, jnp.arange(n_blocks) * MOE_BLOCK, side='right'),
                          N_EXPERTS - 1).astype(jnp.int32)
    x_rows = jnp.concatenate([xf, jnp.zeros((1, D), xf.dtype)], 0)[row_tok].reshape(n_blocks, MOE_BLOCK, D)

    def expert_block(args):
        xb, e = args
        hcat = xb @ w_in[layer, e] + b_in[layer, e]
        gate, lin = hcat[:, :D_EXPERT], hcat[:, D_EXPERT:]
        gate = jnp.minimum(gate, SWIGLU_LIMIT)
        lin = jnp.clip(lin, -SWIGLU_LIMIT, SWIGLU_LIMIT)
        act = gate * jax.nn.sigmoid(SWIGLU_ALPHA * gate) * (lin + 1.0)
        return act @ w_out[layer, e] + b_out[layer, e]

    y_rows = lax.map(expert_block, (x_rows, block_e)).reshape(-1, D)
    y_sel = y_rows[dest] * g_flat[order][:, None].astype(y_rows.dtype)
    out = jax.ops.segment_sum(y_sel, tok_sorted, num_segments=N)
    return out.reshape(B, S, D).astype(h.dtype)


def setup_inputs(seed: int = 0) -> dict:
    key = jax.random.key(seed)
    ks = jax.random.split(key, 20)
    n_dil = (DEPTH + 1) // 2
    n_ret = DEPTH // 2
    beta = DEEPNORM_BETA
    W, MW, D = SELF_WIDTH, MEM_WIDTH, D_MODEL

    def normal(k, shape, scale):
        return jax.random.normal(k, shape, jnp.float32) * scale

    col_dil = jnp.concatenate([jnp.ones((2 * W,)), jnp.full((W,), beta), jnp.ones((MW,))]).astype(jnp.float32)
    col_ret = jnp.concatenate([jnp.ones((2 * W,)), jnp.full((W,), beta), jnp.ones((W + MW,))]).astype(jnp.float32)
    col_mem = jnp.concatenate([jnp.ones((MW,)), jnp.full((MW,), beta)]).astype(jnp.float32)
    return {
        "x": normal(ks[0], (BATCH, SEQ, D), 1.0),
        "mem": normal(ks[1], (BATCH, MEM_LEN, D), 1.0),
        "w_in_dil": normal(ks[2], (n_dil, D, 3 * W + MW), D ** -0.5) * col_dil,
        "w_in_ret": normal(ks[3], (n_ret, D, 4 * W + MW), D ** -0.5) * col_ret,
        "ret_gn_g": 1.0 + normal(ks[4], (n_ret, W), 0.02),
        "w_mem_kv": normal(ks[5], (DEPTH, D, 2 * MW), D ** -0.5) * col_mem,
        "w_mix_out": normal(ks[6], (DEPTH, MIX_WIDTH, D), beta * MIX_WIDTH ** -0.5),
        "ln_mix_g": 1.0 + normal(ks[7], (DEPTH, D), 0.02),
        "ln_mix_b": normal(ks[8], (DEPTH, D), 0.01),
        "router_w": normal(ks[9], (DEPTH, D, N_EXPERTS), D ** -0.5),
        "router_b": normal(ks[10], (DEPTH, N_EXPERTS), 0.01),
        "moe_w_in": normal(ks[11], (DEPTH, N_EXPERTS, D, 2 * D_EXPERT), D ** -0.5),
        "moe_b_in": normal(ks[12], (DEPTH, N_EXPERTS, 2 * D_EXPERT), 0.02),
        "moe_w_out": normal(ks[13], (DEPTH, N_EXPERTS, D_EXPERT, D), beta * D_EXPERT ** -0.5),
        "moe_b_out": normal(ks[14], (DEPTH, N_EXPERTS, D), 0.01),
        "ln_ffn_g": 1.0 + normal(ks[15], (DEPTH, D), 0.02),
        "ln_ffn_b": normal(ks[16], (DEPTH, D), 0.01),
    }


def reference(x, mem, w_in_dil, w_in_ret, ret_gn_g, w_mem_kv, w_mix_out, ln_mix_g, ln_mix_b,
              router_w, router_b, moe_w_in, moe_b_in, moe_w_out, moe_b_out, ln_ffn_g, ln_ffn_b):
    W = SELF_WIDTH
    slopes = jnp.asarray(_alibi_slopes(N_SELF_HEADS), jnp.float32)
    log_gamma = jnp.log1p(-jnp.exp2(-(5.0 + jnp.arange(N_RET_HEADS, dtype=jnp.float32))))
    h = x
    for layer in range(DEPTH):
        kind = layer % N_MIXERS
        slot = layer // N_MIXERS
        if kind == 0:
            proj = h @ w_in_dil[slot]
            q, k, v, q_mem = jnp.split(proj, [W, 2 * W, 3 * W], axis=-1)
            self_out = _dilated_attention(q, k, v, slopes)
        else:
            proj = h @ w_in_ret[slot]
            q, k, v, g, q_mem = jnp.split(proj, [W, 2 * W, 3 * W, 4 * W], axis=-1)
            self_out = _retention(q, k, v, g, ret_gn_g[slot], log_gamma)
        mem_k, mem_v = jnp.split(mem @ w_mem_kv[layer], [MEM_WIDTH], axis=-1)
        mem_out = _memory_attention(q_mem, mem_k, mem_v)
        mix = jnp.concatenate([self_out, mem_out], axis=-1) @ w_mix_out[layer]
        h = _layer_norm(DEEPNORM_ALPHA * h + mix, ln_mix_g[layer], ln_mix_b[layer])
        ffn = _moe(h, layer, router_w, router_b, moe_w_in, moe_b_in, moe_w_out, moe_b_out)
        h = _layer_norm(DEEPNORM_ALPHA * h + ffn, ln_ffn_g[layer], ln_ffn_b[layer])
    return h
```

```python
import math
from contextlib import ExitStack

import numpy as np

import concourse.bass as bass
import concourse.mybir as mybir
from concourse.bass_utils import run_bass_kernel_spmd

F32 = mybir.dt.float32
BF16 = mybir.dt.bfloat16
I32 = mybir.dt.int32
AF = mybir.ActivationFunctionType
ALU = mybir.AluOpType
AX = mybir.AxisListType

NCORES = 8
D = 2048
NT = 1024
NTI = NT // 128
NTOK = 8192
HD = 128
W = 1536
MW = 512
NE = 32
NEL = 4
CAP = 1280
NSL = CAP // 128
NROW = NTOK + 128
ALPHA = (2 * 2) ** 0.25
LN_EPS = 1e-5
BIG = 1.0e6
SCALE = HD ** -0.5
DIL = (1, 4, 16)
TPREV = (128, 512, 2048)


def sl(start, cnt, step):
    return slice(start, start + (cnt - 1) * step + 1, step)


def _alibi_slopes(n):
    def pow2(m):
        start = 2.0 ** (-8.0 / m)
        return [start ** (i + 1) for i in range(m)]
    if math.log2(n).is_integer():
        s = pow2(n)
    else:
        c = 2 ** math.floor(math.log2(n))
        s = pow2(c) + pow2(2 * c)[0::2][: n - c]
    return sorted(s, reverse=True)


class Dep:
    __slots__ = ("w", "r", "wg")

    def __init__(self):
        self.w = {}
        self.r = {}
        self.wg = None


class Tile:
    def __init__(self, h, name, psum=False):
        self.h = h
        self.name = name
        self.psum = psum
        self.deps = {None: Dep()}

    def __getitem__(self, idx):
        return self.h[idx]


class Prog:
    def __init__(self, nc, es):
        self.nc = nc
        self.es = es
        self.engs = {"pe": nc.tensor, "act": nc.scalar, "dve": nc.vector, "pool": nc.gpsimd, "sp": nc.sync}
        self.sem = {k: es.enter_context(nc.semaphore("s_" + k)) for k in ("pe", "act", "dve", "pool")}
        self.cnt = {k: 0 for k in self.sem}
        self.pending = {k: False for k in self.sem}
        self.seen = {k: {} for k in self.engs}
        self.slots = {}
        self.rr = {}
        self.cc_sem = es.enter_context(nc.semaphore("s_cc"))
        self.cc_cnt = 0
        self.nwait = 0

    def dma_class(self, name, n):
        self.slots[name] = [[self.es.enter_context(self.nc.semaphore("d_%s%d" % (name, i))), 0, "d_%s%d" % (name, i)]
                            for i in range(n)]
        self.rr[name] = 0

    def _wait(self, eng, evs):
        seen = self.seen[eng]
        for key, (sem, val) in evs.items():
            if eng == "pe" and key == "pe":
                continue
            if seen.get(key, 0) >= val:
                continue
            self.engs[eng].wait_ge(sem, val)
            seen[key] = val
            self.nwait += 1

    @staticmethod
    def _merge(dst, src):
        for k, (s, v) in src.items():
            if k not in dst or dst[k][1] < v:
                dst[k] = (s, v)

    def _collect(self, reads, writes, accum):
        evs = {}
        for t, key in reads:
            if key is None:
                for d in t.deps.values():
                    self._merge(evs, d.w)
            else:
                self._merge(evs, t.deps[None].w)
                if key in t.deps:
                    self._merge(evs, t.deps[key].w)
        for t, key in writes:
            if key is None:
                for d in t.deps.values():
                    self._merge(evs, d.r)
                    if not (accum and d.wg == accum):
                        self._merge(evs, d.w)
            else:
                self._merge(evs, t.deps[None].w)
                self._merge(evs, t.deps[None].r)
                if key in t.deps:
                    d = t.deps[key]
                    self._merge(evs, d.r)
                    if not (accum and d.wg == accum):
                        self._merge(evs, d.w)
        return evs

    def _register(self, ev, reads, writes, accum):
        k, s, v = ev
        for t, key in reads:
            d = t.deps.setdefault(key, Dep())
            self._merge(d.r, {k: (s, v)})
        for t, key in writes:
            if key is None and not accum:
                t.deps = {None: Dep()}
                t.deps[None].w = {k: (s, v)}
            else:
                if key is None:
                    for kk in [kk for kk in t.deps if kk is not None]:
                        del t.deps[kk]
                d = t.deps.setdefault(key, Dep())
                if accum:
                    if d.wg != accum:
                        d.w = {}
                        d.r = {}
                        d.wg = accum
                    self._merge(d.w, {k: (s, v)})
                else:
                    d.w = {k: (s, v)}
                    d.r = {}
                    d.wg = None

    @staticmethod
    def _norm(reads, writes):
        r2 = [(t, k) for t, k in reads if not t.psum]
        w2 = [((t, None) if t.psum else (t, k)) for t, k in writes] + [(t, None) for t, k in reads if t.psum]
        seen, w3 = set(), []
        for t, k in w2:
            if (id(t), k) not in seen:
                seen.add((id(t), k))
                w3.append((t, k))
        return r2, w3

    def op(self, eng, fn, reads=(), writes=(), inc=True, accum=False):
        reads, writes = self._norm(reads, writes)
        self._wait(eng, self._collect(reads, writes, accum))
        ins = fn(self.engs[eng])
        if inc:
            self.cnt[eng] += 1
            ins.then_inc(self.sem[eng], 1)
            ev = (eng, self.sem[eng], self.cnt[eng])
            self.pending[eng] = False
        else:
            ev = (eng, self.sem[eng], self.cnt[eng] + 1)
            self.pending[eng] = True
        self._register(ev, reads, writes, accum)
        return ins

    def dma(self, q, cls, fn, reads=(), writes=(), accum=False):
        slot = self.slots[cls][self.rr[cls] % len(self.slots[cls])]
        self.rr[cls] += 1
        evs = self._collect(reads, writes, accum)
        if slot[1] > 0:
            self._merge(evs, {slot[2]: (slot[0], slot[1])})
        self._wait(q, evs)
        ins = fn(self.engs[q])
        slot[1] += 16
        ins.then_inc(slot[0], 16)
        self._register((slot[2], slot[0], slot[1]), reads, writes, accum)
        return ins

    def collective(self, fn, reads=(), writes=()):
        self._wait("pool", self._collect(reads, writes, False))
        ins = fn(self.engs["pool"])
        self.cc_cnt += 1
        ins.then_inc(self.cc_sem, 1)
        self._register(("cc", self.cc_sem, self.cc_cnt), reads, writes, False)

    def barrier(self):
        evs = {}
        for k in self.sem:
            assert not self.pending[k], k
            if self.cnt[k]:
                evs[k] = (self.sem[k], self.cnt[k])
        for cls in self.slots.values():
            for s in cls:
                if s[1]:
                    evs[s[2]] = (s[0], s[1])
        if self.cc_cnt:
            evs["cc"] = (self.cc_sem, self.cc_cnt)
        for e in self.engs:
            self._wait(e, evs)


class Builder:
    def __init__(self, cfg):
        self.cfg = cfg
        self.nc = bass.Bass("TRN2", target_bir_lowering=False)
        self.es = ExitStack()
        self.P = Prog(self.nc, self.es)
        for name, n in (("ld", 8), ("w", 6), ("g", 4), ("s", 8), ("sc", 8), ("st", 6)):
            self.P.dma_class(name, n)
        self.ins = {}
        self.uid = 0
        self.capreg = self.nc.gpsimd.to_reg(CAP - 1)

    def inp(self, name, shape, dt=F32):
        h = self.nc.dram_tensor(name, list(shape), dt, kind="ExternalInput")
        self.ins[name] = (tuple(shape), dt)
        return Tile(h, name)

    def outp(self, name, shape, dt=F32):
        return Tile(self.nc.dram_tensor(name, list(shape), dt, kind="ExternalOutput"), name)

    def dram(self, name, shape, dt, shared=False):
        if shared:
            h = self.nc.dram_tensor(name, list(shape), dt, addr_space="Shared")
        else:
            h = self.nc.dram_tensor(name, list(shape), dt)
        return Tile(h, name)

    def sb(self, st, name, shape, dt):
        self.uid += 1
        return Tile(st.enter_context(self.nc.sbuf_tensor("%s_%d" % (name, self.uid), list(shape), dt)), name)

    def ps(self, st, name, shape, dt):
        self.uid += 1
        return Tile(st.enter_context(self.nc.psum_tensor("%s_%d" % (name, self.uid), list(shape), dt)), name, psum=True)

    def load(self, dst, dst_ap, src, src_ap, q="sp", cls="ld", dkey=None, skey=None, accum=False):
        self.P.dma(q, cls, lambda e: e.dma_start(out=dst_ap, in_=src_ap), reads=[(src, skey)], writes=[(dst, dkey)], accum=accum)

    def layer_norm_tile(self, st_tiles, r, rkey, g_bc, b_bc, out, out_ap, okey):
        P = self.P
        stats, mv, rstd, xn = st_tiles
        for k in range(4):
            P.op("dve", lambda e, k=k: e.bn_stats(out=stats[:, k, :], in_=r[:, k * 512:(k + 1) * 512]),
                 reads=[(r, rkey)], writes=[(stats, k)])
        P.op("dve", lambda e: e.bn_aggr(out=mv[:, :], in_=stats[:, :, :]), reads=[(stats, None)], writes=[(mv, None)])
        P.op("dve", lambda e: e.tensor_scalar(out=rstd[:, :], in0=mv[:, 1:2], scalar1=LN_EPS, scalar2=None, op0=ALU.add),
             reads=[(mv, None)], writes=[(rstd, None)])
        P.op("act", lambda e: e.activation(out=rstd[:, :], in_=rstd[:, :], func=AF.Sqrt), reads=[(rstd, None)], writes=[(rstd, None)])
        P.op("dve", lambda e: e.reciprocal(out=rstd[:, :], in_=rstd[:, :]), reads=[(rstd, None)], writes=[(rstd, None)])
        P.op("dve", lambda e: e.tensor_scalar(out=xn[:, :], in0=r[:, :], scalar1=mv[:, 0:1], scalar2=rstd[:, 0:1],
                                                 op0=ALU.subtract, op1=ALU.mult),
             reads=[(r, rkey), (mv, None), (rstd, None)], writes=[(xn, None)])
        P.op("pool", lambda e: e.tensor_tensor(out=xn[:, :], in0=xn[:, :], in1=g_bc[:, :], op=ALU.mult),
             reads=[(xn, None), (g_bc, None)], writes=[(xn, None)])
        P.op("dve", lambda e: e.tensor_tensor(out=out_ap, in0=xn[:, :], in1=b_bc[:, :], op=ALU.add),
             reads=[(xn, None), (b_bc, None)], writes=[(out, okey)])

    def consts(self):
        P, I = self.P, self.I
        cs = self.es
        C = {}
        I["ident"] = self.inp("ident", [128, 128])
        C["ident_f"] = self.sb(cs, "ident_f", [128, 128], F32)
        C["ident_b"] = self.sb(cs, "ident_b", [128, 128], BF16)
        C["ones_f"] = self.sb(cs, "ones_f", [128, 128], F32)
        C["ones_b"] = self.sb(cs, "ones_b", [128, 128], BF16)
        self.load(C["ident_f"], C["ident_f"][:, :], I["ident"], I["ident"][:, :])
        P.op("dve", lambda e: e.tensor_copy(out=C["ident_b"][:, :], in_=C["ident_f"][:, :]),
             reads=[(C["ident_f"], None)], writes=[(C["ident_b"], None)])
        P.op("dve", lambda e: e.memset(C["ones_f"][:, :], 1.0), writes=[(C["ones_f"], None)])
        P.op("dve", lambda e: e.memset(C["ones_b"][:, :], 1.0), writes=[(C["ones_b"], None)])
        self.C = C

    def build(self):
        kind = self.cfg["kind"]
        self.I = {}
        self.dbg = {}
        self.consts()
        if kind == "A":
            self.build_A()
        elif kind == "B":
            self.build_B()
        else:
            self.build_C()
        self.P.barrier()
        return self.nc

    def build_A(self):
        nc, P, I, C = self.nc, self.P, self.I, self.C
        L = self.cfg["L"]
        I["x_own"] = self.inp("x_own", [NT, D])
        I["router_w"] = self.inp("router_w", [D, NE])
        I["router_b"] = self.inp("router_b", [1, NE])
        h1o = self.outp("h1_out", [NT, D])
        h1b = self.outp("h1b_out", [NT, D], BF16)
        go = self.outp("g_out", [NT, NE])
        self.h1o = h1o
        if self.cfg.get("mixer", True):
            if L == 0:
                self.mixer_dil()
            else:
                self.mixer_ret()
        else:
            self.P.dma("sp", "st", lambda e: e.dma_start(out=h1o[:, :], in_=I["x_own"][:, :]),
                       reads=[(I["x_own"], None)], writes=[(h1o, None)])
        P.barrier()
        hbuf = h1o
        with ExitStack() as st:
            hf = [self.sb(st, "hf", [128, D], F32) for _ in range(2)]
            hb = [self.sb(st, "hb", [128, D], BF16) for _ in range(2)]
            hT = [self.sb(st, "hT", [128, 16, 128], F32) for _ in range(2)]
            rw = self.sb(st, "rw", [128, 16, NE], F32)
            rb = self.sb(st, "rb", [1, NE], F32)
            lg = self.sb(st, "lg", [128, NTI, NE], F32)
            G = self.sb(st, "G", [128, NTI, NE], F32)
            ex = self.sb(st, "ex", [128, NTI, NE], F32)
            msk = self.sb(st, "msk", [128, NTI, NE], F32)
            m8 = self.sb(st, "m8", [128, NTI, 8], F32)
            nm = self.sb(st, "nm", [128, NTI], F32)
            ssum = self.sb(st, "ssum", [128, NTI], F32)
            ptr = [self.ps(st, "ptr", [128, 4, 128], F32) for _ in range(2)]
            plg = self.ps(st, "plg", [128, NTI, NE], F32)
            with nc.allow_non_contiguous_dma(reason="router weights 128B runs"):
                self.load(rw, rw[:, :, :], I["router_w"], I["router_w"].h.ap().rearrange("(c p) e -> p c e", p=128))
            self.load(rb, rb[:, :], I["router_b"], I["router_b"][:, :])
            for i in range(NTI):
                f, b_, t_ = hf[i % 2], hb[i % 2], hT[i % 2]
                self.load(f, f[:, :], hbuf, hbuf[i * 128:(i + 1) * 128, :])
                P.op("act", lambda e, f=f, b_=b_: e.activation(out=b_[:, :], in_=f[:, :], func=AF.Copy),
                     reads=[(f, None)], writes=[(b_, None)])
                self.P.dma("sp", "st", lambda e, b_=b_, i=i: e.dma_start(out=h1b[i * 128:(i + 1) * 128, :], in_=b_[:, :]),
                           reads=[(b_, None)], writes=[(h1b, i)])
                for cg in range(4):
                    pt = ptr[cg % 2]
                    for k in range(4):
                        c = cg * 4 + k
                        P.op("pe", lambda e, pt=pt, k=k, c=c, f=f: e.transpose(out=pt[:, k, :], in_=f[:, c * 128:(c + 1) * 128],
                                                                              identity=C["ident_f"][:, :]),
                             reads=[(f, None), (C["ident_f"], None)], writes=[(pt, k)])
                    if cg % 2 == 0:
                        P.op("dve", lambda e, pt=pt, t_=t_, cg=cg: e.tensor_copy(out=t_[:, cg * 4:(cg + 1) * 4, :], in_=pt[:, :, :]),
                             reads=[(pt, None)], writes=[(t_, cg)])
                    else:
                        P.op("act", lambda e, pt=pt, t_=t_, cg=cg: e.activation(out=t_[:, cg * 4:(cg + 1) * 4, :], in_=pt[:, :, :], func=AF.Copy),
                             reads=[(pt, None)], writes=[(t_, cg)])
                for c in range(16):
                    P.op("pe", lambda e, t_=t_, c=c, i=i: e.matmul(out=plg[:, i, :], lhsT=t_[:, c, :], rhs=rw[:, c, :],
                                                                   start=(c == 0), stop=False),
                         reads=[(t_, c // 4), (rw, None)], writes=[(plg, i)], inc=False)
                P.op("pe", lambda e, i=i: e.matmul(out=plg[:, i, :], lhsT=C["ones_f"][0:1, :], rhs=rb[0:1, :], start=False, stop=True),
                     reads=[(C["ones_f"], None), (rb, None)], writes=[(plg, i)])
            P.op("act", lambda e: e.activation(out=lg[:, :, :], in_=plg[:, :, :], func=AF.Copy), reads=[(plg, None)], writes=[(lg, None)])
            for i in range(NTI):
                P.op("dve", lambda e, i=i: e.max(out=m8[:, i, :], in_=lg[:, i, :]), reads=[(lg, None)], writes=[(m8, i)])
                P.op("dve", lambda e, i=i: e.tensor_scalar(out=msk[:, i, :], in0=lg[:, i, :], scalar1=m8[:, i, 3:4], scalar2=None,
                                                             op0=ALU.is_ge), reads=[(lg, None), (m8, i)], writes=[(msk, i)])
                P.op("dve", lambda e, i=i: e.tensor_scalar(out=nm[:, i:i + 1], in0=m8[:, i, 0:1], scalar1=-1.0, scalar2=None, op0=ALU.mult),
                     reads=[(m8, i)], writes=[(nm, i)])
                P.op("act", lambda e, i=i: e.activation(out=ex[:, i, :], in_=lg[:, i, :], func=AF.Exp, bias=nm[:, i:i + 1], scale=1.0),
                     reads=[(lg, None), (nm, i)], writes=[(ex, i)])
                P.op("dve", lambda e, i=i: e.tensor_tensor(out=ex[:, i, :], in0=ex[:, i, :], in1=msk[:, i, :], op=ALU.mult),
                     reads=[(ex, i), (msk, i)], writes=[(ex, i)])
                P.op("dve", lambda e, i=i: e.reduce_sum(out=ssum[:, i:i + 1], in_=ex[:, i, :], axis=AX.X),
                     reads=[(ex, i)], writes=[(ssum, i)])
                P.op("dve", lambda e, i=i: e.reciprocal(out=ssum[:, i:i + 1], in_=ssum[:, i:i + 1]),
                     reads=[(ssum, i)], writes=[(ssum, i)])
                P.op("dve", lambda e, i=i: e.tensor_scalar(out=G[:, i, :], in0=ex[:, i, :], scalar1=ssum[:, i:i + 1], scalar2=None, op0=ALU.mult),
                     reads=[(ex, i), (ssum, i)], writes=[(G, i)])
            self.P.dma("sp", "st", lambda e: e.dma_start(out=go.h.ap().rearrange("(i p) e -> p i e", p=128), in_=G[:, :, :]),
                       reads=[(G, None)], writes=[(go, None)])
            P.barrier()

    def build_B(self):
        nc, P, I, C = self.nc, self.P, self.I, self.C
        NL = self.cfg.get("nel", NEL)
        I["h1all"] = self.inp("h1all", [NROW, D], BF16)
        I["gall"] = self.inp("gall", [NTOK, NE])
        I["moe_win"] = self.inp("moe_win", [NL, 8, 128, 16, 512])
        I["moe_wout"] = self.inp("moe_wout", [NL, 4, 128, 16, 512])
        I["moe_bin"] = self.inp("moe_bin", [128, NL, 32])
        I["moe_bout"] = self.inp("moe_bout", [1, NL, D])
        I["psel"] = self.inp("psel", [128, NL, NE])
        I["padinit"] = self.inp("padinit", [128, NSL, 2])
        I["tokidf"] = self.inp("tokidf", [128, 64])
        I["tri"] = self.inp("tri", [128, 128])
        ypart = self.outp("ypart", [NROW * 4, 512])
        idxb = [self.dram("idxb%d" % j, [CAP, 2], F32) for j in range(NL)]
        h1all = I["h1all"]

        zb = self.sb(self.es, "zero_f", [128, 2048], F32)
        P.op("pool", lambda e: e.memset(zb[:, :], 0.0), writes=[(zb, None)])
        ypv = ypart.h.ap().rearrange("(a p r) n -> a p (r n)", p=128, r=4)
        for a in range(NROW // 128):
            self.P.dma("sp", "st", lambda e, a=a: e.dma_start(out=ypv[a], in_=zb[:, :]),
                       reads=[(zb, None)], writes=[(ypart, None)], accum="z")

        with ExitStack() as st:
            NLA = max(NL, 2)
            Gl = self.sb(st, "Gl", [128, 64, NE], F32)
            tmp = self.sb(st, "tmp", [128, 64, NE], F32)
            psel = self.sb(st, "psel", [128, NL, NE], F32)
            tokf = self.sb(st, "tokf", [128, 64], F32)
            tri = self.sb(st, "tri", [128, 128], F32)
            Gmy = self.sb(st, "Gmy", [128, NLA, 64], F32)
            mk = self.sb(st, "mk", [128, NLA, 64], F32)
            sA = self.sb(st, "sA", [128, NLA, 64], F32)
            sB = self.sb(st, "sB", [128, NLA, 64], F32)
            pos = self.sb(st, "pos", [128, NLA, 64], F32)
            posi = self.sb(st, "posi", [128, NLA, 64], I32)
            vals = self.sb(st, "vals", [128, NLA, 64, 2], F32)
            padi = self.sb(st, "padi", [128, NSL, 2], F32)
            ppw = self.ps(st, "ppw", [128, NLA * 64], F32)
            ptot = self.ps(st, "ptot", [128, NLA * 64], F32)
            with nc.allow_non_contiguous_dma(reason="gate matrix 128B runs"):
                self.load(Gl, Gl[:, :, :], I["gall"], I["gall"].h.ap().rearrange("(c p) e -> p c e", p=128))
            self.load(psel, psel[:, :, :], I["psel"], I["psel"][:, :, :])
            self.load(tokf, tokf[:, :], I["tokidf"], I["tokidf"][:, :])
            self.load(tri, tri[:, :], I["tri"], I["tri"][:, :])
            self.load(padi, padi[:, :, :], I["padinit"], I["padinit"][:, :, :])
            for j in range(NL):
                iv = idxb[j]
                with nc.allow_non_contiguous_dma(reason="8B rows"):
                    self.P.dma("sp", "st", lambda e, iv=iv: e.dma_start(out=iv.h.ap().rearrange("(i p) t -> p i t", p=128), in_=padi[:, :, :]),
                               reads=[(padi, None)], writes=[(iv, None)])
                P.op("dve", lambda e, j=j: e.tensor_tensor(out=tmp[:, :, :], in0=Gl[:, :, :],
                                                             in1=psel[:, j:j + 1, :].to_broadcast([128, 64, NE]), op=ALU.mult),
                     reads=[(Gl, None), (psel, None)], writes=[(tmp, None)])
                P.op("dve", lambda e, j=j: e.reduce_sum(out=Gmy[:, j, :], in_=tmp[:, :, :], axis=AX.X),
                     reads=[(tmp, None)], writes=[(Gmy, j)])
            if NLA > NL:
                P.op("dve", lambda e: e.memset(Gmy[:, NL:, :], 0.0), writes=[(Gmy, "pad")])
            P.op("dve", lambda e: e.tensor_single_scalar(out=mk[:, :, :], in_=Gmy[:, :, :], scalar=0.0, op=ALU.is_gt),
                 reads=[(Gmy, None)], writes=[(mk, None)])
            mk2 = mk.h.ap().rearrange("p j c -> p (j c)")
            P.op("pe", lambda e: e.matmul(out=ppw[:, :], lhsT=tri[:, :], rhs=mk2, start=True, stop=True),
                 reads=[(tri, None), (mk, None)], writes=[(ppw, None)])
            P.op("pe", lambda e: e.matmul(out=ptot[:, :], lhsT=C["ones_f"][:, :], rhs=mk2, start=True, stop=True),
                 reads=[(C["ones_f"], None), (mk, None)], writes=[(ptot, None)])
            P.op("act", lambda e: e.activation(out=sA.h.ap().rearrange("p j c -> p (j c)"), in_=ptot[:, :], func=AF.Copy),
                 reads=[(ptot, None)], writes=[(sA, None)])
            a, b_ = sA, sB
            s = 1
            while s < 64:
                P.op("dve", lambda e, a=a, b_=b_, s=s: e.tensor_tensor(out=b_[:, :, s:], in0=a[:, :, s:], in1=a[:, :, :64 - s], op=ALU.add),
                     reads=[(a, None)], writes=[(b_, "hi")])
                P.op("pool", lambda e, a=a, b_=b_, s=s: e.tensor_copy(out=b_[:, :, :s], in_=a[:, :, :s]),
                     reads=[(a, None)], writes=[(b_, "lo")])
                a, b_ = b_, a
                s *= 2
            flat = lambda t: t.h.ap().rearrange("p j c -> p (j c)")
            P.op("dve", lambda e, a=a: e.tensor_tensor(out=flat(pos), in0=flat(a), in1=ptot[:, :], op=ALU.subtract),
                 reads=[(a, None), (ptot, None)], writes=[(pos, None)])
            P.op("dve", lambda e: e.tensor_tensor(out=flat(pos), in0=flat(pos), in1=ppw[:, :], op=ALU.add),
                 reads=[(pos, None), (ppw, None)], writes=[(pos, None)])
            P.op("dve", lambda e: e.tensor_scalar(out=mk[:, :, :], in0=mk[:, :, :], scalar1=-BIG, scalar2=BIG, op0=ALU.mult, op1=ALU.add),
                 reads=[(mk, None)], writes=[(mk, None)])
            P.op("dve", lambda e: e.tensor_tensor(out=pos[:, :, :], in0=pos[:, :, :], in1=mk[:, :, :], op=ALU.add),
                 reads=[(pos, None), (mk, None)], writes=[(pos, None)])
            P.op("dve", lambda e: e.tensor_copy(out=posi[:, :, :], in_=pos[:, :, :]), reads=[(pos, None)], writes=[(posi, None)])
            for j in range(NL):
                P.op("pool", lambda e, j=j: e.tensor_copy(out=vals[:, j, :, 0], in_=tokf[:, :]), reads=[(tokf, None)], writes=[(vals, (j, 0))])
                P.op("pool", lambda e, j=j: e.tensor_copy(out=vals[:, j, :, 1], in_=Gmy[:, j, :]), reads=[(Gmy, None)], writes=[(vals, (j, 1))])
            if "pos" in self.cfg.get("debug", ()):
                self.debug_out("dbg_pos", pos, [128, NLA, 64])
            for j in range(NL):
                iv = idxb[j]
                for c in range(64):
                    P.dma("pool", "sc", lambda e, j=j, c=c, iv=iv: e.indirect_dma_start(
                        out=iv[:, :], out_offset=bass.IndirectOffsetOnAxis(ap=posi[:, j, c:c + 1], axis=0),
                        in_=vals[:, j, c, :], in_offset=None, bounds_check=self.capreg, oob_is_err=False),
                        reads=[(vals, None), (posi, None)], writes=[(iv, None)], accum="sc")
            P.barrier()

        with ExitStack() as st:
            xeT = self.sb(st, "xeT", [128, 16, CAP], BF16)
            actT = self.sb(st, "actT", [128, 16, CAP], BF16)
            wp = [self.sb(st, "wp", [128, 16, 512], BF16) for _ in range(2)]
            wo = [self.sb(st, "wo", [128, 16, 512], BF16) for _ in range(2)]
            xg = [self.sb(st, "xg", [128, D], BF16) for _ in range(2)]
            itl = [self.sb(st, "itl", [128, 2], F32) for _ in range(2)]
            idi = [self.sb(st, "idi", [128, 1], I32) for _ in range(2)]
            id8f = [self.sb(st, "id8f", [128, 4], F32) for _ in range(NSL)]
            id8 = [self.sb(st, "id8", [128, 4], I32) for _ in range(NSL)]
            io8 = self.sb(st, "io8", [128, 4], F32)
            t8 = [self.sb(st, "t8", [128, 1], F32) for _ in range(2)]
            gat = self.sb(st, "gat", [128, NSL], F32)
            bin_t = self.sb(st, "bin_t", [128, NL, 32], F32)
            bin1 = self.sb(st, "bin1", [128, NL, 16], F32)
            bout = self.sb(st, "bout", [1, NL, D], BF16)
            gc = [self.sb(st, "gc", [128, 512], F32) for _ in range(2)]
            sg = [self.sb(st, "sg", [128, 512], F32) for _ in range(2)]
            lc = [self.sb(st, "lc", [128, 512], F32) for _ in range(2)]
            ysc = [self.sb(st, "ysc", [128, 512], F32) for _ in range(4)]
            pT = [self.ps(st, "pT", [128, 8, 128], BF16) for _ in range(2)]
            pg = [self.ps(st, "pg", [128, 512], F32) for _ in range(2)]
            pl = [self.ps(st, "pl", [128, 512], F32) for _ in range(2)]
            py = [self.ps(st, "py", [128, 512], F32) for _ in range(2)]

            self.load(bin_t, bin_t[:, :, :], I["moe_bin"], I["moe_bin"][:, :, :])
            P.op("dve", lambda e: e.tensor_scalar(out=bin1[:, :, :], in0=bin_t[:, :, 16:32], scalar1=1.0, scalar2=None, op0=ALU.add),
                 reads=[(bin_t, None)], writes=[(bin1, None)])
            self.load(bout, bout.h.ap().rearrange("o j (a n) -> o (j a) n", a=4), I["moe_bout"],
                      I["moe_bout"].h.ap().rearrange("o j (a n) -> o (j a) n", a=4), q="pool", cls="w")
            for k in range(4):
                P.op("dve", lambda e, k=k: e.memset(io8[:, k:k + 1], float(k)), writes=[(io8, k)])
            n_sw = 0
            n_y = 0
            batches = [(0, 512), (512, 512), (1024, 256)]
            for j in range(NL):
                iv = idxb[j]
                for i in range(NSL):
                    it_, ii_, x_ = itl[i % 2], idi[i % 2], xg[i % 2]
                    self.load(it_, it_[:, :], iv, iv[i * 128:(i + 1) * 128, :])
                    P.op("dve", lambda e, it_=it_, ii_=ii_: e.tensor_copy(out=ii_[:, :], in_=it_[:, 0:1]), reads=[(it_, None)], writes=[(ii_, None)])
                    P.op("dve", lambda e, it_=it_, i=i: e.tensor_copy(out=gat[:, i:i + 1], in_=it_[:, 1:2]), reads=[(it_, None)], writes=[(gat, i)])
                    P.op("dve", lambda e, it_=it_, i=i: e.tensor_scalar(out=t8[i % 2][:, :], in0=it_[:, 0:1], scalar1=4.0, scalar2=None, op0=ALU.mult),
                         reads=[(it_, None)], writes=[(t8[i % 2], None)])
                    P.op("dve", lambda e, i=i: e.tensor_scalar(out=id8f[i][:, :], in0=io8[:, :], scalar1=t8[i % 2][:, 0:1], scalar2=None, op0=ALU.add),
                         reads=[(t8[i % 2], None), (io8, None)], writes=[(id8f[i], None)])
                    P.op("dve", lambda e, i=i: e.tensor_copy(out=id8[i][:, :], in_=id8f[i][:, :]), reads=[(id8f[i], None)], writes=[(id8[i], None)])
                    P.dma("pool", "g", lambda e, x_=x_, ii_=ii_: e.indirect_dma_start(
                        out=x_[:, :], out_offset=None, in_=h1all[:, :],
                        in_offset=bass.IndirectOffsetOnAxis(ap=ii_[:, 0:1], axis=0)),
                        reads=[(h1all, None), (ii_, None)], writes=[(x_, None)])
                    for cg in range(2):
                        pt = pT[cg % 2]
                        for k in range(8):
                            c = cg * 8 + k
                            P.op("pe", lambda e, pt=pt, k=k, c=c, x_=x_: e.transpose(out=pt[:, k, :], in_=x_[:, c * 128:(c + 1) * 128],
                                                                                  identity=C["ident_b"][:, :]),
                                 reads=[(x_, None), (C["ident_b"], None)], writes=[(pt, k)])
                        if cg == 0:
                            P.op("dve", lambda e, pt=pt, i=i: e.tensor_copy(out=xeT[:, 0:8, i * 128:(i + 1) * 128], in_=pt[:, :, :]),
                                 reads=[(pt, None)], writes=[(xeT, i)], accum="x%d" % j)
                        else:
                            P.op("act", lambda e, pt=pt, i=i: e.activation(out=xeT[:, 8:16, i * 128:(i + 1) * 128], in_=pt[:, :, :], func=AF.Copy),
                                 reads=[(pt, None)], writes=[(xeT, i)], accum="x%d" % j)
                for k in range(8):
                    w_ = wp[k % 2]
                    self.load(w_, w_[:, :, :], I["moe_win"], I["moe_win"].h.ap()[j, k], q="pool", cls="w")
                    for jj in range(2):
                        m = 2 * k + jj
                        for bi, (s0, nb) in enumerate(batches):
                            g_, l_ = pg[n_sw % 2], pl[n_sw % 2]
                            gc_, sg_, lc_ = gc[n_sw % 2], sg[n_sw % 2], lc[n_sw % 2]
                            n_sw += 1
                            rk = [(xeT, t) for t in range(s0 // 128, (s0 + nb) // 128)]
                            for c in range(16):
                                P.op("pe", lambda e, g_=g_, w_=w_, c=c, jj=jj, s0=s0, nb=nb: e.matmul(
                                    out=g_[:, 0:nb], lhsT=w_[:, c, jj * 128:(jj + 1) * 128], rhs=xeT[:, c, s0:s0 + nb],
                                    start=(c == 0), stop=(c == 15)), reads=[(w_, None)] + rk, writes=[(g_, None)], inc=(c == 15))
                            for c in range(16):
                                P.op("pe", lambda e, l_=l_, w_=w_, c=c, jj=jj, s0=s0, nb=nb: e.matmul(
                                    out=l_[:, 0:nb], lhsT=w_[:, c, 256 + jj * 128:256 + (jj + 1) * 128], rhs=xeT[:, c, s0:s0 + nb],
                                    start=(c == 0), stop=(c == 15)), reads=[(w_, None)] + rk, writes=[(l_, None)], inc=(c == 15))
                            P.op("dve", lambda e, g_=g_, gc_=gc_, m=m, nb=nb, j=j: e.tensor_scalar(
                                out=gc_[:, 0:nb], in0=g_[:, 0:nb], scalar1=bin_t[:, j, m:m + 1], scalar2=7.0, op0=ALU.add, op1=ALU.min),
                                reads=[(g_, None), (bin_t, None)], writes=[(gc_, None)])
                            P.op("act", lambda e, gc_=gc_, sg_=sg_, nb=nb: e.activation(out=sg_[:, 0:nb], in_=gc_[:, 0:nb], func=AF.Sigmoid, scale=1.702),
                                 reads=[(gc_, None)], writes=[(sg_, None)])
                            P.op("dve", lambda e, l_=l_, lc_=lc_, m=m, nb=nb, j=j: e.tensor_scalar(
                                out=lc_[:, 0:nb], in0=l_[:, 0:nb], scalar1=bin1[:, j, m:m + 1], scalar2=8.0, op0=ALU.add, op1=ALU.min),
                                reads=[(l_, None), (bin1, None)], writes=[(lc_, None)])
                            P.op("dve", lambda e, gc_=gc_, lc_=lc_, nb=nb: e.scalar_tensor_tensor(
                                out=lc_[:, 0:nb], in0=lc_[:, 0:nb], scalar=-6.0, in1=gc_[:, 0:nb], op0=ALU.max, op1=ALU.mult),
                                reads=[(gc_, None), (lc_, None)], writes=[(lc_, None)])
                            P.op("dve", lambda e, sg_=sg_, lc_=lc_, m=m, s0=s0, nb=nb: e.tensor_tensor(
                                out=actT[:, m, s0:s0 + nb], in0=lc_[:, 0:nb], in1=sg_[:, 0:nb], op=ALU.mult),
                                reads=[(sg_, None), (lc_, None)], writes=[(actT, bi)], accum="a%d" % j)
                for m2 in range(4):
                    o_ = wo[m2 % 2]
                    self.load(o_, o_[:, :, :], I["moe_wout"], I["moe_wout"].h.ap()[j, m2], q="pool", cls="w")
                    for i in range(NSL):
                        y_ = py[n_y % 2]
                        ys_ = ysc[n_y % 4]
                        n_y += 1
                        bi = 0 if i < 4 else (1 if i < 8 else 2)
                        for c in range(16):
                            P.op("pe", lambda e, y_=y_, o_=o_, c=c, i=i: e.matmul(
                                out=y_[:, :], lhsT=actT[:, c, i * 128:(i + 1) * 128], rhs=o_[:, c, :], start=(c == 0), stop=False),
                                reads=[(actT, bi), (o_, None)], writes=[(y_, None)], inc=False)
                        P.op("pe", lambda e, y_=y_, m2=m2, j=j: e.matmul(
                            out=y_[:, :], lhsT=C["ones_b"][0:1, :], rhs=bout[0:1, j, m2 * 512:(m2 + 1) * 512], start=False, stop=True),
                            reads=[(C["ones_b"], None), (bout, None)], writes=[(y_, None)])
                        P.op("act", lambda e, y_=y_, ys_=ys_, i=i: e.activation(out=ys_[:, :], in_=y_[:, :], func=AF.Copy, scale=gat[:, i:i + 1]),
                             reads=[(y_, None), (gat, i)], writes=[(ys_, None)])
                        P.dma("pool", "s", lambda e, ys_=ys_, i=i, m2=m2: e.indirect_dma_start(
                            out=ypart[:, :], out_offset=bass.IndirectOffsetOnAxis(ap=id8[i][:, m2:m2 + 1], axis=0),
                            in_=ys_[:, :], in_offset=None, compute_op=ALU.add),
                            reads=[(ys_, None), (id8[i], None)], writes=[(ypart, None)], accum="y%d" % j)
            P.barrier()

    def build_C(self):
        nc, P, I, C = self.nc, self.P, self.I, self.C
        I["parts"] = self.inp("parts", [NCORES, NT, D])
        I["h1"] = self.inp("h1", [NT, D])
        I["ln_g"] = self.inp("ln_g", [1, D])
        I["ln_b"] = self.inp("ln_b", [1, D])
        h2 = self.outp("h2_out", [NT, D])
        with ExitStack() as st:
            gb = self.sb(st, "gb", [128, D], F32)
            bb = self.sb(st, "bb", [128, D], F32)
            pt_ = [self.sb(st, "pt", [128, NCORES, D], F32) for _ in range(1)]
            hf = [self.sb(st, "hf2", [128, D], F32) for _ in range(2)]
            acc = [self.sb(st, "acc", [128, D], F32) for _ in range(2)]
            ot = [self.sb(st, "ot", [128, D], F32) for _ in range(2)]
            lnt = (self.sb(st, "stats", [128, 4, 6], F32), self.sb(st, "mv", [128, 2], F32),
                   self.sb(st, "rstd", [128, 1], F32), self.sb(st, "xn", [128, D], F32))
            self.load(gb, gb[:, :], I["ln_g"], I["ln_g"].h.ap().partition_broadcast(128))
            self.load(bb, bb[:, :], I["ln_b"], I["ln_b"].h.ap().partition_broadcast(128))
            for i in range(NTI):
                p_, f, a_, o_ = pt_[0], hf[i % 2], acc[i % 2], ot[i % 2]
                for r in range(NCORES):
                    self.load(p_, p_[:, r, :], I["parts"], I["parts"].h.ap()[r, i * 128:(i + 1) * 128, :], dkey=r)
                self.load(f, f[:, :], I["h1"], I["h1"][i * 128:(i + 1) * 128, :])
                P.op("dve", lambda e, p_=p_, f=f, a_=a_: e.scalar_tensor_tensor(out=a_[:, :], in0=f[:, :], scalar=ALPHA, in1=p_[:, 0, :],
                                                                                op0=ALU.mult, op1=ALU.add),
                     reads=[(f, None), (p_, 0)], writes=[(a_, None)])
                for r in range(1, NCORES):
                    eng = "dve" if r % 2 == 0 else "pool"
                    P.op(eng, lambda e, p_=p_, a_=a_, r=r: e.tensor_tensor(out=a_[:, :], in0=a_[:, :], in1=p_[:, r, :], op=ALU.add),
                         reads=[(a_, None), (p_, r)], writes=[(a_, None)])
                self.layer_norm_tile(lnt, a_, None, gb, bb, o_, o_[:, :], None)
                self.P.dma("sp", "st", lambda e, o_=o_, i=i: e.dma_start(out=h2[i * 128:(i + 1) * 128, :], in_=o_[:, :]),
                           reads=[(o_, None)], writes=[(h2, i)])
            P.barrier()

    def debug_out(self, name, tile, shape, dt=F32):
        o = self.outp(name, shape, dt)
        self.dbg[name] = o
        idx = tuple(slice(None) for _ in shape)
        self.P.dma("sp", "st", lambda e: e.dma_start(out=o[idx], in_=tile[idx]), reads=[(tile, None)], writes=[(o, None)])

    def stage_xT(self, st_unused, src, ntiles, dstT, tok0, xs, pT):
        P, C = self.P, self.C
        for t in range(ntiles):
            x_ = xs[t % 2]
            self.load(x_, x_.h.ap().rearrange("p (a n) -> p a n", a=4), src,
                      src[t * 128:(t + 1) * 128, :].rearrange("p (a n) -> p a n", a=4), q="pool", cls="w")
            for cg in range(2):
                for k in range(8):
                    c = cg * 8 + k
                    P.op("pe", lambda e, k=k, c=c, x_=x_: e.transpose(out=pT[:, k, :], in_=x_[:, c * 128:(c + 1) * 128], identity=C["ident_b"][:, :]),
                         reads=[(x_, None), (C["ident_b"], None)], writes=[(pT, k)])
                o0 = tok0 + t * 128
                if cg == 0:
                    P.op("dve", lambda e, cg=cg, o0=o0: e.tensor_copy(out=dstT[:, 0:8, o0:o0 + 128], in_=pT[:, :, :]),
                         reads=[(pT, None)], writes=[(dstT, ("t", o0 // 128))], accum="stage")
                else:
                    P.op("act", lambda e, cg=cg, o0=o0: e.activation(out=dstT[:, 8:16, o0:o0 + 128], in_=pT[:, :, :], func=AF.Copy),
                         reads=[(pT, None)], writes=[(dstT, ("t", o0 // 128))], accum="stage")

    def mem_kv(self, outer, wmem_in, memb_in):
        P, C = self.P, self.C
        memK = self.sb(outer, "memK", [128, 4, 256], BF16)
        memV = self.sb(outer, "memV", [128, 2, 512], BF16)
        with ExitStack() as st:
            wm = self.sb(st, "wm", [128, 16, 1024], BF16)
            memT = self.sb(st, "memT", [128, 16, 256], BF16)
            xs = [self.sb(st, "xs", [128, D], BF16) for _ in range(2)]
            pT = self.ps(st, "pT", [128, 8, 128], BF16)
            pa = [self.ps(st, "pa", [128, 512], F32) for _ in range(2)]
            self.load(wm, wm[:, :, :], wmem_in, wmem_in.h.ap().rearrange("(c p) n -> p c n", p=128), q="pool", cls="w")
            self.stage_xT(st, memb_in, 2, memT, 0, xs, pT)
            n = 0
            for mh in range(4):
                a_ = pa[n % 2]; n += 1
                for c in range(16):
                    P.op("pe", lambda e, a_=a_, c=c, mh=mh: e.matmul(out=a_[:, 0:256], lhsT=wm[:, c, mh * 128:(mh + 1) * 128], rhs=memT[:, c, :],
                                                                      start=(c == 0), stop=(c == 15)),
                         reads=[(wm, None), (memT, None)], writes=[(a_, None)], inc=(c == 15))
                P.op("act", lambda e, a_=a_, mh=mh: e.activation(out=memK[:, mh, :], in_=a_[:, 0:256], func=AF.Copy),
                     reads=[(a_, None)], writes=[(memK, mh)])
            for mt in range(2):
                a_ = pa[n % 2]; n += 1
                for c in range(16):
                    P.op("pe", lambda e, a_=a_, c=c, mt=mt: e.matmul(out=a_[:, :], lhsT=memT[:, c, mt * 128:(mt + 1) * 128], rhs=wm[:, c, 512:1024],
                                                                      start=(c == 0), stop=(c == 15)),
                         reads=[(wm, None), (memT, None)], writes=[(a_, None)], inc=(c == 15))
                P.op("dve", lambda e, a_=a_, mt=mt: e.tensor_copy(out=memV[:, mt, :], in_=a_[:, :]), reads=[(a_, None)], writes=[(memV, mt)])
            P.barrier()
        return memK, memV

    def mem_attn(self, st, w_in, col0, srcT, tok0, memK, memV, catT, wbuf, qbuf, pa, pss, poo, ex, rd):
        P, C = self.P, self.C
        n = 0
        for mh in range(4):
            w_ = wbuf[mh % len(wbuf)]
            with self.nc.allow_non_contiguous_dma(reason="512B runs"):
                self.load(w_, w_[:, :, :], w_in, w_in.h.ap()[:, col0 + mh * 128:col0 + (mh + 1) * 128].rearrange("(c p) n -> p c n", p=128), q="pool", cls="w")
            for hf_ in range(2):
                a_ = pa[n % 2]; n += 1
                for c in range(16):
                    P.op("pe", lambda e, a_=a_, c=c, w_=w_, hf_=hf_: e.matmul(out=a_[:, :], lhsT=w_[:, c, :], rhs=srcT[:, c, tok0 + hf_ * 512:tok0 + (hf_ + 1) * 512],
                                                                             start=(c == 0), stop=(c == 15)),
                         reads=[(w_, None), (srcT, None)], writes=[(a_, None)], inc=(c == 15))
                P.op("act", lambda e, a_=a_, hf_=hf_: e.activation(out=qbuf[:, hf_ * 512:(hf_ + 1) * 512], in_=a_[:, :], func=AF.Copy),
                     reads=[(a_, None)], writes=[(qbuf, hf_)])
            for hf_ in range(2):
                for mt in range(2):
                    P.op("pe", lambda e, mt=mt, mh=mh, hf_=hf_: e.matmul(out=pss[mt][:, :], lhsT=memK[:, mh, mt * 128:(mt + 1) * 128],
                                                                        rhs=qbuf[:, hf_ * 512:(hf_ + 1) * 512], start=True, stop=True),
                         reads=[(memK, None), (qbuf, hf_)], writes=[(pss[mt], None)])
                    P.op("act", lambda e, mt=mt: e.activation(out=ex[:, mt, :], in_=pss[mt][:, :], func=AF.Exp, scale=SCALE),
                         reads=[(pss[mt], None)], writes=[(ex, mt)])
                for mt in range(2):
                    P.op("pe", lambda e, mt=mt, mh=mh: e.matmul(out=poo[0][:, :], lhsT=memV[:, mt, mh * 128:(mh + 1) * 128], rhs=ex[:, mt, :],
                                                               start=(mt == 0), stop=(mt == 1)),
                         reads=[(memV, None), (ex, mt)], writes=[(poo[0], None)], inc=(mt == 1))
                for mt in range(2):
                    P.op("pe", lambda e, mt=mt: e.matmul(out=poo[1][:, :], lhsT=C["ones_b"][:, :], rhs=ex[:, mt, :], start=(mt == 0), stop=(mt == 1)),
                         reads=[(C["ones_b"], None), (ex, mt)], writes=[(poo[1], None)], inc=(mt == 1))
                P.op("dve", lambda e: e.reciprocal(out=rd[:, 0:512], in_=poo[1][:, :]), reads=[(poo[1], None)], writes=[(rd, None)])
                P.op("dve", lambda e, mh=mh, hf_=hf_: e.tensor_tensor(out=catT[:, 12 + mh, hf_ * 512:(hf_ + 1) * 512], in0=poo[0][:, :], in1=rd[:, 0:512], op=ALU.mult),
                     reads=[(poo[0], None), (rd, None)], writes=[(catT, ("m", mh, hf_))])

    def mix_out(self, catT, wmix_in, xres_in, lng_in, lnb_in, out_t):
        P, C = self.P, self.C
        with ExitStack() as st:
            wm = self.sb(st, "wmx", [128, 16, D], BF16)
            gb = self.sb(st, "gb", [128, D], F32)
            bb = self.sb(st, "bb", [128, D], F32)
            xt = [self.sb(st, "xt", [128, D], F32) for _ in range(2)]
            rt = [self.sb(st, "rt", [128, D], F32) for _ in range(1)]
            ot = [self.sb(st, "ot", [128, D], F32) for _ in range(1)]
            lnt = (self.sb(st, "stats", [128, 4, 6], F32), self.sb(st, "mv", [128, 2], F32),
                   self.sb(st, "rstd", [128, 1], F32), self.sb(st, "xn", [128, D], F32))
            pm = [self.ps(st, "pm", [128, 512], F32) for _ in range(4)]
            for n in range(4):
                self.load(wm, wm[:, :, n * 512:(n + 1) * 512], wmix_in, wmix_in.h.ap()[:, n * 512:(n + 1) * 512].rearrange("(c p) n -> p c n", p=128),
                          q="pool", cls="w", dkey=n)
            self.load(gb, gb[:, :], lng_in, lng_in.h.ap().partition_broadcast(128))
            self.load(bb, bb[:, :], lnb_in, lnb_in.h.ap().partition_broadcast(128))
            for i in range(NTI):
                x_, r_, o_ = xt[i % 2], rt[0], ot[0]
                self.load(x_, x_[:, :], xres_in, xres_in[i * 128:(i + 1) * 128, :])
                for n in range(4):
                    for c in range(16):
                        P.op("pe", lambda e, n=n, c=c, i=i: e.matmul(out=pm[n][:, :], lhsT=catT[:, c, i * 128:(i + 1) * 128], rhs=wm[:, c, n * 512:(n + 1) * 512],
                                                                    start=(c == 0), stop=(c == 15)),
                             reads=[(catT, None), (wm, n)], writes=[(pm[n], None)], inc=(c == 15))
                    P.op("dve", lambda e, n=n, x_=x_, r_=r_: e.scalar_tensor_tensor(out=r_[:, n * 512:(n + 1) * 512], in0=x_[:, n * 512:(n + 1) * 512], scalar=ALPHA,
                                                                                    in1=pm[n][:, :], op0=ALU.mult, op1=ALU.add),
                         reads=[(x_, None), (pm[n], None)], writes=[(r_, n)])
                self.layer_norm_tile(lnt, r_, None, gb, bb, o_, o_[:, :], None)
                self.P.dma("sp", "st", lambda e, o_=o_, i=i: e.dma_start(out=out_t[i * 128:(i + 1) * 128, :], in_=o_[:, :]),
                           reads=[(o_, None)], writes=[(out_t, i)])
            P.barrier()

    def mixer_dil(self):
        nc, P, I, C = self.nc, self.P, self.I, self.C
        I["x_prev"] = self.inp("x_prev", [2048, D])
        I["memb"] = self.inp("memb", [256, D])
        I["w_in"] = self.inp("w_in", [D, 3 * W + MW])
        I["w_mem"] = self.inp("w_mem", [D, 2 * MW])
        I["w_mix"] = self.inp("w_mix", [D, D])
        I["ln1_g"] = self.inp("ln1_g", [1, D])
        I["ln1_b"] = self.inp("ln1_b", [1, D])
        I["attn_c"] = self.inp("attn_c", [128, 4, 128])
        w_in = I["w_in"]
        slopes = _alibi_slopes(12)
        with ExitStack() as outer:
            catT = self.sb(outer, "catT", [128, 16, NT], BF16)
            memK, memV = self.mem_kv(outer, I["w_mem"], I["memb"])
            with ExitStack() as st:
                xT = self.sb(st, "xT", [128, 16, 3072], BF16)
                xs = [self.sb(st, "xs", [128, D], BF16) for _ in range(2)]
                wq = [self.sb(st, "wq", [128, 16, 128], BF16) for _ in range(1)]
                wk = [self.sb(st, "wk", [128, 16, 128], BF16) for _ in range(1)]
                wv = [self.sb(st, "wv", [128, 16, 128], BF16) for _ in range(1)]
                nat = self.sb(st, "nat", [128, 3072], BF16)
                qTd = self.sb(st, "qTd", [128, NT], BF16)
                kTd = self.sb(st, "kTd", [128, 3072], BF16)
                vTd = self.sb(st, "vTd", [128, 3072], BF16)
                Vt = self.sb(st, "Vt", [128, 32, 128], BF16)
                Ob = self.sb(st, "Ob", [128, 3, NT], BF16)
                den = self.sb(st, "den", [128, NT], F32)
                rd = self.sb(st, "rd", [128, NT], F32)
                dtab = self.sb(st, "dtab", [128, 4, 128], F32)
                zz = [self.sb(st, "zz", [128, 2, 128], F32) for _ in range(2)]
                pp = [self.sb(st, "pp", [128, 2, 128], BF16) for _ in range(2)]
                exm = self.sb(st, "exm", [128, 2, 512], BF16)
                pT = self.ps(st, "pT", [128, 8, 128], BF16)
                pa = [self.ps(st, "pa", [128, 512], F32) for _ in range(2)]
                pss = [self.ps(st, "pss", [128, 512], F32) for _ in range(2)]
                poo = [self.ps(st, "poo", [128, 512], F32) for _ in range(2)]
                self.load(dtab, dtab[:, :, :], I["attn_c"], I["attn_c"][:, :, :])
                self.stage_xT(st, I["x_prev"], 16, xT, 0, xs, pT)
                self.stage_xT(st, I["x_own"], 8, xT, 2048, xs, pT)
                npa = 0
                nq = 0
                nh = 0
                stop = self.cfg.get("stop", 99)
                for j in range(4 if stop > 0 else 0):
                    for g in range(self.cfg.get("gmax", 3)):
                        hh = 4 * g + j
                        d = DIL[g]
                        Tp = TPREV[g]
                        Q = 128 if g < 2 else 64
                        ch = slopes[hh] * d
                        wq_, wk_, wv_ = wq[0], wk[0], wv[0]
                        nh += 1
                        with nc.allow_non_contiguous_dma(reason="512B runs"):
                            for w_, c0 in ((wq_, hh * 128), (wk_, W + hh * 128), (wv_, 2 * W + hh * 128)):
                                self.load(w_, w_[:, :, :], w_in, w_in.h.ap()[:, c0:c0 + 128].rearrange("(c p) n -> p c n", p=128), q="pool", cls="w")
                        Lq = NT // d
                        Lk = 128 + Lq
                        tot = Tp + NT
                        def proj(w_, c0tok, ntok, dstd, L_, eng):
                            nonlocal npa
                            s0 = 0
                            while s0 < ntok:
                                nb = min(512, ntok - s0)
                                a_ = pa[npa % 2]; npa += 1
                                for c in range(16):
                                    P.op("pe", lambda e, a_=a_, c=c, w_=w_, s0=s0, nb=nb: e.matmul(
                                        out=a_[:, 0:nb], lhsT=w_[:, c, :], rhs=xT[:, c, c0tok + s0:c0tok + s0 + nb], start=(c == 0), stop=(c == 15)),
                                        reads=[(w_, None), (xT, None)], writes=[(a_, None)], inc=(c == 15))
                                P.op("act", lambda e, a_=a_, s0=s0, nb=nb: e.activation(out=nat[:, s0:s0 + nb], in_=a_[:, 0:nb], func=AF.Copy),
                                     reads=[(a_, None)], writes=[(nat, s0 // 512)])
                                s0 += nb
                            src = nat.h.ap()[:, 0:ntok].rearrange("p (l r) -> p r l", r=d)
                            dst = dstd.h.ap()[:, 0:ntok].rearrange("p (r l) -> p r l", r=d)
                            P.op(eng, lambda e, src=src, dst=dst: e.tensor_copy(out=dst, in_=src), reads=[(nat, None)], writes=[(dstd, None)])
                        proj(wq_, 2048, NT, qTd, Lq, "dve")
                        proj(wk_, 2048 - Tp, tot, kTd, Lk, "pool")
                        proj(wv_, 2048 - Tp, tot, vTd, Lk, "dve")
                        nbk = (Lk + 127) // 128
                        blocks = [(r, b, r * Lk + b * 128, min(128, Lk - b * 128)) for r in range(d) for b in range(nbk)]
                        for b0 in range(0, len(blocks), 8):
                            grp = blocks[b0:b0 + 8]
                            for gi, (r, b, p0, cnt) in enumerate(grp):
                                P.op("pe", lambda e, gi=gi, p0=p0, cnt=cnt: e.transpose(out=pT[0:cnt, gi, :], in_=vTd[:, p0:p0 + cnt], identity=C["ident_b"][:, :]),
                                     reads=[(vTd, None), (C["ident_b"], None)], writes=[(pT, gi)])
                            ng = len(grp)
                            P.op("act", lambda e, b0=b0, ng=ng: e.activation(out=Vt[:, b0:b0 + ng, :], in_=pT[:, 0:ng, :], func=AF.Copy),
                                 reads=[(pT, None)], writes=[(Vt, ("v", b0))], accum="v%d" % hh)
                        nto = Lq // Q
                        for r in range(d if stop > 1 else 0):
                            for n in range(nto):
                                z_, p_ = zz[nq % 2], pp[nq % 2]
                                s_, o_ = pss[nq % 2], poo[nq % 2]
                                nq += 1
                                q0 = r + d * n * Q
                                qd0 = r * Lq + n * Q
                                kp0 = r * Lk + n * Q
                                kc0 = r * Lk + 128 + n * Q
                                vprev = r * nbk + (n * Q) // 128
                                vcur = r * nbk + (128 + n * Q) // 128
                                tbl = (2 if g < 2 else 3) if n == 0 else 0
                                P.op("pe", lambda e, s_=s_, kp0=kp0, qd0=qd0, Q=Q: e.matmul(
                                    out=s_[:, 0:Q], lhsT=kTd[:, kp0:kp0 + 128], rhs=qTd[:, qd0:qd0 + Q], start=True, stop=True),
                                    reads=[(kTd, None), (qTd, None)], writes=[(s_, "p")])
                                P.op("pe", lambda e, s_=s_, kc0=kc0, qd0=qd0, Q=Q: e.matmul(
                                    out=s_[0:Q, 128:128 + Q], lhsT=kTd[:, kc0:kc0 + Q], rhs=qTd[:, qd0:qd0 + Q], start=True, stop=True),
                                    reads=[(kTd, None), (qTd, None)], writes=[(s_, "c")])
                                P.op("dve", lambda e, s_=s_, z_=z_, tbl=tbl, Q=Q, ch=ch: e.scalar_tensor_tensor(
                                    out=z_[:, 0, 0:Q], in0=dtab[:, tbl, 0:Q], scalar=-ch / SCALE, in1=s_[:, 0:Q], op0=ALU.mult, op1=ALU.add),
                                    reads=[(dtab, None), (s_, "p")], writes=[(z_, "p")])
                                P.op("dve", lambda e, s_=s_, z_=z_, Q=Q, ch=ch: e.scalar_tensor_tensor(
                                    out=z_[0:Q, 1, 0:Q], in0=dtab[0:Q, 1, 0:Q], scalar=-ch / SCALE, in1=s_[0:Q, 128:128 + Q], op0=ALU.mult, op1=ALU.add),
                                    reads=[(dtab, None), (s_, "c")], writes=[(z_, "c")])
                                P.op("act", lambda e, z_=z_, p_=p_, Q=Q: e.activation(out=p_[:, 0, 0:Q], in_=z_[:, 0, 0:Q], func=AF.Exp, scale=SCALE),
                                     reads=[(z_, "p")], writes=[(p_, "p")])
                                P.op("act", lambda e, z_=z_, p_=p_, Q=Q: e.activation(out=p_[0:Q, 1, 0:Q], in_=z_[0:Q, 1, 0:Q], func=AF.Exp, scale=SCALE),
                                     reads=[(z_, "c")], writes=[(p_, "c")])
                                P.op("pe", lambda e, o_=o_, p_=p_, vprev=vprev, Q=Q: e.matmul(
                                    out=o_[:, 0:Q], lhsT=Vt[:, vprev, :], rhs=p_[:, 0, 0:Q], start=True, stop=False),
                                    reads=[(Vt, None), (p_, "p")], writes=[(o_, "o")], inc=False)
                                P.op("pe", lambda e, o_=o_, p_=p_, vcur=vcur, Q=Q: e.matmul(
                                    out=o_[:, 0:Q], lhsT=Vt[0:Q, vcur, :], rhs=p_[0:Q, 1, 0:Q], start=False, stop=True),
                                    reads=[(Vt, None), (p_, "c")], writes=[(o_, "o")])
                                P.op("pe", lambda e, o_=o_, p_=p_, Q=Q: e.matmul(
                                    out=o_[:, 128:128 + Q], lhsT=C["ones_b"][:, :], rhs=p_[:, 0, 0:Q], start=True, stop=False),
                                    reads=[(C["ones_b"], None), (p_, "p")], writes=[(o_, "s")], inc=False)
                                P.op("pe", lambda e, o_=o_, p_=p_, Q=Q: e.matmul(
                                    out=o_[:, 128:128 + Q], lhsT=C["ones_b"][0:Q, :], rhs=p_[0:Q, 1, 0:Q], start=False, stop=True),
                                    reads=[(C["ones_b"], None), (p_, "c")], writes=[(o_, "s")])
                                P.op("act", lambda e, o_=o_, g=g, q0=q0, d=d, Q=Q: e.activation(out=Ob[:, g, sl(q0, Q, d)], in_=o_[:, 0:Q], func=AF.Copy),
                                     reads=[(o_, "o")], writes=[(Ob, g)], accum="ob%d" % hh)
                                if g == 0:
                                    P.op("dve", lambda e, o_=o_, q0=q0, d=d, Q=Q: e.tensor_copy(out=den[:, sl(q0, Q, d)], in_=o_[:, 128:128 + Q]),
                                         reads=[(o_, "s")], writes=[(den, None)], accum="den%d" % hh)
                                else:
                                    P.op("dve", lambda e, o_=o_, q0=q0, d=d, Q=Q: e.tensor_tensor(out=den[:, sl(q0, Q, d)], in0=den[:, sl(q0, Q, d)],
                                                                                                in1=o_[:, 128:128 + Q], op=ALU.add),
                                         reads=[(o_, "s"), (den, None)], writes=[(den, None)], accum="den%d" % hh)
                    if stop > 1:
                        P.op("dve", lambda e: e.reciprocal(out=rd[:, :], in_=den[:, :]), reads=[(den, None)], writes=[(rd, None)])
                    for g in range(3 if stop > 1 else 0):
                        eng = "dve" if g != 1 else "pool"
                        P.op(eng, lambda e, g=g, j=j: e.tensor_tensor(out=catT[:, 4 * g + j, :], in0=Ob[:, g, :], in1=rd[:, :], op=ALU.mult),
                             reads=[(Ob, g), (rd, None)], writes=[(catT, ("s", 4 * g + j))])
                if stop > 2:
                    self.mem_attn(st, w_in, 3 * W, xT, 2048, memK, memV, catT, wq, qTd, pa, pss, poo, exm, rd)
                if "cat" in self.cfg.get("debug", ()):
                    self.debug_out("dbg_cat", catT, [128, 16, NT], BF16)
                P.barrier()
            self.mix_out(catT, I["w_mix"], I["x_own"], I["ln1_g"], I["ln1_b"], self.h1o)


    def mixer_ret(self):
        nc, P, I, C = self.nc, self.P, self.I, self.C
        I["h_prev"] = self.inp("h_prev", [3 * NT, D])
        I["memb"] = self.inp("memb", [256, D])
        I["w_in"] = self.inp("w_in", [D, 4 * W + MW])
        I["w_mem"] = self.inp("w_mem", [D, 2 * MW])
        I["w_mix"] = self.inp("w_mix", [D, D])
        I["ln1_g"] = self.inp("ln1_g", [1, D])
        I["ln1_b"] = self.inp("ln1_b", [1, D])
        I["gn_g"] = self.inp("gn_g", [128, 12])
        I["decT"] = self.inp("decT", [128, 12, 128])
        I["qdtab"] = self.inp("qdtab", [128, 12, 128])
        I["kdtab"] = self.inp("kdtab", [128, 12 * 128])
        w_in = I["w_in"]
        gam = [1.0 - 2.0 ** (-(5.0 + h)) for h in range(12)]
        cdec = [g_ ** 128 for g_ in gam]
        NCH = 32
        with ExitStack() as outer:
            memK, memV = self.mem_kv(outer, I["w_mem"], I["memb"])
            Vown = self.sb(outer, "Vown", [128, 8, W], BF16)
            Sb = self.sb(outer, "Sb", [128, 8, 12, 128], BF16)
            with ExitStack() as st:
                wk = self.sb(st, "wk_all", [128, 16, W], BF16)
                wv = self.sb(st, "wv_all", [128, 16, W], BF16)
                hT = self.sb(st, "hTb", [128, 16, 512], BF16)
                xs = [self.sb(st, "xs", [128, D], BF16) for _ in range(2)]
                kdt = self.sb(st, "kdt", [128, W], F32)
                Kd = [self.sb(st, "Kd", [128, W], BF16) for _ in range(2)]
                Vb = [self.sb(st, "Vb", [128, W], BF16) for _ in range(2)]
                S = self.sb(st, "S", [128, 12, 128], F32)
                pT = self.ps(st, "pT", [128, 8, 128], BF16)
                pj = [self.ps(st, "pj", [128, 512], F32) for _ in range(4)]
                pkv = [self.ps(st, "pkv", [128, 4, 128], F32) for _ in range(2)]
                for n in range(3):
                    with nc.allow_non_contiguous_dma(reason="2KB runs"):
                        self.load(wk, wk[:, :, n * 512:(n + 1) * 512], w_in, w_in.h.ap()[:, W + n * 512:W + (n + 1) * 512].rearrange("(c p) n -> p c n", p=128),
                                  q="pool", cls="w", dkey=n)
                        self.load(wv, wv[:, :, n * 512:(n + 1) * 512], w_in, w_in.h.ap()[:, 2 * W + n * 512:2 * W + (n + 1) * 512].rearrange("(c p) n -> p c n", p=128),
                                  q="pool", cls="w", dkey=n)
                self.load(kdt, kdt[:, :], I["kdtab"], I["kdtab"][:, :])
                P.op("dve", lambda e: e.memset(S[:, :, :], 0.0), writes=[(S, None)])
                npj = 0
                nkv = 0
                for blk in range(8):
                    if blk < 6:
                        src = Tile(I["h_prev"].h.ap()[blk * 512:(blk + 1) * 512, :], "hp")
                        src.deps = I["h_prev"].deps
                    else:
                        src = Tile(I["x_own"].h.ap()[(blk - 6) * 512:(blk - 5) * 512, :], "ho")
                        src.deps = I["x_own"].deps
                    self.stage_xT(st, src, 4, hT, 0, xs, pT)
                    for ci in range(4):
                        n = blk * 4 + ci
                        kd_, vb_ = Kd[n % 2], Vb[n % 2]
                        for which, w_, dst in (("k", wk, kd_), ("v", wv, vb_)):
                            for gq in range(3):
                                a_ = pj[npj % 4]; npj += 1
                                for c in range(16):
                                    P.op("pe", lambda e, a_=a_, c=c, w_=w_, gq=gq, ci=ci: e.matmul(
                                        out=a_[:, :], lhsT=hT[:, c, ci * 128:(ci + 1) * 128], rhs=w_[:, c, gq * 512:(gq + 1) * 512], start=(c == 0), stop=(c == 15)),
                                        reads=[(hT, None), (w_, gq)], writes=[(a_, None)], inc=(c == 15))
                                if which == "k":
                                    P.op("dve", lambda e, a_=a_, dst=dst, gq=gq: e.tensor_tensor(out=dst[:, gq * 512:(gq + 1) * 512], in0=a_[:, :],
                                                                                                 in1=kdt[:, gq * 512:(gq + 1) * 512], op=ALU.mult),
                                         reads=[(a_, None), (kdt, None)], writes=[(dst, gq)])
                                else:
                                    P.op("act", lambda e, a_=a_, dst=dst, gq=gq: e.activation(out=dst[:, gq * 512:(gq + 1) * 512], in_=a_[:, :], func=AF.Copy),
                                         reads=[(a_, None)], writes=[(dst, gq)])
                                    if n >= 24:
                                        P.op("pool", lambda e, dst=dst, gq=gq, n=n: e.tensor_copy(out=Vown[:, n - 24, gq * 512:(gq + 1) * 512], in_=dst[:, gq * 512:(gq + 1) * 512]),
                                             reads=[(dst, gq)], writes=[(Vown, (n - 24, gq))])
                        if n >= 24:
                            P.op("act", lambda e, n=n: e.activation(out=Sb[:, n - 24, :, :], in_=S[:, :, :], func=AF.Copy),
                                 reads=[(S, None)], writes=[(Sb, n - 24)])
                        if n == NCH - 1:
                            break
                        for hg in range(3):
                            kv_ = pkv[nkv % 2]; nkv += 1
                            for hh in range(4):
                                h = hg * 4 + hh
                                P.op("pe", lambda e, kv_=kv_, hh=hh, h=h, kd_=kd_, vb_=vb_: e.matmul(
                                    out=kv_[:, hh, :], lhsT=kd_[:, h * 128:(h + 1) * 128], rhs=vb_[:, h * 128:(h + 1) * 128], start=True, stop=True),
                                    reads=[(kd_, None), (vb_, None)], writes=[(kv_, None)])
                            for hh in range(4):
                                h = hg * 4 + hh
                                P.op("dve", lambda e, kv_=kv_, hh=hh, h=h: e.scalar_tensor_tensor(
                                    out=S[:, h, :], in0=S[:, h, :], scalar=cdec[h], in1=kv_[:, hh, :], op0=ALU.mult, op1=ALU.add),
                                    reads=[(kv_, None), (S, h)], writes=[(S, h)])
                P.barrier()
            catT = self.sb(outer, "catT", [128, 16, NT], BF16)
            with ExitStack() as st:
                hT = self.sb(st, "hTo", [128, 16, NT], BF16)
                xs = [self.sb(st, "xs", [128, D], BF16) for _ in range(2)]
                wq = [self.sb(st, "wq", [128, 16, 128], BF16) for _ in range(1)]
                wkf = self.sb(st, "wkf", [128, 16, 128], BF16)
                wg = self.sb(st, "wg", [128, 16, 128], BF16)
                qT = self.sb(st, "qT", [128, NT], BF16)
                kT = self.sb(st, "kT", [128, NT], BF16)
                gT = self.sb(st, "gT", [128, NT], F32)
                sgm = self.sb(st, "sgm", [128, NT], F32)
                qTd = self.sb(st, "qTd", [128, NT], BF16)
                decT = self.sb(st, "decT", [128, 12, 128], F32)
                qdt = self.sb(st, "qdt", [128, 12, 128], F32)
                gng = self.sb(st, "gng", [128, 12], F32)
                onesd = self.sb(st, "onesd", [128, 128], F32)
                r32 = self.sb(st, "r32", [128, NT], F32)
                r2 = self.sb(st, "r2", [128, NT], F32)
                t1 = self.sb(st, "t1", [128, NT], F32)
                t2 = self.sb(st, "t2", [128, NT], F32)
                pp = [self.sb(st, "pp", [128, 128], BF16) for _ in range(2)]
                exm = self.sb(st, "exm", [128, 2, 512], BF16)
                rd = self.sb(st, "rd", [128, NT], F32)
                pT = self.ps(st, "pT", [128, 8, 128], BF16)
                pa = [self.ps(st, "pa", [128, 512], F32) for _ in range(2)]
                pss = [self.ps(st, "pss", [128, 512], F32) for _ in range(2)]
                poo = [self.ps(st, "poo", [128, 512], F32) for _ in range(2)]
                self.load(decT, decT[:, :, :], I["decT"], I["decT"][:, :, :])
                self.load(qdt, qdt[:, :, :], I["qdtab"], I["qdtab"][:, :, :])
                self.load(gng, gng[:, :], I["gn_g"], I["gn_g"][:, :])
                P.op("dve", lambda e: e.memset(onesd[:, :], 1.0 / 128.0), writes=[(onesd, None)])
                self.stage_xT(st, I["x_own"], 8, hT, 0, xs, pT)
                npa = 0
                nq = 0
                for h in range(12):
                    with nc.allow_non_contiguous_dma(reason="512B runs"):
                        for w_, c0 in ((wq[0], h * 128), (wkf, W + h * 128), (wg, 3 * W + h * 128)):
                            self.load(w_, w_[:, :, :], w_in, w_in.h.ap()[:, c0:c0 + 128].rearrange("(c p) n -> p c n", p=128), q="pool", cls="w")
                    for w_, dst, eng in ((wq[0], qT, "act"), (wkf, kT, "dve"), (wg, gT, "act")):
                        for hf_ in range(2):
                            a_ = pa[npa % 2]; npa += 1
                            for c in range(16):
                                P.op("pe", lambda e, a_=a_, c=c, w_=w_, hf_=hf_: e.matmul(out=a_[:, :], lhsT=w_[:, c, :], rhs=hT[:, c, hf_ * 512:(hf_ + 1) * 512],
                                                                                         start=(c == 0), stop=(c == 15)),
                                     reads=[(w_, None), (hT, None)], writes=[(a_, None)], inc=(c == 15))
                            if eng == "act":
                                P.op("act", lambda e, a_=a_, dst=dst, hf_=hf_: e.activation(out=dst[:, hf_ * 512:(hf_ + 1) * 512], in_=a_[:, :], func=AF.Copy),
                                     reads=[(a_, None)], writes=[(dst, hf_)])
                            else:
                                P.op("dve", lambda e, a_=a_, dst=dst, hf_=hf_: e.tensor_copy(out=dst[:, hf_ * 512:(hf_ + 1) * 512], in_=a_[:, :]),
                                     reads=[(a_, None)], writes=[(dst, hf_)])
                    P.op("dve", lambda e, h=h: e.tensor_tensor(out=qTd.h.ap().rearrange("p (n q) -> p n q", q=128), in0=qT.h.ap().rearrange("p (n q) -> p n q", q=128),
                                                                in1=qdt[:, h:h + 1, :].to_broadcast([128, 8, 128]), op=ALU.mult),
                         reads=[(qT, None), (qdt, None)], writes=[(qTd, None)])
                    P.op("act", lambda e: e.activation(out=sgm[:, :], in_=gT[:, :], func=AF.Sigmoid), reads=[(gT, None)], writes=[(sgm, None)])
                    P.op("pool", lambda e: e.tensor_tensor(out=sgm[:, :], in0=sgm[:, :], in1=gT[:, :], op=ALU.mult), reads=[(sgm, None), (gT, None)], writes=[(sgm, None)])
                    for n in range(8):
                        s_, o_, p_ = pss[nq % 2], poo[nq % 2], pp[nq % 2]
                        nq += 1
                        P.op("pe", lambda e, s_=s_, n=n: e.matmul(out=s_[:, 0:128], lhsT=kT[:, n * 128:(n + 1) * 128], rhs=qT[:, n * 128:(n + 1) * 128], start=True, stop=True),
                             reads=[(kT, None), (qT, None)], writes=[(s_, None)])
                        P.op("dve", lambda e, s_=s_, p_=p_, h=h: e.tensor_tensor(out=p_[:, :], in0=s_[:, 0:128], in1=decT[:, h, :], op=ALU.mult),
                             reads=[(s_, None), (decT, None)], writes=[(p_, None)])
                        P.op("pe", lambda e, o_=o_, p_=p_, n=n, h=h: e.matmul(out=o_[:, 0:128], lhsT=Vown[:, n, h * 128:(h + 1) * 128], rhs=p_[:, :], start=True, stop=False),
                             reads=[(Vown, None), (p_, None)], writes=[(o_, None)], inc=False)
                        P.op("pe", lambda e, o_=o_, n=n, h=h: e.matmul(out=o_[:, 0:128], lhsT=Sb[:, n, h, :], rhs=qTd[:, n * 128:(n + 1) * 128], start=False, stop=True),
                             reads=[(Sb, None), (qTd, None)], writes=[(o_, None)])
                        P.op("act", lambda e, o_=o_, n=n: e.activation(out=r32[:, n * 128:(n + 1) * 128], in_=o_[:, 0:128], func=AF.Copy),
                             reads=[(o_, None)], writes=[(r32, n)])
                        P.op("act", lambda e, o_=o_, n=n: e.activation(out=r2[:, n * 128:(n + 1) * 128], in_=o_[:, 0:128], func=AF.Square),
                             reads=[(o_, None)], writes=[(r2, n)])
                    for hf_ in range(2):
                        cs = slice(hf_ * 512, (hf_ + 1) * 512)
                        m_, e_ = pa[0], pa[1]
                        P.op("pe", lambda e, m_=m_, cs=cs: e.matmul(out=m_[:, :], lhsT=onesd[:, :], rhs=r32[:, cs], start=True, stop=True),
                             reads=[(onesd, None), (r32, None)], writes=[(m_, None)])
                        P.op("pe", lambda e, e_=e_, cs=cs: e.matmul(out=e_[:, :], lhsT=onesd[:, :], rhs=r2[:, cs], start=True, stop=True),
                             reads=[(onesd, None), (r2, None)], writes=[(e_, None)])
                        P.op("act", lambda e, m_=m_, cs=cs: e.activation(out=t1[:, cs], in_=m_[:, :], func=AF.Square),
                             reads=[(m_, None)], writes=[(t1, hf_)])
                        P.op("dve", lambda e, e_=e_, cs=cs: e.tensor_tensor(out=t1[:, cs], in0=e_[:, :], in1=t1[:, cs], op=ALU.subtract),
                             reads=[(e_, None), (t1, hf_)], writes=[(t1, hf_)])
                        P.op("dve", lambda e, cs=cs: e.tensor_scalar(out=t1[:, cs], in0=t1[:, cs], scalar1=LN_EPS, scalar2=None, op0=ALU.add),
                             reads=[(t1, hf_)], writes=[(t1, hf_)])
                        P.op("act", lambda e, cs=cs: e.activation(out=t1[:, cs], in_=t1[:, cs], func=AF.Sqrt), reads=[(t1, hf_)], writes=[(t1, hf_)])
                        P.op("dve", lambda e, cs=cs: e.reciprocal(out=t1[:, cs], in_=t1[:, cs]), reads=[(t1, hf_)], writes=[(t1, hf_)])
                        P.op("dve", lambda e, m_=m_, cs=cs: e.tensor_tensor(out=t2[:, cs], in0=r32[:, cs], in1=m_[:, :], op=ALU.subtract),
                             reads=[(m_, None), (r32, None)], writes=[(t2, hf_)])
                        P.op("pool", lambda e, cs=cs: e.tensor_tensor(out=t2[:, cs], in0=t2[:, cs], in1=t1[:, cs], op=ALU.mult),
                             reads=[(t1, hf_), (t2, hf_)], writes=[(t2, hf_)])
                        P.op("dve", lambda e, cs=cs, h=h: e.scalar_tensor_tensor(out=catT[:, h, cs], in0=t2[:, cs], scalar=gng[:, h:h + 1], in1=sgm[:, cs],
                                                                                 op0=ALU.mult, op1=ALU.mult),
                             reads=[(t2, hf_), (gng, None), (sgm, None)], writes=[(catT, ("s", h, hf_))])
                self.mem_attn(st, w_in, 4 * W, hT, 0, memK, memV, catT, wq, qT, pa, pss, poo, exm, rd)
                if "cat" in self.cfg.get("debug", ()):
                    self.debug_out("dbg_cat", catT, [128, 16, NT], BF16)
                P.barrier()
            self.mix_out(catT, I["w_mix"], I["x_own"], I["ln1_g"], I["ln1_b"], self.h1o)


def host_consts(c, nel):
    out = {}
    out["ident"] = np.eye(128, dtype=np.float32)
    out["tri"] = (np.arange(128)[:, None] < np.arange(128)[None, :]).astype(np.float32)
    p = np.arange(128)
    ps = np.zeros((128, nel, NE), np.float32)
    for j in range(nel):
        ps[:, j, c * nel + j] = 1.0
    out["psel"] = ps
    pi = np.zeros((128, NSL, 2), np.float32)
    pi[:, :, 0] = NTOK + p[:, None]
    out["padinit"] = pi
    out["tokidf"] = (np.arange(64)[None, :] * 128 + p[:, None]).astype(np.float32)
    return out


def host_attn_consts(c):
    q = c % 4
    BIGD = 1.0e6
    k = np.arange(128)[:, None].astype(np.float32)
    qq = np.arange(128)[None, :].astype(np.float32)
    t0 = np.where(k >= qq, 128.0 + qq - k, BIGD).astype(np.float32)
    t1 = np.where(k <= qq, qq - k, BIGD).astype(np.float32)
    t2 = t0.copy() if q > 0 else np.full_like(t0, BIGD)
    if q == 0:
        t3 = np.full_like(t0, BIGD)
    elif q == 1:
        t3 = t0.copy()
        t3[:64, :] = BIGD
    else:
        t3 = t0.copy()
    return np.ascontiguousarray(np.stack([t0, t1, t2, t3], 1))


def host_ret_consts(c):
    gam = np.array([1.0 - 2.0 ** (-(5.0 + h)) for h in range(12)], np.float64)
    k = np.arange(128)[:, None, None].astype(np.float64)
    q = np.arange(128)[None, None, :].astype(np.float64)
    g3 = gam[None, :, None]
    decT = np.where(q >= k, SCALE * g3 ** np.maximum(q - k, 0.0), 0.0).astype(np.float32)
    qd = np.broadcast_to((g3 ** (q + 1.0)), (128, 12, 128)).astype(np.float32)
    kd = (SCALE * gam[None, :] ** (127.0 - np.arange(128)[:, None])).astype(np.float32)
    kdtab = np.ascontiguousarray(np.repeat(kd[:, :, None], 128, axis=2).reshape(128, 12 * 128))
    return {"decT": np.ascontiguousarray(decT), "qdtab": np.ascontiguousarray(qd), "kdtab": kdtab}


def host_moe_weights(inputs, L, c, nel):
    es = [c * nel + j for j in range(nel)]
    wi = np.asarray(inputs["moe_w_in"][L])[es]
    wi = wi.reshape(nel, 16, 128, 2, 8, 256)
    wi = np.ascontiguousarray(wi.transpose(0, 4, 2, 1, 3, 5)).reshape(nel, 8, 128, 16, 512)
    wo = np.asarray(inputs["moe_w_out"][L])[es]
    wo = wo.reshape(nel, 16, 128, 4, 512)
    wo = np.ascontiguousarray(wo.transpose(0, 3, 2, 1, 4))
    bi = np.asarray(inputs["moe_b_in"][L])[es].reshape(nel, 32, 128)
    bi = np.ascontiguousarray(bi.transpose(2, 0, 1))
    bo = np.ascontiguousarray(np.asarray(inputs["moe_b_out"][L])[es]).reshape(1, nel, D)
    return {"moe_win": wi, "moe_wout": wo, "moe_bin": bi, "moe_bout": bo}


def host_A_inputs(inputs, L, c, h_own, h_all_batch):
    b, q = c // 4, c % 4
    m = {"x_own": h_own}
    m["router_w"] = np.asarray(inputs["router_w"][L])
    m["router_b"] = np.asarray(inputs["router_b"][L]).reshape(1, NE)
    m["memb"] = np.asarray(inputs["mem"][b])
    m["w_mem"] = np.asarray(inputs["w_mem_kv"][L])
    m["w_mix"] = np.asarray(inputs["w_mix_out"][L])
    m["ln1_g"] = np.asarray(inputs["ln_mix_g"][L]).reshape(1, D)
    m["ln1_b"] = np.asarray(inputs["ln_mix_b"][L]).reshape(1, D)
    if L == 0:
        xb = np.asarray(inputs["x"][b])
        xp = np.zeros((2048, D), np.float32)
        n = min(q * NT, 2048)
        if n:
            xp[2048 - n:] = xb[q * NT - n:q * NT]
        m["x_prev"] = xp
        m["w_in"] = np.asarray(inputs["w_in_dil"][0])
        m["attn_c"] = host_attn_consts(c)
    else:
        hp = np.zeros((3 * NT, D), np.float32)
        if q:
            hp[(3 - q) * NT:] = h_all_batch[:q * NT]
        m["h_prev"] = hp
        m["w_in"] = np.asarray(inputs["w_in_ret"][0])
        m["gn_g"] = np.ascontiguousarray(np.asarray(inputs["ret_gn_g"][0]).reshape(12, 128).T)
        m.update(host_ret_consts(c))
    return m


TRACE = {"on": False, "last": None}


def launch(cfg, per_core):
    bld = Builder(cfg)
    nc = bld.build()
    in_maps = []
    for c in range(NCORES):
        m = per_core(c)
        hc = host_consts(c, cfg.get("nel", NEL))
        for name in bld.ins:
            if name not in m and name in hc:
                m[name] = hc[name]
        mm = {}
        for name, (shape, dt) in bld.ins.items():
            if name not in m:
                raise KeyError(name)
            a = np.ascontiguousarray(m[name])
            assert tuple(a.shape) == tuple(shape), (name, a.shape, shape)
            mm[name] = a
        in_maps.append(mm)
    if TRACE["on"]:
        res = run_bass_kernel_spmd(nc, in_maps, core_ids=list(range(NCORES)), trace=True)
        TRACE["last"] = res
    else:
        res = run_bass_kernel_spmd(nc, in_maps, core_ids=list(range(NCORES)))
    return res.results


def moe_layer(inputs, L, h1_own, h1b_own, g_own, nel=NEL):
    import ml_dtypes
    h1all = np.concatenate(list(h1b_own) + [np.zeros((128, D), ml_dtypes.bfloat16)], 0)
    gall = np.concatenate(list(g_own), 0)
    resB = launch({"kind": "B", "L": L, "nel": nel},
                  lambda c: dict(h1all=h1all, gall=gall, **host_moe_weights(inputs, L, c, nel)))
    yp = [resB[c]["ypart"].reshape(NROW, D) for c in range(NCORES)]
    lg = np.asarray(inputs["ln_ffn_g"][L]).reshape(1, D)
    lb = np.asarray(inputs["ln_ffn_b"][L]).reshape(1, D)
    resC = launch({"kind": "C", "L": L},
                  lambda c: dict(parts=np.stack([yp[r][c * NT:(c + 1) * NT] for r in range(NCORES)], 0), h1=h1_own[c], ln_g=lg, ln_b=lb))
    return [resC[c]["h2_out"] for c in range(NCORES)]


def kernel(**inputs):
    x = np.asarray(inputs["x"])
    h_own = [np.ascontiguousarray(x[c // 4, (c % 4) * NT:(c % 4 + 1) * NT]) for c in range(NCORES)]
    h_batch = [x[0], x[1]]
    for L in (0, 1):
        resA = launch({"kind": "A", "L": L, "mixer": True},
                      lambda c: host_A_inputs(inputs, L, c, h_own[c], h_batch[c // 4]))
        h_own = moe_layer(inputs, L, [resA[c]["h1_out"] for c in range(NCORES)], [resA[c]["h1b_out"] for c in range(NCORES)],
                          [resA[c]["g_out"] for c in range(NCORES)])
        h_batch = [np.concatenate(h_own[0:4], 0), np.concatenate(h_own[4:8], 0)]
    return np.stack(h_batch, 0).astype(np.float32)
```

```python
import math
from contextlib import ExitStack

import numpy as np

import concourse.bass as bass
import concourse.mybir as mybir
from concourse.bass_utils import run_bass_kernel_spmd

F32 = mybir.dt.float32
BF16 = mybir.dt.bfloat16
I32 = mybir.dt.int32
AF = mybir.ActivationFunctionType
ALU = mybir.AluOpType
AX = mybir.AxisListType

NCORES = 8
D = 2048
NT = 1024
NTI = NT // 128
NTOK = 8192
HD = 128
W = 1536
MW = 512
NE = 32
NEL = 4
CAP = 1280
NSL = CAP // 128
NROW = NTOK + 128
ALPHA = (2 * 2) ** 0.25
LN_EPS = 1e-5
BIG = 1.0e6
SCALE = HD ** -0.5
DIL = (1, 4, 16)
TPREV = (128, 512, 2048)


def sl(start, cnt, step):
    return slice(start, start + (cnt - 1) * step + 1, step)


def _alibi_slopes(n):
    def pow2(m):
        start = 2.0 ** (-8.0 / m)
        return [start ** (i + 1) for i in range(m)]
    if math.log2(n).is_integer():
        s = pow2(n)
    else:
        c = 2 ** math.floor(math.log2(n))
        s = pow2(c) + pow2(2 * c)[0::2][: n - c]
    return sorted(s, reverse=True)


class Dep:
    __slots__ = ("w", "r", "wg")

    def __init__(self):
        self.w = {}
        self.r = {}
        self.wg = None


class Tile:
    def __init__(self, h, name, psum=False):
        self.h = h
        self.name = name
        self.psum = psum
        self.deps = {None: Dep()}

    def __getitem__(self, idx):
        return self.h[idx]


class Prog:
    def __init__(self, nc, es):
        self.nc = nc
        self.es = es
        self.engs = {"pe": nc.tensor, "act": nc.scalar, "dve": nc.vector, "pool": nc.gpsimd, "sp": nc.sync}
        self.sem = {k: es.enter_context(nc.semaphore("s_" + k)) for k in ("pe", "act", "dve", "pool")}
        self.cnt = {k: 0 for k in self.sem}
        self.pending = {k: False for k in self.sem}
        self.seen = {k: {} for k in self.engs}
        self.slots = {}
        self.rr = {}
        self.cc_sem = es.enter_context(nc.semaphore("s_cc"))
        self.cc_cnt = 0
        self.nwait = 0

    def dma_class(self, name, n):
        self.slots[name] = [[self.es.enter_context(self.nc.semaphore("d_%s%d" % (name, i))), 0, "d_%s%d" % (name, i)]
                            for i in range(n)]
        self.rr[name] = 0

    def _wait(self, eng, evs):
        seen = self.seen[eng]
        for key, (sem, val) in evs.items():
            if eng == "pe" and key == "pe":
                continue
            if seen.get(key, 0) >= val:
                continue
            self.engs[eng].wait_ge(sem, val)
            seen[key] = val
            self.nwait += 1

    @staticmethod
    def _merge(dst, src):
        for k, (s, v) in src.items():
            if k not in dst or dst[k][1] < v:
                dst[k] = (s, v)

    def _collect(self, reads, writes, accum):
        evs = {}
        for t, key in reads:
            if key is None:
                for d in t.deps.values():
                    self._merge(evs, d.w)
            else:
                self._merge(evs, t.deps[None].w)
                if key in t.deps:
                    self._merge(evs, t.deps[key].w)
        for t, key in writes:
            if key is None:
                for d in t.deps.values():
                    self._merge(evs, d.r)
                    if not (accum and d.wg == accum):
                        self._merge(evs, d.w)
            else:
                self._merge(evs, t.deps[None].w)
                self._merge(evs, t.deps[None].r)
                if key in t.deps:
                    d = t.deps[key]
                    self._merge(evs, d.r)
                    if not (accum and d.wg == accum):
                        self._merge(evs, d.w)
        return evs

    def _register(self, ev, reads, writes, accum):
        k, s, v = ev
        for t, key in reads:
            d = t.deps.setdefault(key, Dep())
            self._merge(d.r, {k: (s, v)})
        for t, key in writes:
            if key is None and not accum:
                t.deps = {None: Dep()}
                t.deps[None].w = {k: (s, v)}
            else:
                if key is None:
                    for kk in [kk for kk in t.deps if kk is not None]:
                        del t.deps[kk]
                d = t.deps.setdefault(key, Dep())
                if accum:
                    if d.wg != accum:
                        d.w = {}
                        d.r = {}
                        d.wg = accum
                    self._merge(d.w, {k: (s, v)})
                else:
                    d.w = {k: (s, v)}
                    d.r = {}
                    d.wg = None

    @staticmethod
    def _norm(reads, writes):
        r2 = [(t, k) for t, k in reads if not t.psum]
        w2 = [((t, None) if t.psum else (t, k)) for t, k in writes] + [(t, None) for t, k in reads if t.psum]
        seen, w3 = set(), []
        for t, k in w2:
            if (id(t), k) not in seen:
                seen.add((id(t), k))
                w3.append((t, k))
        return r2, w3

    def op(self, eng, fn, reads=(), writes=(), inc=True, accum=False):
        reads, writes = self._norm(reads, writes)
        self._wait(eng, self._collect(reads, writes, accum))
        ins = fn(self.engs[eng])
        if inc:
            self.cnt[eng] += 1
            ins.then_inc(self.sem[eng], 1)
            ev = (eng, self.sem[eng], self.cnt[eng])
            self.pending[eng] = False
        else:
            ev = (eng, self.sem[eng], self.cnt[eng] + 1)
            self.pending[eng] = True
        self._register(ev, reads, writes, accum)
        return ins

    def dma(self, q, cls, fn, reads=(), writes=(), accum=False):
        slot = self.slots[cls][self.rr[cls] % len(self.slots[cls])]
        self.rr[cls] += 1
        evs = self._collect(reads, writes, accum)
        if slot[1] > 0:
            self._merge(evs, {slot[2]: (slot[0], slot[1])})
        self._wait(q, evs)
        ins = fn(self.engs[q])
        slot[1] += 16
        ins.then_inc(slot[0], 16)
        self._register((slot[2], slot[0], slot[1]), reads, writes, accum)
        return ins

    def collective(self, fn, reads=(), writes=()):
        self._wait("pool", self._collect(reads, writes, False))
        ins = fn(self.engs["pool"])
        self.cc_cnt += 1
        ins.then_inc(self.cc_sem, 1)
        self._register(("cc", self.cc_sem, self.cc_cnt), reads, writes, False)

    def barrier(self):
        evs = {}
        for k in self.sem:
            assert not self.pending[k], k
            if self.cnt[k]:
                evs[k] = (self.sem[k], self.cnt[k])
        for cls in self.slots.values():
            for s in cls:
                if s[1]:
                    evs[s[2]] = (s[0], s[1])
        if self.cc_cnt:
            evs["cc"] = (self.cc_sem, self.cc_cnt)
        for e in self.engs:
            self._wait(e, evs)


class Builder:
    def __init__(self, cfg):
        self.cfg = cfg
        self.nc = bass.Bass("TRN2", target_bir_lowering=False)
        self.es = ExitStack()
        self.P = Prog(self.nc, self.es)
        for name, n in (("ld", 8), ("w", 6), ("g", 4), ("s", 8), ("sc", 8), ("st", 6)):
            self.P.dma_class(name, n)
        self.ins = {}
        self.uid = 0
        self.capreg = self.nc.gpsimd.to_reg(CAP - 1)

    def inp(self, name, shape, dt=F32):
        h = self.nc.dram_tensor(name, list(shape), dt, kind="ExternalInput")
        self.ins[name] = (tuple(shape), dt)
        return Tile(h, name)

    def outp(self, name, shape, dt=F32):
        return Tile(self.nc.dram_tensor(name, list(shape), dt, kind="ExternalOutput"), name)

    def dram(self, name, shape, dt, shared=False):
        if shared:
            h = self.nc.dram_tensor(name, list(shape), dt, addr_space="Shared")
        else:
            h = self.nc.dram_tensor(name, list(shape), dt)
        return Tile(h, name)

    def sb(self, st, name, shape, dt):
        self.uid += 1
        return Tile(st.enter_context(self.nc.sbuf_tensor("%s_%d" % (name, self.uid), list(shape), dt)), name)

    def ps(self, st, name, shape, dt):
        self.uid += 1
        return Tile(st.enter_context(self.nc.psum_tensor("%s_%d" % (name, self.uid), list(shape), dt)), name, psum=True)

    def load(self, dst, dst_ap, src, src_ap, q="sp", cls="ld", dkey=None, skey=None, accum=False):
        self.P.dma(q, cls, lambda e: e.dma_start(out=dst_ap, in_=src_ap), reads=[(src, skey)], writes=[(dst, dkey)], accum=accum)

    def layer_norm_tile(self, st_tiles, r, rkey, g_bc, b_bc, out, out_ap, okey):
        P = self.P
        stats, mv, rstd, xn = st_tiles
        for k in range(4):
            P.op("dve", lambda e, k=k: e.bn_stats(out=stats[:, k, :], in_=r[:, k * 512:(k + 1) * 512]),
                 reads=[(r, rkey)], writes=[(stats, k)])
        P.op("dve", lambda e: e.bn_aggr(out=mv[:, :], in_=stats[:, :, :]), reads=[(stats, None)], writes=[(mv, None)])
        P.op("dve", lambda e: e.tensor_scalar(out=rstd[:, :], in0=mv[:, 1:2], scalar1=LN_EPS, scalar2=None, op0=ALU.add),
             reads=[(mv, None)], writes=[(rstd, None)])
        P.op("act", lambda e: e.activation(out=rstd[:, :], in_=rstd[:, :], func=AF.Sqrt), reads=[(rstd, None)], writes=[(rstd, None)])
        P.op("dve", lambda e: e.reciprocal(out=rstd[:, :], in_=rstd[:, :]), reads=[(rstd, None)], writes=[(rstd, None)])
        P.op("dve", lambda e: e.tensor_scalar(out=xn[:, :], in0=r[:, :], scalar1=mv[:, 0:1], scalar2=rstd[:, 0:1],
                                                 op0=ALU.subtract, op1=ALU.mult),
             reads=[(r, rkey), (mv, None), (rstd, None)], writes=[(xn, None)])
        P.op("pool", lambda e: e.tensor_tensor(out=xn[:, :], in0=xn[:, :], in1=g_bc[:, :], op=ALU.mult),
             reads=[(xn, None), (g_bc, None)], writes=[(xn, None)])
        P.op("dve", lambda e: e.tensor_tensor(out=out_ap, in0=xn[:, :], in1=b_bc[:, :], op=ALU.add),
             reads=[(xn, None), (b_bc, None)], writes=[(out, okey)])

    def consts(self):
        P, I = self.P, self.I
        cs = self.es
        C = {}
        I["ident"] = self.inp("ident", [128, 128])
        C["ident_f"] = self.sb(cs, "ident_f", [128, 128], F32)
        C["ident_b"] = self.sb(cs, "ident_b", [128, 128], BF16)
        C["ones_f"] = self.sb(cs, "ones_f", [128, 128], F32)
        C["ones_b"] = self.sb(cs, "ones_b", [128, 128], BF16)
        self.load(C["ident_f"], C["ident_f"][:, :], I["ident"], I["ident"][:, :])
        P.op("dve", lambda e: e.tensor_copy(out=C["ident_b"][:, :], in_=C["ident_f"][:, :]),
             reads=[(C["ident_f"], None)], writes=[(C["ident_b"], None)])
        P.op("dve", lambda e: e.memset(C["ones_f"][:, :], 1.0), writes=[(C["ones_f"], None)])
        P.op("dve", lambda e: e.memset(C["ones_b"][:, :], 1.0), writes=[(C["ones_b"], None)])
        self.C = C

    def build(self):
        kind = self.cfg["kind"]
        self.I = {}
        self.dbg = {}
        self.consts()
        if kind == "A":
            self.build_A()
        elif kind == "B":
            self.build_B()
        else:
            self.build_C()
        self.P.barrier()
        return self.nc

    def build_A(self):
        nc, P, I, C = self.nc, self.P, self.I, self.C
        L = self.cfg["L"]
        I["x_own"] = self.inp("x_own", [NT, D])
        I["router_w"] = self.inp("router_w", [D, NE])
        I["router_b"] = self.inp("router_b", [1, NE])
        h1o = self.outp("h1_out", [NT, D])
        h1b = self.outp("h1b_out", [NT, D], BF16)
        go = self.outp("g_out", [NT, NE])
        self.h1o = h1o
        if self.cfg.get("mixer", True):
            if L == 0:
                self.mixer_dil()
            else:
                self.mixer_ret()
        else:
            self.P.dma("sp", "st", lambda e: e.dma_start(out=h1o[:, :], in_=I["x_own"][:, :]),
                       reads=[(I["x_own"], None)], writes=[(h1o, None)])
        P.barrier()
        hbuf = h1o
        with ExitStack() as st:
            hf = [self.sb(st, "hf", [128, D], F32) for _ in range(2)]
            hb = [self.sb(st, "hb", [128, D], BF16) for _ in range(2)]
            hT = [self.sb(st, "hT", [128, 16, 128], F32) for _ in range(2)]
            rw = self.sb(st, "rw", [128, 16, NE], F32)
            rb = self.sb(st, "rb", [1, NE], F32)
            lg = self.sb(st, "lg", [128, NTI, NE], F32)
            G = self.sb(st, "G", [128, NTI, NE], F32)
            ex = self.sb(st, "ex", [128, NTI, NE], F32)
            msk = self.sb(st, "msk", [128, NTI, NE], F32)
            m8 = self.sb(st, "m8", [128, NTI, 8], F32)
            nm = self.sb(st, "nm", [128, NTI], F32)
            ssum = self.sb(st, "ssum", [128, NTI], F32)
            ptr = [self.ps(st, "ptr", [128, 4, 128], F32) for _ in range(2)]
            plg = self.ps(st, "plg", [128, NTI, NE], F32)
            with nc.allow_non_contiguous_dma(reason="router weights 128B runs"):
                self.load(rw, rw[:, :, :], I["router_w"], I["router_w"].h.ap().rearrange("(c p) e -> p c e", p=128))
            self.load(rb, rb[:, :], I["router_b"], I["router_b"][:, :])
            for i in range(NTI):
                f, b_, t_ = hf[i % 2], hb[i % 2], hT[i % 2]
                self.load(f, f[:, :], hbuf, hbuf[i * 128:(i + 1) * 128, :])
                P.op("act", lambda e, f=f, b_=b_: e.activation(out=b_[:, :], in_=f[:, :], func=AF.Copy),
                     reads=[(f, None)], writes=[(b_, None)])
                self.P.dma("sp", "st", lambda e, b_=b_, i=i: e.dma_start(out=h1b[i * 128:(i + 1) * 128, :], in_=b_[:, :]),
                           reads=[(b_, None)], writes=[(h1b, i)])
                for cg in range(4):
                    pt = ptr[cg % 2]
                    for k in range(4):
                        c = cg * 4 + k
                        P.op("pe", lambda e, pt=pt, k=k, c=c, f=f: e.transpose(out=pt[:, k, :], in_=f[:, c * 128:(c + 1) * 128],
                                                                              identity=C["ident_f"][:, :]),
                             reads=[(f, None), (C["ident_f"], None)], writes=[(pt, k)])
                    if cg % 2 == 0:
                        P.op("dve", lambda e, pt=pt, t_=t_, cg=cg: e.tensor_copy(out=t_[:, cg * 4:(cg + 1) * 4, :], in_=pt[:, :, :]),
                             reads=[(pt, None)], writes=[(t_, cg)])
                    else:
                        P.op("act", lambda e, pt=pt, t_=t_, cg=cg: e.activation(out=t_[:, cg * 4:(cg + 1) * 4, :], in_=pt[:, :, :], func=AF.Copy),
                             reads=[(pt, None)], writes=[(t_, cg)])
                for c in range(16):
                    P.op("pe", lambda e, t_=t_, c=c, i=i: e.matmul(out=plg[:, i, :], lhsT=t_[:, c, :], rhs=rw[:, c, :],
                                                                   start=(c == 0), stop=False),
                         reads=[(t_, c // 4), (rw, None)], writes=[(plg, i)], inc=False)
                P.op("pe", lambda e, i=i: e.matmul(out=plg[:, i, :], lhsT=C["ones_f"][0:1, :], rhs=rb[0:1, :], start=False, stop=True),
                     reads=[(C["ones_f"], None), (rb, None)], writes=[(plg, i)])
            P.op("act", lambda e: e.activation(out=lg[:, :, :], in_=plg[:, :, :], func=AF.Copy), reads=[(plg, None)], writes=[(lg, None)])
            for i in range(NTI):
                P.op("dve", lambda e, i=i: e.max(out=m8[:, i, :], in_=lg[:, i, :]), reads=[(lg, None)], writes=[(m8, i)])
                P.op("dve", lambda e, i=i: e.tensor_scalar(out=msk[:, i, :], in0=lg[:, i, :], scalar1=m8[:, i, 3:4], scalar2=None,
                                                             op0=ALU.is_ge), reads=[(lg, None), (m8, i)], writes=[(msk, i)])
                P.op("dve", lambda e, i=i: e.tensor_scalar(out=nm[:, i:i + 1], in0=m8[:, i, 0:1], scalar1=-1.0, scalar2=None, op0=ALU.mult),
                     reads=[(m8, i)], writes=[(nm, i)])
                P.op("act", lambda e, i=i: e.activation(out=ex[:, i, :], in_=lg[:, i, :], func=AF.Exp, bias=nm[:, i:i + 1], scale=1.0),
                     reads=[(lg, None), (nm, i)], writes=[(ex, i)])
                P.op("dve", lambda e, i=i: e.tensor_tensor(out=ex[:, i, :], in0=ex[:, i, :], in1=msk[:, i, :], op=ALU.mult),
                     reads=[(ex, i), (msk, i)], writes=[(ex, i)])
                P.op("dve", lambda e, i=i: e.reduce_sum(out=ssum[:, i:i + 1], in_=ex[:, i, :], axis=AX.X),
                     reads=[(ex, i)], writes=[(ssum, i)])
                P.op("dve", lambda e, i=i: e.reciprocal(out=ssum[:, i:i + 1], in_=ssum[:, i:i + 1]),
                     reads=[(ssum, i)], writes=[(ssum, i)])
                P.op("dve", lambda e, i=i: e.tensor_scalar(out=G[:, i, :], in0=ex[:, i, :], scalar1=ssum[:, i:i + 1], scalar2=None, op0=ALU.mult),
                     reads=[(ex, i), (ssum, i)], writes=[(G, i)])
            self.P.dma("sp", "st", lambda e: e.dma_start(out=go.h.ap().rearrange("(i p) e -> p i e", p=128), in_=G[:, :, :]),
                       reads=[(G, None)], writes=[(go, None)])
            P.barrier()

    def build_B(self):
        nc, P, I, C = self.nc, self.P, self.I, self.C
        NL = self.cfg.get("nel", NEL)
        I["h1all"] = self.inp("h1all", [NROW, D], BF16)
        I["gall"] = self.inp("gall", [NTOK, NE])
        I["moe_win"] = self.inp("moe_win", [NL, 8, 128, 16, 512])
        I["moe_wout"] = self.inp("moe_wout", [NL, 4, 128, 16, 512])
        I["moe_bin"] = self.inp("moe_bin", [128, NL, 32])
        I["moe_bout"] = self.inp("moe_bout", [1, NL, D])
        I["psel"] = self.inp("psel", [128, NL, NE])
        I["padinit"] = self.inp("padinit", [128, NSL, 2])
        I["tokidf"] = self.inp("tokidf", [128, 64])
        I["tri"] = self.inp("tri", [128, 128])
        ypart = self.outp("ypart", [NROW * 4, 512])
        idxb = [self.dram("idxb%d" % j, [CAP, 2], F32) for j in range(NL)]
        h1all = I["h1all"]

        NLA = max(NL, 2)
        posi = self.sb(self.es, "posi", [128, NLA, 64], I32)
        zb = self.sb(self.es, "zero_f", [128, 1024], F32)
        P.op("pool", lambda e: e.memset(zb[:, :], 0.0), writes=[(zb, None)])
        vals = self.sb(self.es, "vals", [128, NLA, 64, 2], F32)

        def scatter_ids(j):
            iv = idxb[j]
            for c in range(64):
                P.dma("pool", "sc", lambda e, j=j, c=c, iv=iv: e.indirect_dma_start(
                    out=iv[:, :], out_offset=bass.IndirectOffsetOnAxis(ap=posi[:, j, c:c + 1], axis=0),
                    in_=vals[:, j, c, :], in_offset=None, bounds_check=self.capreg, oob_is_err=False),
                    reads=[(vals, None), (posi, None)], writes=[(iv, None)], accum="sc")

        with ExitStack() as st:
            Gl = self.sb(st, "Gl", [128, 64, NE], F32)
            tmp = self.sb(st, "tmp", [128, 64, NE], F32)
            psel = self.sb(st, "psel", [128, NL, NE], F32)
            tokf = self.sb(st, "tokf", [128, 64], F32)
            tri = self.sb(st, "tri", [128, 128], F32)
            Gmy = self.sb(st, "Gmy", [128, NLA, 64], F32)
            mk = self.sb(st, "mk", [128, NLA, 64], F32)
            sA = self.sb(st, "sA", [128, NLA, 64], F32)
            sB = self.sb(st, "sB", [128, NLA, 64], F32)
            pos = self.sb(st, "pos", [128, NLA, 64], F32)
            padi = self.sb(st, "padi", [128, NSL, 2], F32)
            ppw = self.ps(st, "ppw", [128, NLA * 64], F32)
            ptot = self.ps(st, "ptot", [128, NLA * 64], F32)
            with nc.allow_non_contiguous_dma(reason="gate matrix 128B runs"):
                self.load(Gl, Gl[:, :, :], I["gall"], I["gall"].h.ap().rearrange("(c p) e -> p c e", p=128))
            self.load(psel, psel[:, :, :], I["psel"], I["psel"][:, :, :])
            self.load(tokf, tokf[:, :], I["tokidf"], I["tokidf"][:, :])
            self.load(tri, tri[:, :], I["tri"], I["tri"][:, :])
            self.load(padi, padi[:, :, :], I["padinit"], I["padinit"][:, :, :])
            ypv = ypart.h.ap().rearrange("(a p r) n -> a p (r n)", p=128, r=2)
            for a in range(NROW // 64):
                self.P.dma("sp", "st", lambda e, a=a: e.dma_start(out=ypv[a], in_=zb[:, :]),
                           reads=[(zb, None)], writes=[(ypart, None)], accum="z")

            for j in range(NL):
                iv = idxb[j]
                with nc.allow_non_contiguous_dma(reason="8B rows"):
                    self.P.dma("sp", "st", lambda e, iv=iv: e.dma_start(out=iv.h.ap().rearrange("(i p) t -> p i t", p=128), in_=padi[:, :, :]),
                               reads=[(padi, None)], writes=[(iv, None)])
                P.op("dve", lambda e, j=j: e.tensor_tensor(out=tmp[:, :, :], in0=Gl[:, :, :],
                                                             in1=psel[:, j:j + 1, :].to_broadcast([128, 64, NE]), op=ALU.mult),
                     reads=[(Gl, None), (psel, None)], writes=[(tmp, None)])
                P.op("dve", lambda e, j=j: e.reduce_sum(out=Gmy[:, j, :], in_=tmp[:, :, :], axis=AX.X),
                     reads=[(tmp, None)], writes=[(Gmy, j)])
            if NLA > NL:
                P.op("dve", lambda e: e.memset(Gmy[:, NL:, :], 0.0), writes=[(Gmy, "pad")])
            P.op("dve", lambda e: e.tensor_single_scalar(out=mk[:, :, :], in_=Gmy[:, :, :], scalar=0.0, op=ALU.is_gt),
                 reads=[(Gmy, None)], writes=[(mk, None)])
            mk2 = mk.h.ap().rearrange("p j c -> p (j c)")
            P.op("pe", lambda e: e.matmul(out=ppw[:, :], lhsT=tri[:, :], rhs=mk2, start=True, stop=True),
                 reads=[(tri, None), (mk, None)], writes=[(ppw, None)])
            P.op("pe", lambda e: e.matmul(out=ptot[:, :], lhsT=C["ones_f"][:, :], rhs=mk2, start=True, stop=True),
                 reads=[(C["ones_f"], None), (mk, None)], writes=[(ptot, None)])
            P.op("act", lambda e: e.activation(out=sA.h.ap().rearrange("p j c -> p (j c)"), in_=ptot[:, :], func=AF.Copy),
                 reads=[(ptot, None)], writes=[(sA, None)])
            a, b_ = sA, sB
            s = 1
            while s < 64:
                P.op("dve", lambda e, a=a, b_=b_, s=s: e.tensor_tensor(out=b_[:, :, s:], in0=a[:, :, s:], in1=a[:, :, :64 - s], op=ALU.add),
                     reads=[(a, None)], writes=[(b_, "hi")])
                P.op("pool", lambda e, a=a, b_=b_, s=s: e.tensor_copy(out=b_[:, :, :s], in_=a[:, :, :s]),
                     reads=[(a, None)], writes=[(b_, "lo")])
                a, b_ = b_, a
                s *= 2
            flat = lambda t: t.h.ap().rearrange("p j c -> p (j c)")
            P.op("dve", lambda e, a=a: e.tensor_tensor(out=flat(pos), in0=flat(a), in1=ptot[:, :], op=ALU.subtract),
                 reads=[(a, None), (ptot, None)], writes=[(pos, None)])
            P.op("dve", lambda e: e.tensor_tensor(out=flat(pos), in0=flat(pos), in1=ppw[:, :], op=ALU.add),
                 reads=[(pos, None), (ppw, None)], writes=[(pos, None)])
            P.op("dve", lambda e: e.tensor_scalar(out=mk[:, :, :], in0=mk[:, :, :], scalar1=-BIG, scalar2=BIG, op0=ALU.mult, op1=ALU.add),
                 reads=[(mk, None)], writes=[(mk, None)])
            P.op("dve", lambda e: e.tensor_tensor(out=pos[:, :, :], in0=pos[:, :, :], in1=mk[:, :, :], op=ALU.add),
                 reads=[(pos, None), (mk, None)], writes=[(pos, None)])
            P.op("dve", lambda e: e.tensor_copy(out=posi[:, :, :], in_=pos[:, :, :]), reads=[(pos, None)], writes=[(posi, None)])
            for j in range(NL):
                P.op("pool", lambda e, j=j: e.tensor_copy(out=vals[:, j, :, 0], in_=tokf[:, :]), reads=[(tokf, None)], writes=[(vals, (j, 0))])
                P.op("pool", lambda e, j=j: e.tensor_copy(out=vals[:, j, :, 1], in_=Gmy[:, j, :]), reads=[(Gmy, None)], writes=[(vals, (j, 1))])
            if "pos" in self.cfg.get("debug", ()):
                self.debug_out("dbg_pos", pos, [128, NLA, 64])
            scatter_ids(0)
            P.barrier()

        with ExitStack() as st:
            xeT = self.sb(st, "xeT", [128, 16, CAP], BF16)
            actT = self.sb(st, "actT", [128, 16, CAP], BF16)
            wp = [self.sb(st, "wp", [128, 16, 512], BF16) for _ in range(2)]
            wo = [self.sb(st, "wo", [128, 16, 512], BF16) for _ in range(2)]
            xg = [self.sb(st, "xg", [128, D], BF16) for _ in range(2)]
            itl = [self.sb(st, "itl", [128, 2], F32) for _ in range(2)]
            idi = [self.sb(st, "idi", [128, 1], I32) for _ in range(2)]
            id8f = [[self.sb(st, "id8f", [128, 4], F32) for _ in range(NSL)] for _ in range(2)]
            id8 = [[self.sb(st, "id8", [128, 4], I32) for _ in range(NSL)] for _ in range(2)]
            io8 = self.sb(st, "io8", [128, 4], F32)
            t8 = [self.sb(st, "t8", [128, 1], F32) for _ in range(2)]
            gat = [self.sb(st, "gat", [128, NSL], F32) for _ in range(2)]
            bin_t = self.sb(st, "bin_t", [128, NL, 32], F32)
            bin1 = self.sb(st, "bin1", [128, NL, 16], F32)
            bout = self.sb(st, "bout", [1, NL, D], BF16)
            gc = [self.sb(st, "gc", [128, 512], F32) for _ in range(2)]
            sg = [self.sb(st, "sg", [128, 512], F32) for _ in range(2)]
            lc = [self.sb(st, "lc", [128, 512], F32) for _ in range(2)]
            ysc = [self.sb(st, "ysc", [128, 512], F32) for _ in range(6)]
            pT = [self.ps(st, "pT", [128, 8, 128], BF16) for _ in range(2)]
            pg = [self.ps(st, "pg", [128, 512], F32) for _ in range(2)]
            pl = [self.ps(st, "pl", [128, 512], F32) for _ in range(2)]
            py = [self.ps(st, "py", [128, 512], F32) for _ in range(2)]

            self.load(bin_t, bin_t[:, :, :], I["moe_bin"], I["moe_bin"][:, :, :])
            P.op("dve", lambda e: e.tensor_scalar(out=bin1[:, :, :], in0=bin_t[:, :, 16:32], scalar1=1.0, scalar2=None, op0=ALU.add),
                 reads=[(bin_t, None)], writes=[(bin1, None)])
            self.load(bout, bout.h.ap().rearrange("o j (a n) -> o (j a) n", a=4), I["moe_bout"],
                      I["moe_bout"].h.ap().rearrange("o j (a n) -> o (j a) n", a=4), q="pool", cls="w")
            for k in range(4):
                P.op("dve", lambda e, k=k: e.memset(io8[:, k:k + 1], float(k)), writes=[(io8, k)])
            n_sw = 0
            n_y = 0
            batches = [(0, 512), (512, 512), (1024, 256)]

            def prep_gather(j, i):
                iv = idxb[j]
                it_, ii_, x_ = itl[i % 2], idi[i % 2], xg[i % 2]
                gat_, id8_, id8f_ = gat[j % 2], id8[j % 2], id8f[j % 2]
                self.load(it_, it_[:, :], iv, iv[i * 128:(i + 1) * 128, :])
                P.op("dve", lambda e: e.tensor_copy(out=ii_[:, :], in_=it_[:, 0:1]), reads=[(it_, None)], writes=[(ii_, None)])
                P.op("dve", lambda e: e.tensor_copy(out=gat_[:, i:i + 1], in_=it_[:, 1:2]), reads=[(it_, None)], writes=[(gat_, i)])
                P.op("dve", lambda e: e.tensor_scalar(out=t8[i % 2][:, :], in0=it_[:, 0:1], scalar1=4.0, scalar2=None, op0=ALU.mult),
                     reads=[(it_, None)], writes=[(t8[i % 2], None)])
                P.op("dve", lambda e: e.tensor_scalar(out=id8f_[i][:, :], in0=io8[:, :], scalar1=t8[i % 2][:, 0:1], scalar2=None, op0=ALU.add),
                     reads=[(t8[i % 2], None), (io8, None)], writes=[(id8f_[i], None)])
                P.op("dve", lambda e: e.tensor_copy(out=id8_[i][:, :], in_=id8f_[i][:, :]), reads=[(id8f_[i], None)], writes=[(id8_[i], None)])
                P.dma("pool", "g", lambda e: e.indirect_dma_start(
                    out=x_[:, :], out_offset=None, in_=h1all[:, :], in_offset=bass.IndirectOffsetOnAxis(ap=ii_[:, 0:1], axis=0)),
                    reads=[(h1all, None), (ii_, None)], writes=[(x_, None)])

            def prep_transpose(j, i):
                x_ = xg[i % 2]
                for cg in range(2):
                    pt = pT[cg % 2]
                    for k in range(8):
                        c = cg * 8 + k
                        P.op("pe", lambda e, pt=pt, k=k, c=c: e.transpose(out=pt[:, k, :], in_=x_[:, c * 128:(c + 1) * 128], identity=C["ident_b"][:, :]),
                             reads=[(x_, None), (C["ident_b"], None)], writes=[(pt, k)])
                    if cg == 0:
                        P.op("dve", lambda e, pt=pt: e.tensor_copy(out=xeT[:, 0:8, i * 128:(i + 1) * 128], in_=pt[:, :, :]),
                             reads=[(pt, None)], writes=[(xeT, i)], accum="x%d" % j)
                    else:
                        P.op("act", lambda e, pt=pt: e.activation(out=xeT[:, 8:16, i * 128:(i + 1) * 128], in_=pt[:, :, :], func=AF.Copy),
                             reads=[(pt, None)], writes=[(xeT, i)], accum="x%d" % j)

            for i in range(NSL):
                prep_gather(0, i)
                prep_transpose(0, i)
            for j in range(NL):
                gat_, id8_ = gat[j % 2], id8[j % 2]
                for k in range(8):
                    w_ = wp[k % 2]
                    self.load(w_, w_[:, :, :], I["moe_win"], I["moe_win"].h.ap()[j, k], q="pool", cls="w")
                    if k == 2 and j + 1 < NL:
                        scatter_ids(j + 1)
                    for jj in range(2):
                        m = 2 * k + jj
                        for bi, (s0, nb) in enumerate(batches):
                            g_, l_ = pg[n_sw % 2], pl[n_sw % 2]
                            gc_, sg_, lc_ = gc[n_sw % 2], sg[n_sw % 2], lc[n_sw % 2]
                            n_sw += 1
                            rk = [(xeT, t) for t in range(s0 // 128, (s0 + nb) // 128)]
                            for c in range(16):
                                P.op("pe", lambda e, g_=g_, w_=w_, c=c, jj=jj, s0=s0, nb=nb: e.matmul(
                                    out=g_[:, 0:nb], lhsT=w_[:, c, jj * 128:(jj + 1) * 128], rhs=xeT[:, c, s0:s0 + nb],
                                    start=(c == 0), stop=(c == 15)), reads=[(w_, None)] + rk, writes=[(g_, None)], inc=(c == 15))
                            for c in range(16):
                                P.op("pe", lambda e, l_=l_, w_=w_, c=c, jj=jj, s0=s0, nb=nb: e.matmul(
                                    out=l_[:, 0:nb], lhsT=w_[:, c, 256 + jj * 128:256 + (jj + 1) * 128], rhs=xeT[:, c, s0:s0 + nb],
                                    start=(c == 0), stop=(c == 15)), reads=[(w_, None)] + rk, writes=[(l_, None)], inc=(c == 15))
                            P.op("dve", lambda e, g_=g_, gc_=gc_, m=m, nb=nb, j=j: e.tensor_scalar(
                                out=gc_[:, 0:nb], in0=g_[:, 0:nb], scalar1=bin_t[:, j, m:m + 1], scalar2=7.0, op0=ALU.add, op1=ALU.min),
                                reads=[(g_, None), (bin_t, None)], writes=[(gc_, None)])
                            P.op("act", lambda e, gc_=gc_, sg_=sg_, nb=nb: e.activation(out=sg_[:, 0:nb], in_=gc_[:, 0:nb], func=AF.Sigmoid, scale=1.702),
                                 reads=[(gc_, None)], writes=[(sg_, None)])
                            P.op("dve", lambda e, l_=l_, lc_=lc_, m=m, nb=nb, j=j: e.tensor_scalar(
                                out=lc_[:, 0:nb], in0=l_[:, 0:nb], scalar1=bin1[:, j, m:m + 1], scalar2=8.0, op0=ALU.add, op1=ALU.min),
                                reads=[(l_, None), (bin1, None)], writes=[(lc_, None)])
                            P.op("dve", lambda e, gc_=gc_, lc_=lc_, nb=nb: e.scalar_tensor_tensor(
                                out=lc_[:, 0:nb], in0=lc_[:, 0:nb], scalar=-6.0, in1=gc_[:, 0:nb], op0=ALU.max, op1=ALU.mult),
                                reads=[(gc_, None), (lc_, None)], writes=[(lc_, None)])
                            P.op("dve", lambda e, sg_=sg_, lc_=lc_, m=m, s0=s0, nb=nb: e.tensor_tensor(
                                out=actT[:, m, s0:s0 + nb], in0=lc_[:, 0:nb], in1=sg_[:, 0:nb], op=ALU.mult),
                                reads=[(sg_, None), (lc_, None)], writes=[(actT, bi)], accum="a%d" % j)
                it = 0
                for m2 in range(4):
                    o_ = wo[m2 % 2]
                    self.load(o_, o_[:, :, :], I["moe_wout"], I["moe_wout"].h.ap()[j, m2], q="pool", cls="w")
                    for i in range(NSL):
                        y_ = py[n_y % 2]
                        ys_ = ysc[n_y % len(ysc)]
                        n_y += 1
                        bi = 0 if i < 4 else (1 if i < 8 else 2)
                        for c in range(16):
                            P.op("pe", lambda e, y_=y_, o_=o_, c=c, i=i: e.matmul(
                                out=y_[:, :], lhsT=actT[:, c, i * 128:(i + 1) * 128], rhs=o_[:, c, :], start=(c == 0), stop=False),
                                reads=[(actT, bi), (o_, None)], writes=[(y_, None)], inc=False)
                        P.op("pe", lambda e, y_=y_, m2=m2, j=j: e.matmul(
                            out=y_[:, :], lhsT=C["ones_b"][0:1, :], rhs=bout[0:1, j, m2 * 512:(m2 + 1) * 512], start=False, stop=True),
                            reads=[(C["ones_b"], None), (bout, None)], writes=[(y_, None)])
                        P.op("act", lambda e, y_=y_, ys_=ys_, i=i, gat_=gat_: e.activation(out=ys_[:, :], in_=y_[:, :], func=AF.Copy, scale=gat_[:, i:i + 1]),
                             reads=[(y_, None), (gat_, i)], writes=[(ys_, None)])
                        P.dma("pool", "s", lambda e, ys_=ys_, i=i, m2=m2, id8_=id8_: e.indirect_dma_start(
                            out=ypart[:, :], out_offset=bass.IndirectOffsetOnAxis(ap=id8_[i][:, m2:m2 + 1], axis=0),
                            in_=ys_[:, :], in_offset=None, compute_op=ALU.add),
                            reads=[(ys_, None), (id8_[i], None)], writes=[(ypart, None)], accum="y%d" % j)
                        if j + 1 < NL:
                            if it < NSL:
                                prep_gather(j + 1, it)
                            if 1 <= it <= NSL:
                                prep_transpose(j + 1, it - 1)
                        it += 1
            P.barrier()

    def build_C(self):
        nc, P, I, C = self.nc, self.P, self.I, self.C
        I["parts"] = self.inp("parts", [NCORES, NT, D])
        I["h1"] = self.inp("h1", [NT, D])
        I["ln_g"] = self.inp("ln_g", [1, D])
        I["ln_b"] = self.inp("ln_b", [1, D])
        h2 = self.outp("h2_out", [NT, D])
        with ExitStack() as st:
            gb = self.sb(st, "gb", [128, D], F32)
            bb = self.sb(st, "bb", [128, D], F32)
            pt_ = [self.sb(st, "pt", [128, NCORES, D], F32) for _ in range(2)]
            hf = [self.sb(st, "hf2", [128, D], F32) for _ in range(2)]
            acc = [self.sb(st, "acc", [128, D], F32) for _ in range(2)]
            ot = [self.sb(st, "ot", [128, D], F32) for _ in range(2)]
            lnt = (self.sb(st, "stats", [128, 4, 6], F32), self.sb(st, "mv", [128, 2], F32),
                   self.sb(st, "rstd", [128, 1], F32), self.sb(st, "xn", [128, D], F32))
            self.load(gb, gb[:, :], I["ln_g"], I["ln_g"].h.ap().partition_broadcast(128))
            self.load(bb, bb[:, :], I["ln_b"], I["ln_b"].h.ap().partition_broadcast(128))
            for i in range(NTI):
                p_, f, a_, o_ = pt_[i % 2], hf[i % 2], acc[i % 2], ot[i % 2]
                for r in range(NCORES):
                    self.load(p_, p_[:, r, :], I["parts"], I["parts"].h.ap()[r, i * 128:(i + 1) * 128, :], dkey=r)
                self.load(f, f[:, :], I["h1"], I["h1"][i * 128:(i + 1) * 128, :])
                P.op("dve", lambda e, p_=p_, f=f, a_=a_: e.scalar_tensor_tensor(out=a_[:, :], in0=f[:, :], scalar=ALPHA, in1=p_[:, 0, :],
                                                                                op0=ALU.mult, op1=ALU.add),
                     reads=[(f, None), (p_, 0)], writes=[(a_, None)])
                for r in range(1, NCORES):
                    eng = "dve" if r % 3 != 0 else "pool"
                    P.op(eng, lambda e, p_=p_, a_=a_, r=r: e.tensor_tensor(out=a_[:, :], in0=a_[:, :], in1=p_[:, r, :], op=ALU.add),
                         reads=[(a_, None), (p_, r)], writes=[(a_, None)])
                self.layer_norm_tile(lnt, a_, None, gb, bb, o_, o_[:, :], None)
                self.P.dma("sp", "st", lambda e, o_=o_, i=i: e.dma_start(out=h2[i * 128:(i + 1) * 128, :], in_=o_[:, :]),
                           reads=[(o_, None)], writes=[(h2, i)])
            P.barrier()

    def debug_out(self, name, tile, shape, dt=F32):
        o = self.outp(name, shape, dt)
        self.dbg[name] = o
        idx = tuple(slice(None) for _ in shape)
        self.P.dma("sp", "st", lambda e: e.dma_start(out=o[idx], in_=tile[idx]), reads=[(tile, None)], writes=[(o, None)])

    def stage_xT(self, st_unused, src, ntiles, dstT, tok0, xs, pT):
        P, C = self.P, self.C
        for t in range(ntiles):
            x_ = xs[t % 2]
            self.load(x_, x_.h.ap().rearrange("p (a n) -> p a n", a=4), src,
                      src[t * 128:(t + 1) * 128, :].rearrange("p (a n) -> p a n", a=4), q="pool", cls="w")
            for cg in range(2):
                for k in range(8):
                    c = cg * 8 + k
                    P.op("pe", lambda e, k=k, c=c, x_=x_: e.transpose(out=pT[:, k, :], in_=x_[:, c * 128:(c + 1) * 128], identity=C["ident_b"][:, :]),
                         reads=[(x_, None), (C["ident_b"], None)], writes=[(pT, k)])
                o0 = tok0 + t * 128
                if cg == 0:
                    P.op("dve", lambda e, cg=cg, o0=o0: e.tensor_copy(out=dstT[:, 0:8, o0:o0 + 128], in_=pT[:, :, :]),
                         reads=[(pT, None)], writes=[(dstT, ("t", o0 // 128))], accum="stage")
                else:
                    P.op("act", lambda e, cg=cg, o0=o0: e.activation(out=dstT[:, 8:16, o0:o0 + 128], in_=pT[:, :, :], func=AF.Copy),
                         reads=[(pT, None)], writes=[(dstT, ("t", o0 // 128))], accum="stage")

    def mem_kv(self, outer, wmem_in, memb_in):
        P, C = self.P, self.C
        memK = self.sb(outer, "memK", [128, 4, 256], BF16)
        memV = self.sb(outer, "memV", [128, 2, 512], BF16)
        with ExitStack() as st:
            wm = self.sb(st, "wm", [128, 16, 1024], BF16)
            memT = self.sb(st, "memT", [128, 16, 256], BF16)
            xs = [self.sb(st, "xs", [128, D], BF16) for _ in range(2)]
            pT = self.ps(st, "pT", [128, 8, 128], BF16)
            pa = [self.ps(st, "pa", [128, 512], F32) for _ in range(2)]
            self.load(wm, wm[:, :, :], wmem_in, wmem_in.h.ap().rearrange("(c p) n -> p c n", p=128), q="pool", cls="w")
            self.stage_xT(st, memb_in, 2, memT, 0, xs, pT)
            n = 0
            for mh in range(4):
                a_ = pa[n % 2]; n += 1
                for c in range(16):
                    P.op("pe", lambda e, a_=a_, c=c, mh=mh: e.matmul(out=a_[:, 0:256], lhsT=wm[:, c, mh * 128:(mh + 1) * 128], rhs=memT[:, c, :],
                                                                      start=(c == 0), stop=(c == 15)),
                         reads=[(wm, None), (memT, None)], writes=[(a_, None)], inc=(c == 15))
                P.op("act", lambda e, a_=a_, mh=mh: e.activation(out=memK[:, mh, :], in_=a_[:, 0:256], func=AF.Copy),
                     reads=[(a_, None)], writes=[(memK, mh)])
            for mt in range(2):
                a_ = pa[n % 2]; n += 1
                for c in range(16):
                    P.op("pe", lambda e, a_=a_, c=c, mt=mt: e.matmul(out=a_[:, :], lhsT=memT[:, c, mt * 128:(mt + 1) * 128], rhs=wm[:, c, 512:1024],
                                                                      start=(c == 0), stop=(c == 15)),
                         reads=[(wm, None), (memT, None)], writes=[(a_, None)], inc=(c == 15))
                P.op("dve", lambda e, a_=a_, mt=mt: e.tensor_copy(out=memV[:, mt, :], in_=a_[:, :]), reads=[(a_, None)], writes=[(memV, mt)])
            P.barrier()
        return memK, memV

    def mem_attn(self, st, w_in, col0, srcT, tok0, memK, memV, catT, wbuf, qbuf, pa, pss, poo, ex, rd):
        P, C = self.P, self.C
        n = 0
        for mh in range(4):
            w_ = wbuf[mh % len(wbuf)]
            with self.nc.allow_non_contiguous_dma(reason="512B runs"):
                self.load(w_, w_[:, :, :], w_in, w_in.h.ap()[:, col0 + mh * 128:col0 + (mh + 1) * 128].rearrange("(c p) n -> p c n", p=128), q="pool", cls="w")
            for hf_ in range(2):
                a_ = pa[n % 2]; n += 1
                for c in range(16):
                    P.op("pe", lambda e, a_=a_, c=c, w_=w_, hf_=hf_: e.matmul(out=a_[:, :], lhsT=w_[:, c, :], rhs=srcT[:, c, tok0 + hf_ * 512:tok0 + (hf_ + 1) * 512],
                                                                             start=(c == 0), stop=(c == 15)),
                         reads=[(w_, None), (srcT, None)], writes=[(a_, None)], inc=(c == 15))
                P.op("act", lambda e, a_=a_, hf_=hf_: e.activation(out=qbuf[:, hf_ * 512:(hf_ + 1) * 512], in_=a_[:, :], func=AF.Copy),
                     reads=[(a_, None)], writes=[(qbuf, hf_)])
            for hf_ in range(2):
                for mt in range(2):
                    P.op("pe", lambda e, mt=mt, mh=mh, hf_=hf_: e.matmul(out=pss[mt][:, :], lhsT=memK[:, mh, mt * 128:(mt + 1) * 128],
                                                                        rhs=qbuf[:, hf_ * 512:(hf_ + 1) * 512], start=True, stop=True),
                         reads=[(memK, None), (qbuf, hf_)], writes=[(pss[mt], None)])
                    P.op("act", lambda e, mt=mt: e.activation(out=ex[:, mt, :], in_=pss[mt][:, :], func=AF.Exp, scale=SCALE),
                         reads=[(pss[mt], None)], writes=[(ex, mt)])
                for mt in range(2):
                    P.op("pe", lambda e, mt=mt, mh=mh: e.matmul(out=poo[0][:, :], lhsT=memV[:, mt, mh * 128:(mh + 1) * 128], rhs=ex[:, mt, :],
                                                               start=(mt == 0), stop=(mt == 1)),
                         reads=[(memV, None), (ex, mt)], writes=[(poo[0], None)], inc=(mt == 1))
                for mt in range(2):
                    P.op("pe", lambda e, mt=mt: e.matmul(out=poo[1][:, :], lhsT=C["ones_b"][:, :], rhs=ex[:, mt, :], start=(mt == 0), stop=(mt == 1)),
                         reads=[(C["ones_b"], None), (ex, mt)], writes=[(poo[1], None)], inc=(mt == 1))
                P.op("dve", lambda e: e.reciprocal(out=rd[:, 0:512], in_=poo[1][:, :]), reads=[(poo[1], None)], writes=[(rd, None)])
                P.op("dve", lambda e, mh=mh, hf_=hf_: e.tensor_tensor(out=catT[:, 12 + mh, hf_ * 512:(hf_ + 1) * 512], in0=poo[0][:, :], in1=rd[:, 0:512], op=ALU.mult),
                     reads=[(poo[0], None), (rd, None)], writes=[(catT, ("m", mh, hf_))])

    def mix_out(self, catT, wmix_in, xres_in, lng_in, lnb_in, out_t):
        P, C = self.P, self.C
        with ExitStack() as st:
            wm = self.sb(st, "wmx", [128, 16, D], BF16)
            gb = self.sb(st, "gb", [128, D], F32)
            bb = self.sb(st, "bb", [128, D], F32)
            xt = [self.sb(st, "xt", [128, D], F32) for _ in range(2)]
            rt = [self.sb(st, "rt", [128, D], F32) for _ in range(1)]
            ot = [self.sb(st, "ot", [128, D], F32) for _ in range(1)]
            lnt = (self.sb(st, "stats", [128, 4, 6], F32), self.sb(st, "mv", [128, 2], F32),
                   self.sb(st, "rstd", [128, 1], F32), self.sb(st, "xn", [128, D], F32))
            pm = [self.ps(st, "pm", [128, 512], F32) for _ in range(4)]
            for n in range(4):
                self.load(wm, wm[:, :, n * 512:(n + 1) * 512], wmix_in, wmix_in.h.ap()[:, n * 512:(n + 1) * 512].rearrange("(c p) n -> p c n", p=128),
                          q="pool", cls="w", dkey=n)
            self.load(gb, gb[:, :], lng_in, lng_in.h.ap().partition_broadcast(128))
            self.load(bb, bb[:, :], lnb_in, lnb_in.h.ap().partition_broadcast(128))
            for i in range(NTI):
                x_, r_, o_ = xt[i % 2], rt[0], ot[0]
                self.load(x_, x_[:, :], xres_in, xres_in[i * 128:(i + 1) * 128, :])
                for n in range(4):
                    for c in range(16):
                        P.op("pe", lambda e, n=n, c=c, i=i: e.matmul(out=pm[n][:, :], lhsT=catT[:, c, i * 128:(i + 1) * 128], rhs=wm[:, c, n * 512:(n + 1) * 512],
                                                                    start=(c == 0), stop=(c == 15)),
                             reads=[(catT, None), (wm, n)], writes=[(pm[n], None)], inc=(c == 15))
                    P.op("dve", lambda e, n=n, x_=x_, r_=r_: e.scalar_tensor_tensor(out=r_[:, n * 512:(n + 1) * 512], in0=x_[:, n * 512:(n + 1) * 512], scalar=ALPHA,
                                                                                    in1=pm[n][:, :], op0=ALU.mult, op1=ALU.add),
                         reads=[(x_, None), (pm[n], None)], writes=[(r_, n)])
                self.layer_norm_tile(lnt, r_, None, gb, bb, o_, o_[:, :], None)
                self.P.dma("sp", "st", lambda e, o_=o_, i=i: e.dma_start(out=out_t[i * 128:(i + 1) * 128, :], in_=o_[:, :]),
                           reads=[(o_, None)], writes=[(out_t, i)])
            P.barrier()

    def mixer_dil(self):
        nc, P, I, C = self.nc, self.P, self.I, self.C
        I["x_prev"] = self.inp("x_prev", [2048, D])
        I["memb"] = self.inp("memb", [256, D])
        I["w_in"] = self.inp("w_in", [D, 3 * W + MW])
        I["w_mem"] = self.inp("w_mem", [D, 2 * MW])
        I["w_mix"] = self.inp("w_mix", [D, D])
        I["ln1_g"] = self.inp("ln1_g", [1, D])
        I["ln1_b"] = self.inp("ln1_b", [1, D])
        I["attn_c"] = self.inp("attn_c", [128, 4, 128])
        w_in = I["w_in"]
        slopes = _alibi_slopes(12)
        with ExitStack() as outer:
            catT = self.sb(outer, "catT", [128, 16, NT], BF16)
            memK, memV = self.mem_kv(outer, I["w_mem"], I["memb"])
            with ExitStack() as st:
                xT = self.sb(st, "xT", [128, 16, 3072], BF16)
                xs = [self.sb(st, "xs", [128, D], BF16) for _ in range(2)]
                wq = [self.sb(st, "wq", [128, 16, 128], BF16) for _ in range(1)]
                wk = [self.sb(st, "wk", [128, 16, 128], BF16) for _ in range(1)]
                wv = [self.sb(st, "wv", [128, 16, 128], BF16) for _ in range(1)]
                nat = self.sb(st, "nat", [128, 3072], BF16)
                qTd = self.sb(st, "qTd", [128, NT], BF16)
                kTd = self.sb(st, "kTd", [128, 3072], BF16)
                vTd = self.sb(st, "vTd", [128, 3072], BF16)
                Vt = self.sb(st, "Vt", [128, 32, 128], BF16)
                Ob = self.sb(st, "Ob", [128, 3, NT], BF16)
                den = self.sb(st, "den", [128, NT], F32)
                rd = self.sb(st, "rd", [128, NT], F32)
                dtab = self.sb(st, "dtab", [128, 4, 128], F32)
                zz = [self.sb(st, "zz", [128, 2, 128], F32) for _ in range(2)]
                pp = [self.sb(st, "pp", [128, 2, 128], BF16) for _ in range(2)]
                exm = self.sb(st, "exm", [128, 2, 512], BF16)
                pT = self.ps(st, "pT", [128, 8, 128], BF16)
                pa = [self.ps(st, "pa", [128, 512], F32) for _ in range(2)]
                pss = [self.ps(st, "pss", [128, 512], F32) for _ in range(2)]
                poo = [self.ps(st, "poo", [128, 512], F32) for _ in range(2)]
                self.load(dtab, dtab[:, :, :], I["attn_c"], I["attn_c"][:, :, :])
                self.stage_xT(st, I["x_prev"], 16, xT, 0, xs, pT)
                self.stage_xT(st, I["x_own"], 8, xT, 2048, xs, pT)
                npa = 0
                nq = 0
                nh = 0
                stop = self.cfg.get("stop", 99)
                for j in range(4 if stop > 0 else 0):
                    for g in range(self.cfg.get("gmax", 3)):
                        hh = 4 * g + j
                        d = DIL[g]
                        Tp = TPREV[g]
                        Q = 128 if g < 2 else 64
                        ch = slopes[hh] * d
                        wq_, wk_, wv_ = wq[0], wk[0], wv[0]
                        nh += 1
                        with nc.allow_non_contiguous_dma(reason="512B runs"):
                            for w_, c0 in ((wq_, hh * 128), (wk_, W + hh * 128), (wv_, 2 * W + hh * 128)):
                                self.load(w_, w_[:, :, :], w_in, w_in.h.ap()[:, c0:c0 + 128].rearrange("(c p) n -> p c n", p=128), q="pool", cls="w")
                        Lq = NT // d
                        Lk = 128 + Lq
                        tot = Tp + NT
                        def proj(w_, c0tok, ntok, dstd, L_, eng):
                            nonlocal npa
                            s0 = 0
                            while s0 < ntok:
                                nb = min(512, ntok - s0)
                                a_ = pa[npa % 2]; npa += 1
                                for c in range(16):
                                    P.op("pe", lambda e, a_=a_, c=c, w_=w_, s0=s0, nb=nb: e.matmul(
                                        out=a_[:, 0:nb], lhsT=w_[:, c, :], rhs=xT[:, c, c0tok + s0:c0tok + s0 + nb], start=(c == 0), stop=(c == 15)),
                                        reads=[(w_, None), (xT, None)], writes=[(a_, None)], inc=(c == 15))
                                P.op("act", lambda e, a_=a_, s0=s0, nb=nb: e.activation(out=nat[:, s0:s0 + nb], in_=a_[:, 0:nb], func=AF.Copy),
                                     reads=[(a_, None)], writes=[(nat, s0 // 512)])
                                s0 += nb
                            src = nat.h.ap()[:, 0:ntok].rearrange("p (l r) -> p r l", r=d)
                            dst = dstd.h.ap()[:, 0:ntok].rearrange("p (r l) -> p r l", r=d)
                            P.op(eng, lambda e, src=src, dst=dst: e.tensor_copy(out=dst, in_=src), reads=[(nat, None)], writes=[(dstd, None)])
                        proj(wq_, 2048, NT, qTd, Lq, "dve")
                        proj(wk_, 2048 - Tp, tot, kTd, Lk, "pool")
                        proj(wv_, 2048 - Tp, tot, vTd, Lk, "dve")
                        nbk = (Lk + 127) // 128
                        blocks = [(r, b, r * Lk + b * 128, min(128, Lk - b * 128)) for r in range(d) for b in range(nbk)]
                        for b0 in range(0, len(blocks), 8):
                            grp = blocks[b0:b0 + 8]
                            for gi, (r, b, p0, cnt) in enumerate(grp):
                                P.op("pe", lambda e, gi=gi, p0=p0, cnt=cnt: e.transpose(out=pT[0:cnt, gi, :], in_=vTd[:, p0:p0 + cnt], identity=C["ident_b"][:, :]),
                                     reads=[(vTd, None), (C["ident_b"], None)], writes=[(pT, gi)])
                            ng = len(grp)
                            P.op("act", lambda e, b0=b0, ng=ng: e.activation(out=Vt[:, b0:b0 + ng, :], in_=pT[:, 0:ng, :], func=AF.Copy),
                                 reads=[(pT, None)], writes=[(Vt, ("v", b0))], accum="v%d" % hh)
                        nto = Lq // Q
                        for r in range(d if stop > 1 else 0):
                            for n in range(nto):
                                z_, p_ = zz[nq % 2], pp[nq % 2]
                                s_, o_ = pss[nq % 2], poo[nq % 2]
                                nq += 1
                                q0 = r + d * n * Q
                                qd0 = r * Lq + n * Q
                                kp0 = r * Lk + n * Q
                                kc0 = r * Lk + 128 + n * Q
                                vprev = r * nbk + (n * Q) // 128
                                vcur = r * nbk + (128 + n * Q) // 128
                                tbl = (2 if g < 2 else 3) if n == 0 else 0
                                P.op("pe", lambda e, s_=s_, kp0=kp0, qd0=qd0, Q=Q: e.matmul(
                                    out=s_[:, 0:Q], lhsT=kTd[:, kp0:kp0 + 128], rhs=qTd[:, qd0:qd0 + Q], start=True, stop=True),
                                    reads=[(kTd, None), (qTd, None)], writes=[(s_, "p")])
                                P.op("pe", lambda e, s_=s_, kc0=kc0, qd0=qd0, Q=Q: e.matmul(
                                    out=s_[0:Q, 128:128 + Q], lhsT=kTd[:, kc0:kc0 + Q], rhs=qTd[:, qd0:qd0 + Q], start=True, stop=True),
                                    reads=[(kTd, None), (qTd, None)], writes=[(s_, "c")])
                                P.op("dve", lambda e, s_=s_, z_=z_, tbl=tbl, Q=Q, ch=ch: e.scalar_tensor_tensor(
                                    out=z_[:, 0, 0:Q], in0=dtab[:, tbl, 0:Q], scalar=-ch / SCALE, in1=s_[:, 0:Q], op0=ALU.mult, op1=ALU.add),
                                    reads=[(dtab, None), (s_, "p")], writes=[(z_, "p")])
                                P.op("dve", lambda e, s_=s_, z_=z_, Q=Q, ch=ch: e.scalar_tensor_tensor(
                                    out=z_[0:Q, 1, 0:Q], in0=dtab[0:Q, 1, 0:Q], scalar=-ch / SCALE, in1=s_[0:Q, 128:128 + Q], op0=ALU.mult, op1=ALU.add),
                                    reads=[(dtab, None), (s_, "c")], writes=[(z_, "c")])
                                P.op("act", lambda e, z_=z_, p_=p_, Q=Q: e.activation(out=p_[:, 0, 0:Q], in_=z_[:, 0, 0:Q], func=AF.Exp, scale=SCALE),
                                     reads=[(z_, "p")], writes=[(p_, "p")])
                                P.op("act", lambda e, z_=z_, p_=p_, Q=Q: e.activation(out=p_[0:Q, 1, 0:Q], in_=z_[0:Q, 1, 0:Q], func=AF.Exp, scale=SCALE),
                                     reads=[(z_, "c")], writes=[(p_, "c")])
                                P.op("pe", lambda e, o_=o_, p_=p_, vprev=vprev, Q=Q: e.matmul(
                                    out=o_[:, 0:Q], lhsT=Vt[:, vprev, :], rhs=p_[:, 0, 0:Q], start=True, stop=False),
                                    reads=[(Vt, None), (p_, "p")], writes=[(o_, "o")], inc=False)
                                P.op("pe", lambda e, o_=o_, p_=p_, vcur=vcur, Q=Q: e.matmul(
                                    out=o_[:, 0:Q], lhsT=Vt[0:Q, vcur, :], rhs=p_[0:Q, 1, 0:Q], start=False, stop=True),
                                    reads=[(Vt, None), (p_, "c")], writes=[(o_, "o")])
                                P.op("pe", lambda e, o_=o_, p_=p_, Q=Q: e.matmul(
                                    out=o_[:, 128:128 + Q], lhsT=C["ones_b"][:, :], rhs=p_[:, 0, 0:Q], start=True, stop=False),
                                    reads=[(C["ones_b"], None), (p_, "p")], writes=[(o_, "s")], inc=False)
                                P.op("pe", lambda e, o_=o_, p_=p_, Q=Q: e.matmul(
                                    out=o_[:, 128:128 + Q], lhsT=C["ones_b"][0:Q, :], rhs=p_[0:Q, 1, 0:Q], start=False, stop=True),
                                    reads=[(C["ones_b"], None), (p_, "c")], writes=[(o_, "s")])
                                P.op("act", lambda e, o_=o_, g=g, q0=q0, d=d, Q=Q: e.activation(out=Ob[:, g, sl(q0, Q, d)], in_=o_[:, 0:Q], func=AF.Copy),
                                     reads=[(o_, "o")], writes=[(Ob, g)], accum="ob%d" % hh)
                                if g == 0:
                                    P.op("dve", lambda e, o_=o_, q0=q0, d=d, Q=Q: e.tensor_copy(out=den[:, sl(q0, Q, d)], in_=o_[:, 128:128 + Q]),
                                         reads=[(o_, "s")], writes=[(den, None)], accum="den%d" % hh)
                                else:
                                    P.op("dve", lambda e, o_=o_, q0=q0, d=d, Q=Q: e.tensor_tensor(out=den[:, sl(q0, Q, d)], in0=den[:, sl(q0, Q, d)],
                                                                                                in1=o_[:, 128:128 + Q], op=ALU.add),
                                         reads=[(o_, "s"), (den, None)], writes=[(den, None)], accum="den%d" % hh)
                    if stop > 1:
                        P.op("dve", lambda e: e.reciprocal(out=rd[:, :], in_=den[:, :]), reads=[(den, None)], writes=[(rd, None)])
                    for g in range(3 if stop > 1 else 0):
                        eng = "dve" if g != 1 else "pool"
                        P.op(eng, lambda e, g=g, j=j: e.tensor_tensor(out=catT[:, 4 * g + j, :], in0=Ob[:, g, :], in1=rd[:, :], op=ALU.mult),
                             reads=[(Ob, g), (rd, None)], writes=[(catT, ("s", 4 * g + j))])
                if stop > 2:
                    self.mem_attn(st, w_in, 3 * W, xT, 2048, memK, memV, catT, wq, qTd, pa, pss, poo, exm, rd)
                if "cat" in self.cfg.get("debug", ()):
                    self.debug_out("dbg_cat", catT, [128, 16, NT], BF16)
                P.barrier()
            self.mix_out(catT, I["w_mix"], I["x_own"], I["ln1_g"], I["ln1_b"], self.h1o)


    def mixer_ret(self):
        nc, P, I, C = self.nc, self.P, self.I, self.C
        I["h_prev"] = self.inp("h_prev", [3 * NT, D])
        I["memb"] = self.inp("memb", [256, D])
        I["w_in"] = self.inp("w_in", [D, 4 * W + MW])
        I["w_mem"] = self.inp("w_mem", [D, 2 * MW])
        I["w_mix"] = self.inp("w_mix", [D, D])
        I["ln1_g"] = self.inp("ln1_g", [1, D])
        I["ln1_b"] = self.inp("ln1_b", [1, D])
        I["gn_g"] = self.inp("gn_g", [128, 12])
        I["decT"] = self.inp("decT", [128, 12, 128])
        I["qdtab"] = self.inp("qdtab", [128, 12, 128])
        I["kdtab"] = self.inp("kdtab", [128, 12 * 128])
        w_in = I["w_in"]
        gam = [1.0 - 2.0 ** (-(5.0 + h)) for h in range(12)]
        cdec = [g_ ** 128 for g_ in gam]
        NCH = 32
        with ExitStack() as outer:
            memK, memV = self.mem_kv(outer, I["w_mem"], I["memb"])
            Vown = self.sb(outer, "Vown", [128, 8, W], BF16)
            Sb = self.sb(outer, "Sb", [128, 8, 12, 128], BF16)
            with ExitStack() as st:
                wk = self.sb(st, "wk_all", [128, 16, W], BF16)
                wv = self.sb(st, "wv_all", [128, 16, W], BF16)
                hT = self.sb(st, "hTb", [128, 16, 512], BF16)
                xs = [self.sb(st, "xs", [128, D], BF16) for _ in range(2)]
                kdt = self.sb(st, "kdt", [128, W], F32)
                Kd = [self.sb(st, "Kd", [128, W], BF16) for _ in range(2)]
                Vb = [self.sb(st, "Vb", [128, W], BF16) for _ in range(2)]
                S = self.sb(st, "S", [128, 12, 128], F32)
                pT = self.ps(st, "pT", [128, 8, 128], BF16)
                pj = [self.ps(st, "pj", [128, 512], F32) for _ in range(4)]
                pkv = [self.ps(st, "pkv", [128, 4, 128], F32) for _ in range(2)]
                for n in range(3):
                    with nc.allow_non_contiguous_dma(reason="2KB runs"):
                        self.load(wk, wk[:, :, n * 512:(n + 1) * 512], w_in, w_in.h.ap()[:, W + n * 512:W + (n + 1) * 512].rearrange("(c p) n -> p c n", p=128),
                                  q="pool", cls="w", dkey=n)
                        self.load(wv, wv[:, :, n * 512:(n + 1) * 512], w_in, w_in.h.ap()[:, 2 * W + n * 512:2 * W + (n + 1) * 512].rearrange("(c p) n -> p c n", p=128),
                                  q="pool", cls="w", dkey=n)
                self.load(kdt, kdt[:, :], I["kdtab"], I["kdtab"][:, :])
                P.op("dve", lambda e: e.memset(S[:, :, :], 0.0), writes=[(S, None)])
                npj = 0
                nkv = 0
                for blk in range(8):
                    if blk < 6:
                        src = Tile(I["h_prev"].h.ap()[blk * 512:(blk + 1) * 512, :], "hp")
                        src.deps = I["h_prev"].deps
                    else:
                        src = Tile(I["x_own"].h.ap()[(blk - 6) * 512:(blk - 5) * 512, :], "ho")
                        src.deps = I["x_own"].deps
                    self.stage_xT(st, src, 4, hT, 0, xs, pT)
                    for ci in range(4):
                        n = blk * 4 + ci
                        kd_, vb_ = Kd[n % 2], Vb[n % 2]
                        for which, w_, dst in (("k", wk, kd_), ("v", wv, vb_)):
                            for gq in range(3):
                                a_ = pj[npj % 4]; npj += 1
                                for c in range(16):
                                    P.op("pe", lambda e, a_=a_, c=c, w_=w_, gq=gq, ci=ci: e.matmul(
                                        out=a_[:, :], lhsT=hT[:, c, ci * 128:(ci + 1) * 128], rhs=w_[:, c, gq * 512:(gq + 1) * 512], start=(c == 0), stop=(c == 15)),
                                        reads=[(hT, None), (w_, gq)], writes=[(a_, None)], inc=(c == 15))
                                if which == "k":
                                    P.op("dve", lambda e, a_=a_, dst=dst, gq=gq: e.tensor_tensor(out=dst[:, gq * 512:(gq + 1) * 512], in0=a_[:, :],
                                                                                                 in1=kdt[:, gq * 512:(gq + 1) * 512], op=ALU.mult),
                                         reads=[(a_, None), (kdt, None)], writes=[(dst, gq)])
                                else:
                                    P.op("act", lambda e, a_=a_, dst=dst, gq=gq: e.activation(out=dst[:, gq * 512:(gq + 1) * 512], in_=a_[:, :], func=AF.Copy),
                                         reads=[(a_, None)], writes=[(dst, gq)])
                                    if n >= 24:
                                        P.op("pool", lambda e, dst=dst, gq=gq, n=n: e.tensor_copy(out=Vown[:, n - 24, gq * 512:(gq + 1) * 512], in_=dst[:, gq * 512:(gq + 1) * 512]),
                                             reads=[(dst, gq)], writes=[(Vown, (n - 24, gq))])
                        if n >= 24:
                            P.op("act", lambda e, n=n: e.activation(out=Sb[:, n - 24, :, :], in_=S[:, :, :], func=AF.Copy),
                                 reads=[(S, None)], writes=[(Sb, n - 24)])
                        if n == NCH - 1:
                            break
                        for hg in range(3):
                            kv_ = pkv[nkv % 2]; nkv += 1
                            for hh in range(4):
                                h = hg * 4 + hh
                                P.op("pe", lambda e, kv_=kv_, hh=hh, h=h, kd_=kd_, vb_=vb_: e.matmul(
                                    out=kv_[:, hh, :], lhsT=kd_[:, h * 128:(h + 1) * 128], rhs=vb_[:, h * 128:(h + 1) * 128], start=True, stop=True),
                                    reads=[(kd_, None), (vb_, None)], writes=[(kv_, None)])
                            for hh in range(4):
                                h = hg * 4 + hh
                                P.op("dve", lambda e, kv_=kv_, hh=hh, h=h: e.scalar_tensor_tensor(
                                    out=S[:, h, :], in0=S[:, h, :], scalar=cdec[h], in1=kv_[:, hh, :], op0=ALU.mult, op1=ALU.add),
                                    reads=[(kv_, None), (S, h)], writes=[(S, h)])
                P.barrier()
            catT = self.sb(outer, "catT", [128, 16, NT], BF16)
            with ExitStack() as st:
                hT = self.sb(st, "hTo", [128, 16, NT], BF16)
                xs = [self.sb(st, "xs", [128, D], BF16) for _ in range(2)]
                wq = [self.sb(st, "wq", [128, 16, 128], BF16) for _ in range(2)]
                wkf2 = [self.sb(st, "wkf", [128, 16, 128], BF16) for _ in range(2)]
                wg2 = [self.sb(st, "wg", [128, 16, 128], BF16) for _ in range(2)]
                qT = self.sb(st, "qT", [128, NT], BF16)
                kT = self.sb(st, "kT", [128, NT], BF16)
                gT = self.sb(st, "gT", [128, NT], F32)
                sgm = self.sb(st, "sgm", [128, NT], F32)
                qTd = self.sb(st, "qTd", [128, NT], BF16)
                decT = self.sb(st, "decT", [128, 12, 128], F32)
                qdt = self.sb(st, "qdt", [128, 12, 128], F32)
                gng = self.sb(st, "gng", [128, 12], F32)
                onesd = self.sb(st, "onesd", [128, 128], F32)
                r32 = self.sb(st, "r32", [128, NT], F32)
                r2 = self.sb(st, "r2", [128, NT], F32)
                t1 = self.sb(st, "t1", [128, NT], F32)
                t2 = self.sb(st, "t2", [128, NT], F32)
                pp = [self.sb(st, "pp", [128, 128], BF16) for _ in range(2)]
                exm = self.sb(st, "exm", [128, 2, 512], BF16)
                rd = self.sb(st, "rd", [128, NT], F32)
                pT = self.ps(st, "pT", [128, 8, 128], BF16)
                pa = [self.ps(st, "pa", [128, 512], F32) for _ in range(2)]
                pss = [self.ps(st, "pss", [128, 512], F32) for _ in range(2)]
                poo = [self.ps(st, "poo", [128, 512], F32) for _ in range(2)]
                self.load(decT, decT[:, :, :], I["decT"], I["decT"][:, :, :])
                self.load(qdt, qdt[:, :, :], I["qdtab"], I["qdtab"][:, :, :])
                self.load(gng, gng[:, :], I["gn_g"], I["gn_g"][:, :])
                P.op("dve", lambda e: e.memset(onesd[:, :], 1.0 / 128.0), writes=[(onesd, None)])
                self.stage_xT(st, I["x_own"], 8, hT, 0, xs, pT)
                npa = 0
                nq = 0
                for h in range(12):
                    wq_h, wkf, wg = wq[h % 2], wkf2[h % 2], wg2[h % 2]
                    with nc.allow_non_contiguous_dma(reason="512B runs"):
                        for w_, c0 in ((wq_h, h * 128), (wkf, W + h * 128), (wg, 3 * W + h * 128)):
                            self.load(w_, w_[:, :, :], w_in, w_in.h.ap()[:, c0:c0 + 128].rearrange("(c p) n -> p c n", p=128), q="pool", cls="w")
                    for w_, dst, eng in ((wq_h, qT, "act"), (wkf, kT, "dve"), (wg, gT, "act")):
                        for hf_ in range(2):
                            a_ = pa[npa % 2]; npa += 1
                            for c in range(16):
                                P.op("pe", lambda e, a_=a_, c=c, w_=w_, hf_=hf_: e.matmul(out=a_[:, :], lhsT=w_[:, c, :], rhs=hT[:, c, hf_ * 512:(hf_ + 1) * 512],
                                                                                         start=(c == 0), stop=(c == 15)),
                                     reads=[(w_, None), (hT, None)], writes=[(a_, None)], inc=(c == 15))
                            if eng == "act":
                                P.op("act", lambda e, a_=a_, dst=dst, hf_=hf_: e.activation(out=dst[:, hf_ * 512:(hf_ + 1) * 512], in_=a_[:, :], func=AF.Copy),
                                     reads=[(a_, None)], writes=[(dst, hf_)])
                            else:
                                P.op("dve", lambda e, a_=a_, dst=dst, hf_=hf_: e.tensor_copy(out=dst[:, hf_ * 512:(hf_ + 1) * 512], in_=a_[:, :]),
                                     reads=[(a_, None)], writes=[(dst, hf_)])
                    P.op("dve", lambda e, h=h: e.tensor_tensor(out=qTd.h.ap().rearrange("p (n q) -> p n q", q=128), in0=qT.h.ap().rearrange("p (n q) -> p n q", q=128),
                                                                in1=qdt[:, h:h + 1, :].to_broadcast([128, 8, 128]), op=ALU.mult),
                         reads=[(qT, None), (qdt, None)], writes=[(qTd, None)])
                    P.op("act", lambda e: e.activation(out=sgm[:, :], in_=gT[:, :], func=AF.Sigmoid), reads=[(gT, None)], writes=[(sgm, None)])
                    P.op("pool", lambda e: e.tensor_tensor(out=sgm[:, :], in0=sgm[:, :], in1=gT[:, :], op=ALU.mult), reads=[(sgm, None), (gT, None)], writes=[(sgm, None)])
                    for n in range(8):
                        s_, o_, p_ = pss[nq % 2], poo[nq % 2], pp[nq % 2]
                        nq += 1
                        P.op("pe", lambda e, s_=s_, n=n: e.matmul(out=s_[:, 0:128], lhsT=kT[:, n * 128:(n + 1) * 128], rhs=qT[:, n * 128:(n + 1) * 128], start=True, stop=True),
                             reads=[(kT, None), (qT, None)], writes=[(s_, None)])
                        P.op("dve", lambda e, s_=s_, p_=p_, h=h: e.tensor_tensor(out=p_[:, :], in0=s_[:, 0:128], in1=decT[:, h, :], op=ALU.mult),
                             reads=[(s_, None), (decT, None)], writes=[(p_, None)])
                        P.op("pe", lambda e, o_=o_, p_=p_, n=n, h=h: e.matmul(out=o_[:, 0:128], lhsT=Vown[:, n, h * 128:(h + 1) * 128], rhs=p_[:, :], start=True, stop=False),
                             reads=[(Vown, None), (p_, None)], writes=[(o_, None)], inc=False)
                        P.op("pe", lambda e, o_=o_, n=n, h=h: e.matmul(out=o_[:, 0:128], lhsT=Sb[:, n, h, :], rhs=qTd[:, n * 128:(n + 1) * 128], start=False, stop=True),
                             reads=[(Sb, None), (qTd, None)], writes=[(o_, None)])
                        P.op("act", lambda e, o_=o_, n=n: e.activation(out=r32[:, n * 128:(n + 1) * 128], in_=o_[:, 0:128], func=AF.Copy),
                             reads=[(o_, None)], writes=[(r32, n)])
                        P.op("act", lambda e, o_=o_, n=n: e.activation(out=r2[:, n * 128:(n + 1) * 128], in_=o_[:, 0:128], func=AF.Square),
                             reads=[(o_, None)], writes=[(r2, n)])
                    for hf_ in range(2):
                        cs = slice(hf_ * 512, (hf_ + 1) * 512)
                        m_, e_ = pa[0], pa[1]
                        P.op("pe", lambda e, m_=m_, cs=cs: e.matmul(out=m_[:, :], lhsT=onesd[:, :], rhs=r32[:, cs], start=True, stop=True),
                             reads=[(onesd, None), (r32, None)], writes=[(m_, None)])
                        P.op("pe", lambda e, e_=e_, cs=cs: e.matmul(out=e_[:, :], lhsT=onesd[:, :], rhs=r2[:, cs], start=True, stop=True),
                             reads=[(onesd, None), (r2, None)], writes=[(e_, None)])
                        P.op("act", lambda e, m_=m_, cs=cs: e.activation(out=t1[:, cs], in_=m_[:, :], func=AF.Square),
                             reads=[(m_, None)], writes=[(t1, hf_)])
                        P.op("dve", lambda e, e_=e_, cs=cs: e.tensor_tensor(out=t1[:, cs], in0=e_[:, :], in1=t1[:, cs], op=ALU.subtract),
                             reads=[(e_, None), (t1, hf_)], writes=[(t1, hf_)])
                        P.op("dve", lambda e, cs=cs: e.tensor_scalar(out=t1[:, cs], in0=t1[:, cs], scalar1=LN_EPS, scalar2=None, op0=ALU.add),
                             reads=[(t1, hf_)], writes=[(t1, hf_)])
                        P.op("act", lambda e, cs=cs: e.activation(out=t1[:, cs], in_=t1[:, cs], func=AF.Sqrt), reads=[(t1, hf_)], writes=[(t1, hf_)])
                        P.op("dve", lambda e, cs=cs: e.reciprocal(out=t1[:, cs], in_=t1[:, cs]), reads=[(t1, hf_)], writes=[(t1, hf_)])
                        P.op("dve", lambda e, m_=m_, cs=cs: e.tensor_tensor(out=t2[:, cs], in0=r32[:, cs], in1=m_[:, :], op=ALU.subtract),
                             reads=[(m_, None), (r32, None)], writes=[(t2, hf_)])
                        P.op("pool", lambda e, cs=cs: e.tensor_tensor(out=t2[:, cs], in0=t2[:, cs], in1=t1[:, cs], op=ALU.mult),
                             reads=[(t1, hf_), (t2, hf_)], writes=[(t2, hf_)])
                        P.op("dve", lambda e, cs=cs, h=h: e.scalar_tensor_tensor(out=catT[:, h, cs], in0=t2[:, cs], scalar=gng[:, h:h + 1], in1=sgm[:, cs],
                                                                                 op0=ALU.mult, op1=ALU.mult),
                             reads=[(t2, hf_), (gng, None), (sgm, None)], writes=[(catT, ("s", h, hf_))])
                self.mem_attn(st, w_in, 4 * W, hT, 0, memK, memV, catT, wq, qT, pa, pss, poo, exm, rd)
                if "cat" in self.cfg.get("debug", ()):
                    self.debug_out("dbg_cat", catT, [128, 16, NT], BF16)
                P.barrier()
            self.mix_out(catT, I["w_mix"], I["x_own"], I["ln1_g"], I["ln1_b"], self.h1o)


def host_consts(c, nel):
    out = {}
    out["ident"] = np.eye(128, dtype=np.float32)
    out["tri"] = (np.arange(128)[:, None] < np.arange(128)[None, :]).astype(np.float32)
    p = np.arange(128)
    ps = np.zeros((128, nel, NE), np.float32)
    for j in range(nel):
        ps[:, j, c * nel + j] = 1.0
    out["psel"] = ps
    pi = np.zeros((128, NSL, 2), np.float32)
    pi[:, :, 0] = NTOK + p[:, None]
    out["padinit"] = pi
    out["tokidf"] = (np.arange(64)[None, :] * 128 + p[:, None]).astype(np.float32)
    return out


def host_attn_consts(c):
    q = c % 4
    BIGD = 1.0e6
    k = np.arange(128)[:, None].astype(np.float32)
    qq = np.arange(128)[None, :].astype(np.float32)
    t0 = np.where(k >= qq, 128.0 + qq - k, BIGD).astype(np.float32)
    t1 = np.where(k <= qq, qq - k, BIGD).astype(np.float32)
    t2 = t0.copy() if q > 0 else np.full_like(t0, BIGD)
    if q == 0:
        t3 = np.full_like(t0, BIGD)
    elif q == 1:
        t3 = t0.copy()
        t3[:64, :] = BIGD
    else:
        t3 = t0.copy()
    return np.ascontiguousarray(np.stack([t0, t1, t2, t3], 1))


def host_ret_consts(c):
    gam = np.array([1.0 - 2.0 ** (-(5.0 + h)) for h in range(12)], np.float64)
    k = np.arange(128)[:, None, None].astype(np.float64)
    q = np.arange(128)[None, None, :].astype(np.float64)
    g3 = gam[None, :, None]
    decT = np.where(q >= k, SCALE * g3 ** np.maximum(q - k, 0.0), 0.0).astype(np.float32)
    qd = np.broadcast_to((g3 ** (q + 1.0)), (128, 12, 128)).astype(np.float32)
    kd = (SCALE * gam[None, :] ** (127.0 - np.arange(128)[:, None])).astype(np.float32)
    kdtab = np.ascontiguousarray(np.repeat(kd[:, :, None], 128, axis=2).reshape(128, 12 * 128))
    return {"decT": np.ascontiguousarray(decT), "qdtab": np.ascontiguousarray(qd), "kdtab": kdtab}


def host_moe_weights(inputs, L, c, nel):
    es = [c * nel + j for j in range(nel)]
    wi = np.asarray(inputs["moe_w_in"][L])[es]
    wi = wi.reshape(nel, 16, 128, 2, 8, 256)
    wi = np.ascontiguousarray(wi.transpose(0, 4, 2, 1, 3, 5)).reshape(nel, 8, 128, 16, 512)
    wo = np.asarray(inputs["moe_w_out"][L])[es]
    wo = wo.reshape(nel, 16, 128, 4, 512)
    wo = np.ascontiguousarray(wo.transpose(0, 3, 2, 1, 4))
    bi = np.asarray(inputs["moe_b_in"][L])[es].reshape(nel, 32, 128)
    bi = np.ascontiguousarray(bi.transpose(2, 0, 1))
    bo = np.ascontiguousarray(np.asarray(inputs["moe_b_out"][L])[es]).reshape(1, nel, D)
    return {"moe_win": wi, "moe_wout": wo, "moe_bin": bi, "moe_bout": bo}


def host_A_inputs(inputs, L, c, h_own, h_all_batch):
    b, q = c // 4, c % 4
    m = {"x_own": h_own}
    m["router_w"] = np.asarray(inputs["router_w"][L])
    m["router_b"] = np.asarray(inputs["router_b"][L]).reshape(1, NE)
    m["memb"] = np.asarray(inputs["mem"][b])
    m["w_mem"] = np.asarray(inputs["w_mem_kv"][L])
    m["w_mix"] = np.asarray(inputs["w_mix_out"][L])
    m["ln1_g"] = np.asarray(inputs["ln_mix_g"][L]).reshape(1, D)
    m["ln1_b"] = np.asarray(inputs["ln_mix_b"][L]).reshape(1, D)
    if L == 0:
        xb = np.asarray(inputs["x"][b])
        xp = np.zeros((2048, D), np.float32)
        n = min(q * NT, 2048)
        if n:
            xp[2048 - n:] = xb[q * NT - n:q * NT]
        m["x_prev"] = xp
        m["w_in"] = np.asarray(inputs["w_in_dil"][0])
        m["attn_c"] = host_attn_consts(c)
    else:
        hp = np.zeros((3 * NT, D), np.float32)
        if q:
            hp[(3 - q) * NT:] = h_all_batch[:q * NT]
        m["h_prev"] = hp
        m["w_in"] = np.asarray(inputs["w_in_ret"][0])
        m["gn_g"] = np.ascontiguousarray(np.asarray(inputs["ret_gn_g"][0]).reshape(12, 128).T)
        m.update(host_ret_consts(c))
    return m


TRACE = {"on": False, "last": None}


def launch(cfg, per_core):
    bld = Builder(cfg)
    nc = bld.build()
    in_maps = []
    for c in range(NCORES):
        m = per_core(c)
        hc = host_consts(c, cfg.get("nel", NEL))
        for name in bld.ins:
            if name not in m and name in hc:
                m[name] = hc[name]
        mm = {}
        for name, (shape, dt) in bld.ins.items():
            if name not in m:
                raise KeyError(name)
            a = np.ascontiguousarray(m[name])
            assert tuple(a.shape) == tuple(shape), (name, a.shape, shape)
            mm[name] = a
        in_maps.append(mm)
    if TRACE["on"]:
        res = run_bass_kernel_spmd(nc, in_maps, core_ids=list(range(NCORES)), trace=True)
        TRACE["last"] = res
    else:
        res = run_bass_kernel_spmd(nc, in_maps, core_ids=list(range(NCORES)))
    return res.results


def moe_layer(inputs, L, h1_own, h1b_own, g_own, nel=NEL):
    import ml_dtypes
    h1all = np.concatenate(list(h1b_own) + [np.zeros((128, D), ml_dtypes.bfloat16)], 0)
    gall = np.concatenate(list(g_own), 0)
    resB = launch({"kind": "B", "L": L, "nel": nel},
                  lambda c: dict(h1all=h1all, gall=gall, **host_moe_weights(inputs, L, c, nel)))
    yp = [resB[c]["ypart"].reshape(NROW, D) for c in range(NCORES)]
    lg = np.asarray(inputs["ln_ffn_g"][L]).reshape(1, D)
    lb = np.asarray(inputs["ln_ffn_b"][L]).reshape(1, D)
    resC = launch({"kind": "C", "L": L},
                  lambda c: dict(parts=np.stack([yp[r][c * NT:(c + 1) * NT] for r in range(NCORES)], 0), h1=h1_own[c], ln_g=lg, ln_b=lb))
    return [resC[c]["h2_out"] for c in range(NCORES)]


def kernel(**inputs):
    x = np.asarray(inputs["x"])
    h_own = [np.ascontiguousarray(x[c // 4, (c % 4) * NT:(c % 4 + 1) * NT]) for c in range(NCORES)]
    h_batch = [x[0], x[1]]
    for L in (0, 1):
        resA = launch({"kind": "A", "L": L, "mixer": True},
                      lambda c: host_A_inputs(inputs, L, c, h_own[c], h_batch[c // 4]))
        h_own = moe_layer(inputs, L, [resA[c]["h1_out"] for c in range(NCORES)], [resA[c]["h1b_out"] for c in range(NCORES)],
                          [resA[c]["g_out"] for c in range(NCORES)])
        h_batch = [np.concatenate(h_own[0:4], 0), np.concatenate(h_own[4:8], 0)]
    return np.stack(h_batch, 0).astype(np.float32)
```

```python
import math
from contextlib import ExitStack

import numpy as np

import concourse.bass as bass
import concourse.mybir as mybir
from concourse.bass_utils import run_bass_kernel_spmd

F32 = mybir.dt.float32
BF16 = mybir.dt.bfloat16
I32 = mybir.dt.int32
AF = mybir.ActivationFunctionType
ALU = mybir.AluOpType
AX = mybir.AxisListType

NCORES = 8
D = 2048
NT = 1024
NTI = NT // 128
NTOK = 8192
HD = 128
W = 1536
MW = 512
NE = 32
NEL = 4
CAP = 1280
NSL = CAP // 128
NROW = NTOK + 128
ALPHA = (2 * 2) ** 0.25
LN_EPS = 1e-5
BIG = 1.0e6
SCALE = HD ** -0.5
DIL = (1, 4, 16)
TPREV = (128, 512, 2048)


def sl(start, cnt, step):
    return slice(start, start + (cnt - 1) * step + 1, step)


def _alibi_slopes(n):
    def pow2(m):
        start = 2.0 ** (-8.0 / m)
        return [start ** (i + 1) for i in range(m)]
    if math.log2(n).is_integer():
        s = pow2(n)
    else:
        c = 2 ** math.floor(math.log2(n))
        s = pow2(c) + pow2(2 * c)[0::2][: n - c]
    return sorted(s, reverse=True)


class Dep:
    __slots__ = ("w", "r", "wg")

    def __init__(self):
        self.w = {}
        self.r = {}
        self.wg = None


class Tile:
    def __init__(self, h, name, psum=False):
        self.h = h
        self.name = name
        self.psum = psum
        self.deps = {None: Dep()}

    def __getitem__(self, idx):
        return self.h[idx]


class Prog:
    def __init__(self, nc, es):
        self.nc = nc
        self.es = es
        self.engs = {"pe": nc.tensor, "act": nc.scalar, "dve": nc.vector, "pool": nc.gpsimd, "sp": nc.sync}
        self.sem = {k: es.enter_context(nc.semaphore("s_" + k)) for k in ("pe", "act", "dve", "pool")}
        self.cnt = {k: 0 for k in self.sem}
        self.pending = {k: False for k in self.sem}
        self.seen = {k: {} for k in self.engs}
        self.slots = {}
        self.rr = {}
        self.cc_sem = es.enter_context(nc.semaphore("s_cc"))
        self.cc_cnt = 0
        self.nwait = 0

    def dma_class(self, name, n):
        self.slots[name] = [[self.es.enter_context(self.nc.semaphore("d_%s%d" % (name, i))), 0, "d_%s%d" % (name, i)]
                            for i in range(n)]
        self.rr[name] = 0

    def _wait(self, eng, evs):
        seen = self.seen[eng]
        for key, (sem, val) in evs.items():
            if eng == "pe" and key == "pe":
                continue
            if seen.get(key, 0) >= val:
                continue
            self.engs[eng].wait_ge(sem, val)
            seen[key] = val
            self.nwait += 1

    @staticmethod
    def _merge(dst, src):
        for k, (s, v) in src.items():
            if k not in dst or dst[k][1] < v:
                dst[k] = (s, v)

    def _collect(self, reads, writes, accum):
        evs = {}
        for t, key in reads:
            if key is None:
                for d in t.deps.values():
                    self._merge(evs, d.w)
            else:
                self._merge(evs, t.deps[None].w)
                if key in t.deps:
                    self._merge(evs, t.deps[key].w)
        for t, key in writes:
            if key is None:
                for d in t.deps.values():
                    self._merge(evs, d.r)
                    if not (accum and d.wg == accum):
                        self._merge(evs, d.w)
            else:
                self._merge(evs, t.deps[None].w)
                self._merge(evs, t.deps[None].r)
                if key in t.deps:
                    d = t.deps[key]
                    self._merge(evs, d.r)
                    if not (accum and d.wg == accum):
                        self._merge(evs, d.w)
        return evs

    def _register(self, ev, reads, writes, accum):
        k, s, v = ev
        for t, key in reads:
            d = t.deps.setdefault(key, Dep())
            self._merge(d.r, {k: (s, v)})
        for t, key in writes:
            if key is None and not accum:
                t.deps = {None: Dep()}
                t.deps[None].w = {k: (s, v)}
            else:
                if key is None:
                    for kk in [kk for kk in t.deps if kk is not None]:
                        del t.deps[kk]
                d = t.deps.setdefault(key, Dep())
                if accum:
                    if d.wg != accum:
                        d.w = {}
                        d.r = {}
                        d.wg = accum
                    self._merge(d.w, {k: (s, v)})
                else:
                    d.w = {k: (s, v)}
                    d.r = {}
                    d.wg = None

    @staticmethod
    def _norm(reads, writes):
        r2 = [(t, k) for t, k in reads if not t.psum]
        w2 = [((t, None) if t.psum else (t, k)) for t, k in writes] + [(t, None) for t, k in reads if t.psum]
        seen, w3 = set(), []
        for t, k in w2:
            if (id(t), k) not in seen:
                seen.add((id(t), k))
                w3.append((t, k))
        return r2, w3

    def op(self, eng, fn, reads=(), writes=(), inc=True, accum=False):
        reads, writes = self._norm(reads, writes)
        self._wait(eng, self._collect(reads, writes, accum))
        ins = fn(self.engs[eng])
        if inc:
            self.cnt[eng] += 1
            ins.then_inc(self.sem[eng], 1)
            ev = (eng, self.sem[eng], self.cnt[eng])
            self.pending[eng] = False
        else:
            ev = (eng, self.sem[eng], self.cnt[eng] + 1)
            self.pending[eng] = True
        self._register(ev, reads, writes, accum)
        return ins

    def dma(self, q, cls, fn, reads=(), writes=(), accum=False):
        slot = self.slots[cls][self.rr[cls] % len(self.slots[cls])]
        self.rr[cls] += 1
        evs = self._collect(reads, writes, accum)
        if slot[1] > 0:
            self._merge(evs, {slot[2]: (slot[0], slot[1])})
        self._wait(q, evs)
        ins = fn(self.engs[q])
        slot[1] += 16
        ins.then_inc(slot[0], 16)
        self._register((slot[2], slot[0], slot[1]), reads, writes, accum)
        return ins

    def collective(self, fn, reads=(), writes=()):
        self._wait("pool", self._collect(reads, writes, False))
        ins = fn(self.engs["pool"])
        self.cc_cnt += 1
        ins.then_inc(self.cc_sem, 1)
        self._register(("cc", self.cc_sem, self.cc_cnt), reads, writes, False)

    def barrier(self):
        evs = {}
        for k in self.sem:
            assert not self.pending[k], k
            if self.cnt[k]:
                evs[k] = (self.sem[k], self.cnt[k])
        for cls in self.slots.values():
            for s in cls:
                if s[1]:
                    evs[s[2]] = (s[0], s[1])
        if self.cc_cnt:
            evs["cc"] = (self.cc_sem, self.cc_cnt)
        for e in self.engs:
            self._wait(e, evs)


class Builder:
    def __init__(self, cfg):
        self.cfg = cfg
        self.nc = bass.Bass("TRN2", target_bir_lowering=False)
        self.es = ExitStack()
        self.P = Prog(self.nc, self.es)
        for name, n in (("ld", 8), ("w", 6), ("g", 4), ("s", 8), ("sc", 8), ("st", 6)):
            self.P.dma_class(name, n)
        self.ins = {}
        self.uid = 0
        self.capreg = self.nc.gpsimd.to_reg(CAP - 1)

    def inp(self, name, shape, dt=F32):
        h = self.nc.dram_tensor(name, list(shape), dt, kind="ExternalInput")
        self.ins[name] = (tuple(shape), dt)
        return Tile(h, name)

    def outp(self, name, shape, dt=F32):
        return Tile(self.nc.dram_tensor(name, list(shape), dt, kind="ExternalOutput"), name)

    def dram(self, name, shape, dt, shared=False):
        if shared:
            h = self.nc.dram_tensor(name, list(shape), dt, addr_space="Shared")
        else:
            h = self.nc.dram_tensor(name, list(shape), dt)
        return Tile(h, name)

    def sb(self, st, name, shape, dt):
        self.uid += 1
        return Tile(st.enter_context(self.nc.sbuf_tensor("%s_%d" % (name, self.uid), list(shape), dt)), name)

    def ps(self, st, name, shape, dt):
        self.uid += 1
        return Tile(st.enter_context(self.nc.psum_tensor("%s_%d" % (name, self.uid), list(shape), dt)), name, psum=True)

    def load(self, dst, dst_ap, src, src_ap, q="sp", cls="ld", dkey=None, skey=None, accum=False):
        self.P.dma(q, cls, lambda e: e.dma_start(out=dst_ap, in_=src_ap), reads=[(src, skey)], writes=[(dst, dkey)], accum=accum)

    def layer_norm_tile(self, st_tiles, r, rkey, g_bc, b_bc, out, out_ap, okey):
        P = self.P
        stats, mv, rstd, xn = st_tiles
        for k in range(4):
            P.op("dve", lambda e, k=k: e.bn_stats(out=stats[:, k, :], in_=r[:, k * 512:(k + 1) * 512]),
                 reads=[(r, rkey)], writes=[(stats, k)])
        P.op("dve", lambda e: e.bn_aggr(out=mv[:, :], in_=stats[:, :, :]), reads=[(stats, None)], writes=[(mv, None)])
        P.op("dve", lambda e: e.tensor_scalar(out=rstd[:, :], in0=mv[:, 1:2], scalar1=LN_EPS, scalar2=None, op0=ALU.add),
             reads=[(mv, None)], writes=[(rstd, None)])
        P.op("act", lambda e: e.activation(out=rstd[:, :], in_=rstd[:, :], func=AF.Sqrt), reads=[(rstd, None)], writes=[(rstd, None)])
        P.op("dve", lambda e: e.reciprocal(out=rstd[:, :], in_=rstd[:, :]), reads=[(rstd, None)], writes=[(rstd, None)])
        P.op("dve", lambda e: e.tensor_scalar(out=xn[:, :], in0=r[:, :], scalar1=mv[:, 0:1], scalar2=rstd[:, 0:1],
                                                 op0=ALU.subtract, op1=ALU.mult),
             reads=[(r, rkey), (mv, None), (rstd, None)], writes=[(xn, None)])
        P.op("dve", lambda e: e.tensor_tensor(out=xn[:, :], in0=xn[:, :], in1=g_bc[:, :], op=ALU.mult),
             reads=[(xn, None), (g_bc, None)], writes=[(xn, None)])
        P.op("dve", lambda e: e.tensor_tensor(out=out_ap, in0=xn[:, :], in1=b_bc[:, :], op=ALU.add),
             reads=[(xn, None), (b_bc, None)], writes=[(out, okey)])

    def consts(self):
        P, I = self.P, self.I
        cs = self.es
        C = {}
        I["ident"] = self.inp("ident", [128, 128])
        C["ident_f"] = self.sb(cs, "ident_f", [128, 128], F32)
        C["ident_b"] = self.sb(cs, "ident_b", [128, 128], BF16)
        C["ones_f"] = self.sb(cs, "ones_f", [128, 128], F32)
        C["ones_b"] = self.sb(cs, "ones_b", [128, 128], BF16)
        self.load(C["ident_f"], C["ident_f"][:, :], I["ident"], I["ident"][:, :])
        P.op("dve", lambda e: e.tensor_copy(out=C["ident_b"][:, :], in_=C["ident_f"][:, :]),
             reads=[(C["ident_f"], None)], writes=[(C["ident_b"], None)])
        P.op("dve", lambda e: e.memset(C["ones_f"][:, :], 1.0), writes=[(C["ones_f"], None)])
        P.op("dve", lambda e: e.memset(C["ones_b"][:, :], 1.0), writes=[(C["ones_b"], None)])
        self.C = C

    def build(self):
        kind = self.cfg["kind"]
        self.I = {}
        self.dbg = {}
        self.consts()
        if kind == "A":
            self.build_A()
        elif kind == "B":
            self.build_B()
        else:
            self.build_C()
        self.P.barrier()
        return self.nc

    def build_A(self):
        nc, P, I, C = self.nc, self.P, self.I, self.C
        L = self.cfg["L"]
        I["x_own"] = self.inp("x_own", [NT, D])
        I["router_w"] = self.inp("router_w", [D, NE])
        I["router_b"] = self.inp("router_b", [1, NE])
        h1o = self.outp("h1_out", [NT, D])
        h1b = self.outp("h1b_out", [NT, D], BF16)
        go = self.outp("g_out", [NT, NE])
        self.h1o = h1o
        if self.cfg.get("mixer", True):
            if L == 0:
                self.mixer_dil()
            else:
                self.mixer_ret()
        else:
            self.P.dma("sp", "st", lambda e: e.dma_start(out=h1o[:, :], in_=I["x_own"][:, :]),
                       reads=[(I["x_own"], None)], writes=[(h1o, None)])
        P.barrier()
        hbuf = h1o
        with ExitStack() as st:
            hf = [self.sb(st, "hf", [128, D], F32) for _ in range(2)]
            hb = [self.sb(st, "hb", [128, D], BF16) for _ in range(2)]
            hT = [self.sb(st, "hT", [128, 16, 128], F32) for _ in range(2)]
            rw = self.sb(st, "rw", [128, 16, NE], F32)
            rb = self.sb(st, "rb", [1, NE], F32)
            lg = self.sb(st, "lg", [128, NTI, NE], F32)
            G = self.sb(st, "G", [128, NTI, NE], F32)
            ex = self.sb(st, "ex", [128, NTI, NE], F32)
            msk = self.sb(st, "msk", [128, NTI, NE], F32)
            m8 = self.sb(st, "m8", [128, NTI, 8], F32)
            nm = self.sb(st, "nm", [128, NTI], F32)
            ssum = self.sb(st, "ssum", [128, NTI], F32)
            ptr = [self.ps(st, "ptr", [128, 4, 128], F32) for _ in range(2)]
            plg = self.ps(st, "plg", [128, NTI, NE], F32)
            with nc.allow_non_contiguous_dma(reason="router weights 128B runs"):
                self.load(rw, rw[:, :, :], I["router_w"], I["router_w"].h.ap().rearrange("(c p) e -> p c e", p=128))
            self.load(rb, rb[:, :], I["router_b"], I["router_b"][:, :])
            for i in range(NTI):
                f, b_, t_ = hf[i % 2], hb[i % 2], hT[i % 2]
                self.load(f, f[:, :], hbuf, hbuf[i * 128:(i + 1) * 128, :])
                P.op("act", lambda e, f=f, b_=b_: e.activation(out=b_[:, :], in_=f[:, :], func=AF.Copy),
                     reads=[(f, None)], writes=[(b_, None)])
                self.P.dma("pool", "st", lambda e, b_=b_, i=i: e.dma_start(out=h1b[i * 128:(i + 1) * 128, :], in_=b_[:, :]),
                           reads=[(b_, None)], writes=[(h1b, i)])
                for cg in range(4):
                    pt = ptr[cg % 2]
                    for k in range(4):
                        c = cg * 4 + k
                        P.op("pe", lambda e, pt=pt, k=k, c=c, f=f: e.transpose(out=pt[:, k, :], in_=f[:, c * 128:(c + 1) * 128],
                                                                              identity=C["ident_f"][:, :]),
                             reads=[(f, None), (C["ident_f"], None)], writes=[(pt, k)])
                    if cg % 2 == 0:
                        P.op("dve", lambda e, pt=pt, t_=t_, cg=cg: e.tensor_copy(out=t_[:, cg * 4:(cg + 1) * 4, :], in_=pt[:, :, :]),
                             reads=[(pt, None)], writes=[(t_, cg)])
                    else:
                        P.op("act", lambda e, pt=pt, t_=t_, cg=cg: e.activation(out=t_[:, cg * 4:(cg + 1) * 4, :], in_=pt[:, :, :], func=AF.Copy),
                             reads=[(pt, None)], writes=[(t_, cg)])
                for c in range(16):
                    P.op("pe", lambda e, t_=t_, c=c, i=i: e.matmul(out=plg[:, i, :], lhsT=t_[:, c, :], rhs=rw[:, c, :],
                                                                   start=(c == 0), stop=False),
                         reads=[(t_, c // 4), (rw, None)], writes=[(plg, i)], inc=False)
                P.op("pe", lambda e, i=i: e.matmul(out=plg[:, i, :], lhsT=C["ones_f"][0:1, :], rhs=rb[0:1, :], start=False, stop=True),
                     reads=[(C["ones_f"], None), (rb, None)], writes=[(plg, i)])
            P.op("act", lambda e: e.activation(out=lg[:, :, :], in_=plg[:, :, :], func=AF.Copy), reads=[(plg, None)], writes=[(lg, None)])
            for i in range(NTI):
                P.op("dve", lambda e, i=i: e.max(out=m8[:, i, :], in_=lg[:, i, :]), reads=[(lg, None)], writes=[(m8, i)])
                P.op("dve", lambda e, i=i: e.tensor_scalar(out=msk[:, i, :], in0=lg[:, i, :], scalar1=m8[:, i, 3:4], scalar2=None,
                                                             op0=ALU.is_ge), reads=[(lg, None), (m8, i)], writes=[(msk, i)])
                P.op("dve", lambda e, i=i: e.tensor_scalar(out=nm[:, i:i + 1], in0=m8[:, i, 0:1], scalar1=-1.0, scalar2=None, op0=ALU.mult),
                     reads=[(m8, i)], writes=[(nm, i)])
                P.op("act", lambda e, i=i: e.activation(out=ex[:, i, :], in_=lg[:, i, :], func=AF.Exp, bias=nm[:, i:i + 1], scale=1.0),
                     reads=[(lg, None), (nm, i)], writes=[(ex, i)])
                P.op("dve", lambda e, i=i: e.tensor_tensor(out=ex[:, i, :], in0=ex[:, i, :], in1=msk[:, i, :], op=ALU.mult),
                     reads=[(ex, i), (msk, i)], writes=[(ex, i)])
                P.op("dve", lambda e, i=i: e.reduce_sum(out=ssum[:, i:i + 1], in_=ex[:, i, :], axis=AX.X),
                     reads=[(ex, i)], writes=[(ssum, i)])
                P.op("dve", lambda e, i=i: e.reciprocal(out=ssum[:, i:i + 1], in_=ssum[:, i:i + 1]),
                     reads=[(ssum, i)], writes=[(ssum, i)])
                P.op("dve", lambda e, i=i: e.tensor_scalar(out=G[:, i, :], in0=ex[:, i, :], scalar1=ssum[:, i:i + 1], scalar2=None, op0=ALU.mult),
                     reads=[(ex, i), (ssum, i)], writes=[(G, i)])
            self.P.dma("sp", "st", lambda e: e.dma_start(out=go.h.ap().rearrange("(i p) e -> p i e", p=128), in_=G[:, :, :]),
                       reads=[(G, None)], writes=[(go, None)])
            P.barrier()

    def build_B(self):
        nc, P, I, C = self.nc, self.P, self.I, self.C
        NL = self.cfg.get("nel", NEL)
        I["h1all"] = self.inp("h1all", [NROW, D], BF16)
        I["gall"] = self.inp("gall", [NTOK, NE])
        I["moe_win"] = self.inp("moe_win", [NL, 8, 128, 16, 512])
        I["moe_wout"] = self.inp("moe_wout", [NL, 4, 128, 16, 512])
        I["moe_bin"] = self.inp("moe_bin", [128, NL, 32])
        I["moe_bout"] = self.inp("moe_bout", [1, NL, D])
        I["psel"] = self.inp("psel", [128, NL, NE])
        I["padinit"] = self.inp("padinit", [128, NSL, 2])
        I["tokidf"] = self.inp("tokidf", [128, 64])
        I["tri"] = self.inp("tri", [128, 128])
        ypart = self.outp("ypart", [NROW * 4, 512])
        idxb = [self.dram("idxb%d" % j, [CAP, 2], F32) for j in range(NL)]
        h1all = I["h1all"]

        NLA = max(NL, 2)
        posi = self.sb(self.es, "posi", [128, NLA, 64], I32)
        zb = self.sb(self.es, "zero_f", [128, 1024], F32)
        P.op("pool", lambda e: e.memset(zb[:, :], 0.0), writes=[(zb, None)])
        vals = self.sb(self.es, "vals", [128, NLA, 64, 2], F32)

        def scatter_ids(j):
            iv = idxb[j]
            for c in range(64):
                P.dma("pool", "sc", lambda e, j=j, c=c, iv=iv: e.indirect_dma_start(
                    out=iv[:, :], out_offset=bass.IndirectOffsetOnAxis(ap=posi[:, j, c:c + 1], axis=0),
                    in_=vals[:, j, c, :], in_offset=None, bounds_check=self.capreg, oob_is_err=False),
                    reads=[(vals, None), (posi, None)], writes=[(iv, None)], accum="sc")

        with ExitStack() as st:
            Gl = self.sb(st, "Gl", [128, 64, NE], F32)
            tmp = self.sb(st, "tmp", [128, 64, NE], F32)
            psel = self.sb(st, "psel", [128, NL, NE], F32)
            tokf = self.sb(st, "tokf", [128, 64], F32)
            tri = self.sb(st, "tri", [128, 128], F32)
            Gmy = self.sb(st, "Gmy", [128, NLA, 64], F32)
            mk = self.sb(st, "mk", [128, NLA, 64], F32)
            sA = self.sb(st, "sA", [128, NLA, 64], F32)
            sB = self.sb(st, "sB", [128, NLA, 64], F32)
            pos = self.sb(st, "pos", [128, NLA, 64], F32)
            padi = self.sb(st, "padi", [128, NSL, 2], F32)
            ppw = self.ps(st, "ppw", [128, NLA * 64], F32)
            ptot = self.ps(st, "ptot", [128, NLA * 64], F32)
            with nc.allow_non_contiguous_dma(reason="gate matrix 128B runs"):
                self.load(Gl, Gl[:, :, :], I["gall"], I["gall"].h.ap().rearrange("(c p) e -> p c e", p=128))
            self.load(psel, psel[:, :, :], I["psel"], I["psel"][:, :, :])
            self.load(tokf, tokf[:, :], I["tokidf"], I["tokidf"][:, :])
            self.load(tri, tri[:, :], I["tri"], I["tri"][:, :])
            self.load(padi, padi[:, :, :], I["padinit"], I["padinit"][:, :, :])

            for j in range(NL):
                iv = idxb[j]
                with nc.allow_non_contiguous_dma(reason="8B rows"):
                    self.P.dma("sp", "st", lambda e, iv=iv: e.dma_start(out=iv.h.ap().rearrange("(i p) t -> p i t", p=128), in_=padi[:, :, :]),
                               reads=[(padi, None)], writes=[(iv, None)])
                P.op("dve", lambda e, j=j: e.tensor_tensor(out=tmp[:, :, :], in0=Gl[:, :, :],
                                                             in1=psel[:, j:j + 1, :].to_broadcast([128, 64, NE]), op=ALU.mult),
                     reads=[(Gl, None), (psel, None)], writes=[(tmp, None)])
                P.op("dve", lambda e, j=j: e.reduce_sum(out=Gmy[:, j, :], in_=tmp[:, :, :], axis=AX.X),
                     reads=[(tmp, None)], writes=[(Gmy, j)])
            if NLA > NL:
                P.op("dve", lambda e: e.memset(Gmy[:, NL:, :], 0.0), writes=[(Gmy, "pad")])
            P.op("dve", lambda e: e.tensor_single_scalar(out=mk[:, :, :], in_=Gmy[:, :, :], scalar=0.0, op=ALU.is_gt),
                 reads=[(Gmy, None)], writes=[(mk, None)])
            mk2 = mk.h.ap().rearrange("p j c -> p (j c)")
            P.op("pe", lambda e: e.matmul(out=ppw[:, :], lhsT=tri[:, :], rhs=mk2, start=True, stop=True),
                 reads=[(tri, None), (mk, None)], writes=[(ppw, None)])
            P.op("pe", lambda e: e.matmul(out=ptot[:, :], lhsT=C["ones_f"][:, :], rhs=mk2, start=True, stop=True),
                 reads=[(C["ones_f"], None), (mk, None)], writes=[(ptot, None)])
            P.op("act", lambda e: e.activation(out=sA.h.ap().rearrange("p j c -> p (j c)"), in_=ptot[:, :], func=AF.Copy),
                 reads=[(ptot, None)], writes=[(sA, None)])
            a, b_ = sA, sB
            s = 1
            while s < 64:
                P.op("dve", lambda e, a=a, b_=b_, s=s: e.tensor_tensor(out=b_[:, :, s:], in0=a[:, :, s:], in1=a[:, :, :64 - s], op=ALU.add),
                     reads=[(a, None)], writes=[(b_, "hi")])
                P.op("pool", lambda e, a=a, b_=b_, s=s: e.tensor_copy(out=b_[:, :, :s], in_=a[:, :, :s]),
                     reads=[(a, None)], writes=[(b_, "lo")])
                a, b_ = b_, a
                s *= 2
            flat = lambda t: t.h.ap().rearrange("p j c -> p (j c)")
            P.op("dve", lambda e, a=a: e.tensor_tensor(out=flat(pos), in0=flat(a), in1=ptot[:, :], op=ALU.subtract),
                 reads=[(a, None), (ptot, None)], writes=[(pos, None)])
            P.op("dve", lambda e: e.tensor_tensor(out=flat(pos), in0=flat(pos), in1=ppw[:, :], op=ALU.add),
                 reads=[(pos, None), (ppw, None)], writes=[(pos, None)])
            P.op("dve", lambda e: e.tensor_scalar(out=mk[:, :, :], in0=mk[:, :, :], scalar1=-BIG, scalar2=BIG, op0=ALU.mult, op1=ALU.add),
                 reads=[(mk, None)], writes=[(mk, None)])
            P.op("dve", lambda e: e.tensor_tensor(out=pos[:, :, :], in0=pos[:, :, :], in1=mk[:, :, :], op=ALU.add),
                 reads=[(pos, None), (mk, None)], writes=[(pos, None)])
            P.op("dve", lambda e: e.tensor_copy(out=posi[:, :, :], in_=pos[:, :, :]), reads=[(pos, None)], writes=[(posi, None)])
            for j in range(NL):
                P.op("pool", lambda e, j=j: e.tensor_copy(out=vals[:, j, :, 0], in_=tokf[:, :]), reads=[(tokf, None)], writes=[(vals, (j, 0))])
                P.op("pool", lambda e, j=j: e.tensor_copy(out=vals[:, j, :, 1], in_=Gmy[:, j, :]), reads=[(Gmy, None)], writes=[(vals, (j, 1))])
            if "pos" in self.cfg.get("debug", ()):
                self.debug_out("dbg_pos", pos, [128, NLA, 64])
            scatter_ids(0)
            P.barrier()

        with ExitStack() as st:
            xeT = self.sb(st, "xeT", [128, 16, CAP], BF16)
            actT = self.sb(st, "actT", [128, 16, CAP], BF16)
            wp = [self.sb(st, "wp", [128, 16, 512], BF16) for _ in range(2)]
            wo = [self.sb(st, "wo", [128, 16, 512], BF16) for _ in range(2)]
            xg = [self.sb(st, "xg", [128, D], BF16) for _ in range(2)]
            itl = [self.sb(st, "itl", [128, 2], F32) for _ in range(2)]
            idi = [self.sb(st, "idi", [128, 1], I32) for _ in range(2)]
            id8f = [[self.sb(st, "id8f", [128, 4], F32) for _ in range(NSL)] for _ in range(2)]
            id8 = [[self.sb(st, "id8", [128, 4], I32) for _ in range(NSL)] for _ in range(2)]
            io8 = self.sb(st, "io8", [128, 4], F32)
            t8 = [self.sb(st, "t8", [128, 1], F32) for _ in range(2)]
            gat = [self.sb(st, "gat", [128, NSL], F32) for _ in range(2)]
            bin_t = self.sb(st, "bin_t", [128, NL, 32], F32)
            bin1 = self.sb(st, "bin1", [128, NL, 16], F32)
            bout = self.sb(st, "bout", [1, NL, D], BF16)
            gc = [self.sb(st, "gc", [128, 512], F32) for _ in range(2)]
            sg = [self.sb(st, "sg", [128, 512], F32) for _ in range(2)]
            lc = [self.sb(st, "lc", [128, 512], F32) for _ in range(2)]
            ysc = [self.sb(st, "ysc", [128, 512], F32) for _ in range(6)]
            pT = [self.ps(st, "pT", [128, 8, 128], BF16) for _ in range(2)]
            pg = [self.ps(st, "pg", [128, 512], F32) for _ in range(2)]
            pl = [self.ps(st, "pl", [128, 512], F32) for _ in range(2)]
            py = [self.ps(st, "py", [128, 512], F32) for _ in range(2)]

            self.load(bin_t, bin_t[:, :, :], I["moe_bin"], I["moe_bin"][:, :, :])
            P.op("dve", lambda e: e.tensor_scalar(out=bin1[:, :, :], in0=bin_t[:, :, 16:32], scalar1=1.0, scalar2=None, op0=ALU.add),
                 reads=[(bin_t, None)], writes=[(bin1, None)])
            self.load(bout, bout.h.ap().rearrange("o j (a n) -> o (j a) n", a=4), I["moe_bout"],
                      I["moe_bout"].h.ap().rearrange("o j (a n) -> o (j a) n", a=4), q="pool", cls="w")
            for k in range(4):
                P.op("dve", lambda e, k=k: e.memset(io8[:, k:k + 1], float(k)), writes=[(io8, k)])
            n_sw = 0
            n_y = 0
            batches = [(0, 512), (512, 512), (1024, 256)]

            def prep_gather(j, i):
                iv = idxb[j]
                it_, ii_, x_ = itl[i % 2], idi[i % 2], xg[i % 2]
                gat_, id8_, id8f_ = gat[j % 2], id8[j % 2], id8f[j % 2]
                self.load(it_, it_[:, :], iv, iv[i * 128:(i + 1) * 128, :])
                P.op("dve", lambda e: e.tensor_copy(out=ii_[:, :], in_=it_[:, 0:1]), reads=[(it_, None)], writes=[(ii_, None)])
                P.op("dve", lambda e: e.tensor_copy(out=gat_[:, i:i + 1], in_=it_[:, 1:2]), reads=[(it_, None)], writes=[(gat_, i)])
                P.op("dve", lambda e: e.tensor_scalar(out=t8[i % 2][:, :], in0=it_[:, 0:1], scalar1=4.0, scalar2=None, op0=ALU.mult),
                     reads=[(it_, None)], writes=[(t8[i % 2], None)])
                P.op("dve", lambda e: e.tensor_scalar(out=id8f_[i][:, :], in0=io8[:, :], scalar1=t8[i % 2][:, 0:1], scalar2=None, op0=ALU.add),
                     reads=[(t8[i % 2], None), (io8, None)], writes=[(id8f_[i], None)])
                P.op("dve", lambda e: e.tensor_copy(out=id8_[i][:, :], in_=id8f_[i][:, :]), reads=[(id8f_[i], None)], writes=[(id8_[i], None)])
                P.dma("pool", "g", lambda e: e.indirect_dma_start(
                    out=x_[:, :], out_offset=None, in_=h1all[:, :], in_offset=bass.IndirectOffsetOnAxis(ap=ii_[:, 0:1], axis=0)),
                    reads=[(h1all, None), (ii_, None)], writes=[(x_, None)])

            def prep_transpose(j, i):
                x_ = xg[i % 2]
                for cg in range(2):
                    pt = pT[cg % 2]
                    for k in range(8):
                        c = cg * 8 + k
                        P.op("pe", lambda e, pt=pt, k=k, c=c: e.transpose(out=pt[:, k, :], in_=x_[:, c * 128:(c + 1) * 128], identity=C["ident_b"][:, :]),
                             reads=[(x_, None), (C["ident_b"], None)], writes=[(pt, k)])
                    if cg == 0:
                        P.op("dve", lambda e, pt=pt: e.tensor_copy(out=xeT[:, 0:8, i * 128:(i + 1) * 128], in_=pt[:, :, :]),
                             reads=[(pt, None)], writes=[(xeT, i)], accum="x%d" % j)
                    else:
                        P.op("act", lambda e, pt=pt: e.activation(out=xeT[:, 8:16, i * 128:(i + 1) * 128], in_=pt[:, :, :], func=AF.Copy),
                             reads=[(pt, None)], writes=[(xeT, i)], accum="x%d" % j)

            def load_wp(j, k):
                if j < NL and k < 8:
                    w_ = wp[k % 2]
                    self.load(w_, w_[:, :, :], I["moe_win"], I["moe_win"].h.ap()[j, k], q="pool", cls="w")

            def load_wo(j, m2):
                if j < NL and m2 < 4:
                    o_ = wo[m2 % 2]
                    self.load(o_, o_[:, :, :], I["moe_wout"], I["moe_wout"].h.ap()[j, m2], q="pool", cls="w")

            load_wp(0, 0)
            load_wp(0, 1)
            for i in range(NSL):
                prep_gather(0, i)
                prep_transpose(0, i)
            ypv = ypart.h.ap().rearrange("(a p r) n -> a p (r n)", p=128, r=2)
            for a in range(NROW // 64):
                self.P.dma("sp", "st", lambda e, a=a: e.dma_start(out=ypv[a], in_=zb[:, :]),
                           reads=[(zb, None)], writes=[(ypart, None)], accum="z")
            for j in range(NL):
                gat_, id8_ = gat[j % 2], id8[j % 2]
                load_wo(j, 0)
                load_wo(j, 1)
                for k in range(8):
                    w_ = wp[k % 2]
                    if k == 2 and j + 1 < NL:
                        scatter_ids(j + 1)
                    for jj in range(2):
                        m = 2 * k + jj
                        for bi, (s0, nb) in enumerate(batches):
                            g_, l_ = pg[n_sw % 2], pl[n_sw % 2]
                            gc_, sg_, lc_ = gc[n_sw % 2], sg[n_sw % 2], lc[n_sw % 2]
                            n_sw += 1
                            rk = [(xeT, t) for t in range(s0 // 128, (s0 + nb) // 128)]
                            for c in range(16):
                                P.op("pe", lambda e, g_=g_, w_=w_, c=c, jj=jj, s0=s0, nb=nb: e.matmul(
                                    out=g_[:, 0:nb], lhsT=w_[:, c, jj * 128:(jj + 1) * 128], rhs=xeT[:, c, s0:s0 + nb],
                                    start=(c == 0), stop=(c == 15)), reads=[(w_, None)] + rk, writes=[(g_, None)], inc=(c == 15))
                            for c in range(16):
                                P.op("pe", lambda e, l_=l_, w_=w_, c=c, jj=jj, s0=s0, nb=nb: e.matmul(
                                    out=l_[:, 0:nb], lhsT=w_[:, c, 256 + jj * 128:256 + (jj + 1) * 128], rhs=xeT[:, c, s0:s0 + nb],
                                    start=(c == 0), stop=(c == 15)), reads=[(w_, None)] + rk, writes=[(l_, None)], inc=(c == 15))
                            P.op("dve", lambda e, g_=g_, gc_=gc_, m=m, nb=nb, j=j: e.tensor_scalar(
                                out=gc_[:, 0:nb], in0=g_[:, 0:nb], scalar1=bin_t[:, j, m:m + 1], scalar2=7.0, op0=ALU.add, op1=ALU.min),
                                reads=[(g_, None), (bin_t, None)], writes=[(gc_, None)])
                            P.op("act", lambda e, gc_=gc_, sg_=sg_, nb=nb: e.activation(out=sg_[:, 0:nb], in_=gc_[:, 0:nb], func=AF.Sigmoid, scale=1.702),
                                 reads=[(gc_, None)], writes=[(sg_, None)])
                            P.op("dve", lambda e, l_=l_, lc_=lc_, m=m, nb=nb, j=j: e.tensor_scalar(
                                out=lc_[:, 0:nb], in0=l_[:, 0:nb], scalar1=bin1[:, j, m:m + 1], scalar2=8.0, op0=ALU.add, op1=ALU.min),
                                reads=[(l_, None), (bin1, None)], writes=[(lc_, None)])
                            P.op("dve", lambda e, gc_=gc_, lc_=lc_, nb=nb: e.scalar_tensor_tensor(
                                out=lc_[:, 0:nb], in0=lc_[:, 0:nb], scalar=-6.0, in1=gc_[:, 0:nb], op0=ALU.max, op1=ALU.mult),
                                reads=[(gc_, None), (lc_, None)], writes=[(lc_, None)])
                            P.op("dve", lambda e, sg_=sg_, lc_=lc_, m=m, s0=s0, nb=nb: e.tensor_tensor(
                                out=actT[:, m, s0:s0 + nb], in0=lc_[:, 0:nb], in1=sg_[:, 0:nb], op=ALU.mult),
                                reads=[(sg_, None), (lc_, None)], writes=[(actT, bi)], accum="a%d" % j)
                    load_wp(j, k + 2)
                it = 0
                for m2 in range(4):
                    o_ = wo[m2 % 2]
                    for i in range(NSL):
                        y_ = py[n_y % 2]
                        ys_ = ysc[n_y % len(ysc)]
                        n_y += 1
                        bi = 0 if i < 4 else (1 if i < 8 else 2)
                        for c in range(16):
                            P.op("pe", lambda e, y_=y_, o_=o_, c=c, i=i: e.matmul(
                                out=y_[:, :], lhsT=actT[:, c, i * 128:(i + 1) * 128], rhs=o_[:, c, :], start=(c == 0), stop=False),
                                reads=[(actT, bi), (o_, None)], writes=[(y_, None)], inc=False)
                        P.op("pe", lambda e, y_=y_, m2=m2, j=j: e.matmul(
                            out=y_[:, :], lhsT=C["ones_b"][0:1, :], rhs=bout[0:1, j, m2 * 512:(m2 + 1) * 512], start=False, stop=True),
                            reads=[(C["ones_b"], None), (bout, None)], writes=[(y_, None)])
                        P.op("act", lambda e, y_=y_, ys_=ys_, i=i, gat_=gat_: e.activation(out=ys_[:, :], in_=y_[:, :], func=AF.Copy, scale=gat_[:, i:i + 1]),
                             reads=[(y_, None), (gat_, i)], writes=[(ys_, None)])
                        P.dma("pool", "s", lambda e, ys_=ys_, i=i, m2=m2, id8_=id8_: e.indirect_dma_start(
                            out=ypart[:, :], out_offset=bass.IndirectOffsetOnAxis(ap=id8_[i][:, m2:m2 + 1], axis=0),
                            in_=ys_[:, :], in_offset=None, compute_op=ALU.add),
                            reads=[(ys_, None), (id8_[i], None)], writes=[(ypart, None)], accum="y%d" % j)
                        if j + 1 < NL:
                            if it < NSL:
                                prep_gather(j + 1, it)
                            if 1 <= it <= NSL:
                                prep_transpose(j + 1, it - 1)
                        it += 1
                    load_wo(j, m2 + 2)
                    if m2 >= 2:
                        load_wp(j + 1, m2 - 2)
            P.barrier()

    def build_C(self):
        nc, P, I, C = self.nc, self.P, self.I, self.C
        I["parts"] = self.inp("parts", [NCORES, NT, D])
        I["h1"] = self.inp("h1", [NT, D])
        I["ln_g"] = self.inp("ln_g", [1, D])
        I["ln_b"] = self.inp("ln_b", [1, D])
        h2 = self.outp("h2_out", [NT, D])
        with ExitStack() as st:
            gb = self.sb(st, "gb", [128, D], F32)
            bb = self.sb(st, "bb", [128, D], F32)
            pt_ = [self.sb(st, "pt", [128, NCORES, D], F32) for _ in range(2)]
            hf = [self.sb(st, "hf2", [128, D], F32) for _ in range(2)]
            acc = [self.sb(st, "acc", [128, D], F32) for _ in range(2)]
            ot = [self.sb(st, "ot", [128, D], F32) for _ in range(2)]
            lnt = (self.sb(st, "stats", [128, 4, 6], F32), self.sb(st, "mv", [128, 2], F32),
                   self.sb(st, "rstd", [128, 1], F32), self.sb(st, "xn", [128, D], F32))
            self.load(gb, gb[:, :], I["ln_g"], I["ln_g"].h.ap().partition_broadcast(128))
            self.load(bb, bb[:, :], I["ln_b"], I["ln_b"].h.ap().partition_broadcast(128))
            for i in range(NTI):
                p_, f, a_, o_ = pt_[i % 2], hf[i % 2], acc[i % 2], ot[i % 2]
                for r in range(NCORES):
                    self.load(p_, p_[:, r, :], I["parts"], I["parts"].h.ap()[r, i * 128:(i + 1) * 128, :], dkey=r, q=("sp" if r % 2 == 0 else "act"))
                self.load(f, f[:, :], I["h1"], I["h1"][i * 128:(i + 1) * 128, :])
                P.op("dve", lambda e, p_=p_, f=f, a_=a_: e.scalar_tensor_tensor(out=a_[:, :], in0=f[:, :], scalar=ALPHA, in1=p_[:, 0, :],
                                                                                op0=ALU.mult, op1=ALU.add),
                     reads=[(f, None), (p_, 0)], writes=[(a_, None)])
                for r in range(1, NCORES):
                    eng = "dve"
                    P.op(eng, lambda e, p_=p_, a_=a_, r=r: e.tensor_tensor(out=a_[:, :], in0=a_[:, :], in1=p_[:, r, :], op=ALU.add),
                         reads=[(a_, None), (p_, r)], writes=[(a_, None)])
                self.layer_norm_tile(lnt, a_, None, gb, bb, o_, o_[:, :], None)
                self.P.dma("pool", "st", lambda e, o_=o_, i=i: e.dma_start(out=h2[i * 128:(i + 1) * 128, :], in_=o_[:, :]),
                           reads=[(o_, None)], writes=[(h2, i)])
            P.barrier()

    def debug_out(self, name, tile, shape, dt=F32):
        o = self.outp(name, shape, dt)
        self.dbg[name] = o
        idx = tuple(slice(None) for _ in shape)
        self.P.dma("sp", "st", lambda e: e.dma_start(out=o[idx], in_=tile[idx]), reads=[(tile, None)], writes=[(o, None)])

    def stage_xT(self, st_unused, src, ntiles, dstT, tok0, xs, pT):
        P, C = self.P, self.C
        for t in range(ntiles):
            x_ = xs[t % 2]
            self.load(x_, x_.h.ap().rearrange("p (a n) -> p a n", a=4), src,
                      src[t * 128:(t + 1) * 128, :].rearrange("p (a n) -> p a n", a=4), q="pool", cls="w")
            for cg in range(2):
                for k in range(8):
                    c = cg * 8 + k
                    P.op("pe", lambda e, k=k, c=c, x_=x_: e.transpose(out=pT[:, k, :], in_=x_[:, c * 128:(c + 1) * 128], identity=C["ident_b"][:, :]),
                         reads=[(x_, None), (C["ident_b"], None)], writes=[(pT, k)])
                o0 = tok0 + t * 128
                if cg == 0:
                    P.op("dve", lambda e, cg=cg, o0=o0: e.tensor_copy(out=dstT[:, 0:8, o0:o0 + 128], in_=pT[:, :, :]),
                         reads=[(pT, None)], writes=[(dstT, ("t", o0 // 128))], accum="stage")
                else:
                    P.op("act", lambda e, cg=cg, o0=o0: e.activation(out=dstT[:, 8:16, o0:o0 + 128], in_=pT[:, :, :], func=AF.Copy),
                         reads=[(pT, None)], writes=[(dstT, ("t", o0 // 128))], accum="stage")

    def mem_kv(self, outer, wmem_in, memb_in):
        P, C = self.P, self.C
        memK = self.sb(outer, "memK", [128, 4, 256], BF16)
        memV = self.sb(outer, "memV", [128, 2, 512], BF16)
        with ExitStack() as st:
            wm = self.sb(st, "wm", [128, 16, 1024], BF16)
            memT = self.sb(st, "memT", [128, 16, 256], BF16)
            xs = [self.sb(st, "xs", [128, D], BF16) for _ in range(2)]
            pT = self.ps(st, "pT", [128, 8, 128], BF16)
            pa = [self.ps(st, "pa", [128, 512], F32) for _ in range(2)]
            self.load(wm, wm[:, :, :], wmem_in, wmem_in.h.ap().rearrange("(c p) n -> p c n", p=128), q="pool", cls="w")
            self.stage_xT(st, memb_in, 2, memT, 0, xs, pT)
            n = 0
            for mh in range(4):
                a_ = pa[n % 2]; n += 1
                for c in range(16):
                    P.op("pe", lambda e, a_=a_, c=c, mh=mh: e.matmul(out=a_[:, 0:256], lhsT=wm[:, c, mh * 128:(mh + 1) * 128], rhs=memT[:, c, :],
                                                                      start=(c == 0), stop=(c == 15)),
                         reads=[(wm, None), (memT, None)], writes=[(a_, None)], inc=(c == 15))
                P.op("act", lambda e, a_=a_, mh=mh: e.activation(out=memK[:, mh, :], in_=a_[:, 0:256], func=AF.Copy),
                     reads=[(a_, None)], writes=[(memK, mh)])
            for mt in range(2):
                a_ = pa[n % 2]; n += 1
                for c in range(16):
                    P.op("pe", lambda e, a_=a_, c=c, mt=mt: e.matmul(out=a_[:, :], lhsT=memT[:, c, mt * 128:(mt + 1) * 128], rhs=wm[:, c, 512:1024],
                                                                      start=(c == 0), stop=(c == 15)),
                         reads=[(wm, None), (memT, None)], writes=[(a_, None)], inc=(c == 15))
                P.op("dve", lambda e, a_=a_, mt=mt: e.tensor_copy(out=memV[:, mt, :], in_=a_[:, :]), reads=[(a_, None)], writes=[(memV, mt)])
            P.barrier()
        return memK, memV

    def mem_attn(self, st, w_in, col0, srcT, tok0, memK, memV, catT, wbuf, qbuf, pa, pss, poo, ex, rd):
        P, C = self.P, self.C
        n = 0
        for mh in range(4):
            w_ = wbuf[mh % len(wbuf)]
            with self.nc.allow_non_contiguous_dma(reason="512B runs"):
                self.load(w_, w_[:, :, :], w_in, w_in.h.ap()[:, col0 + mh * 128:col0 + (mh + 1) * 128].rearrange("(c p) n -> p c n", p=128), q="pool", cls="w")
            for hf_ in range(2):
                a_ = pa[n % 2]; n += 1
                for c in range(16):
                    P.op("pe", lambda e, a_=a_, c=c, w_=w_, hf_=hf_: e.matmul(out=a_[:, :], lhsT=w_[:, c, :], rhs=srcT[:, c, tok0 + hf_ * 512:tok0 + (hf_ + 1) * 512],
                                                                             start=(c == 0), stop=(c == 15)),
                         reads=[(w_, None), (srcT, None)], writes=[(a_, None)], inc=(c == 15))
                P.op("act", lambda e, a_=a_, hf_=hf_: e.activation(out=qbuf[:, hf_ * 512:(hf_ + 1) * 512], in_=a_[:, :], func=AF.Copy),
                     reads=[(a_, None)], writes=[(qbuf, hf_)])
            for hf_ in range(2):
                for mt in range(2):
                    P.op("pe", lambda e, mt=mt, mh=mh, hf_=hf_: e.matmul(out=pss[mt][:, :], lhsT=memK[:, mh, mt * 128:(mt + 1) * 128],
                                                                        rhs=qbuf[:, hf_ * 512:(hf_ + 1) * 512], start=True, stop=True),
                         reads=[(memK, None), (qbuf, hf_)], writes=[(pss[mt], None)])
                    P.op("act", lambda e, mt=mt: e.activation(out=ex[:, mt, :], in_=pss[mt][:, :], func=AF.Exp, scale=SCALE),
                         reads=[(pss[mt], None)], writes=[(ex, mt)])
                for mt in range(2):
                    P.op("pe", lambda e, mt=mt, mh=mh: e.matmul(out=poo[0][:, :], lhsT=memV[:, mt, mh * 128:(mh + 1) * 128], rhs=ex[:, mt, :],
                                                               start=(mt == 0), stop=(mt == 1)),
                         reads=[(memV, None), (ex, mt)], writes=[(poo[0], None)], inc=(mt == 1))
                for mt in range(2):
                    P.op("pe", lambda e, mt=mt: e.matmul(out=poo[1][:, :], lhsT=C["ones_b"][:, :], rhs=ex[:, mt, :], start=(mt == 0), stop=(mt == 1)),
                         reads=[(C["ones_b"], None), (ex, mt)], writes=[(poo[1], None)], inc=(mt == 1))
                P.op("dve", lambda e: e.reciprocal(out=rd[:, 0:512], in_=poo[1][:, :]), reads=[(poo[1], None)], writes=[(rd, None)])
                P.op("dve", lambda e, mh=mh, hf_=hf_: e.tensor_tensor(out=catT[:, 12 + mh, hf_ * 512:(hf_ + 1) * 512], in0=poo[0][:, :], in1=rd[:, 0:512], op=ALU.mult),
                     reads=[(poo[0], None), (rd, None)], writes=[(catT, ("m", mh, hf_))])

    def mix_out(self, catT, wmix_in, xres_in, lng_in, lnb_in, out_t):
        P, C = self.P, self.C
        with ExitStack() as st:
            wm = self.sb(st, "wmx", [128, 16, D], BF16)
            gb = self.sb(st, "gb", [128, D], F32)
            bb = self.sb(st, "bb", [128, D], F32)
            xt = [self.sb(st, "xt", [128, D], F32) for _ in range(2)]
            rt = [self.sb(st, "rt", [128, D], F32) for _ in range(1)]
            ot = [self.sb(st, "ot", [128, D], F32) for _ in range(1)]
            lnt = (self.sb(st, "stats", [128, 4, 6], F32), self.sb(st, "mv", [128, 2], F32),
                   self.sb(st, "rstd", [128, 1], F32), self.sb(st, "xn", [128, D], F32))
            pm = [self.ps(st, "pm", [128, 512], F32) for _ in range(4)]
            for n in range(4):
                self.load(wm, wm[:, :, n * 512:(n + 1) * 512], wmix_in, wmix_in.h.ap()[:, n * 512:(n + 1) * 512].rearrange("(c p) n -> p c n", p=128),
                          q="pool", cls="w", dkey=n)
            self.load(gb, gb[:, :], lng_in, lng_in.h.ap().partition_broadcast(128))
            self.load(bb, bb[:, :], lnb_in, lnb_in.h.ap().partition_broadcast(128))
            for i in range(NTI):
                x_, r_, o_ = xt[i % 2], rt[0], ot[0]
                self.load(x_, x_[:, :], xres_in, xres_in[i * 128:(i + 1) * 128, :])
                for n in range(4):
                    for c in range(16):
                        P.op("pe", lambda e, n=n, c=c, i=i: e.matmul(out=pm[n][:, :], lhsT=catT[:, c, i * 128:(i + 1) * 128], rhs=wm[:, c, n * 512:(n + 1) * 512],
                                                                    start=(c == 0), stop=(c == 15)),
                             reads=[(catT, None), (wm, n)], writes=[(pm[n], None)], inc=(c == 15))
                    P.op("dve", lambda e, n=n, x_=x_, r_=r_: e.scalar_tensor_tensor(out=r_[:, n * 512:(n + 1) * 512], in0=x_[:, n * 512:(n + 1) * 512], scalar=ALPHA,
                                                                                    in1=pm[n][:, :], op0=ALU.mult, op1=ALU.add),
                         reads=[(x_, None), (pm[n], None)], writes=[(r_, n)])
                self.layer_norm_tile(lnt, r_, None, gb, bb, o_, o_[:, :], None)
                self.P.dma("pool", "st", lambda e, o_=o_, i=i: e.dma_start(out=out_t[i * 128:(i + 1) * 128, :], in_=o_[:, :]),
                           reads=[(o_, None)], writes=[(out_t, i)])
            P.barrier()

    def mixer_dil(self):
        nc, P, I, C = self.nc, self.P, self.I, self.C
        I["x_prev"] = self.inp("x_prev", [2048, D])
        I["memb"] = self.inp("memb", [256, D])
        I["w_in"] = self.inp("w_in", [D, 3 * W + MW])
        I["w_mem"] = self.inp("w_mem", [D, 2 * MW])
        I["w_mix"] = self.inp("w_mix", [D, D])
        I["ln1_g"] = self.inp("ln1_g", [1, D])
        I["ln1_b"] = self.inp("ln1_b", [1, D])
        I["attn_c"] = self.inp("attn_c", [128, 4, 128])
        w_in = I["w_in"]
        slopes = _alibi_slopes(12)
        with ExitStack() as outer:
            catT = self.sb(outer, "catT", [128, 16, NT], BF16)
            memK, memV = self.mem_kv(outer, I["w_mem"], I["memb"])
            with ExitStack() as st:
                xT = self.sb(st, "xT", [128, 16, 3072], BF16)
                xs = [self.sb(st, "xs", [128, D], BF16) for _ in range(2)]
                wq = [self.sb(st, "wq", [128, 16, 128], BF16) for _ in range(1)]
                wk = [self.sb(st, "wk", [128, 16, 128], BF16) for _ in range(1)]
                wv = [self.sb(st, "wv", [128, 16, 128], BF16) for _ in range(1)]
                nat = self.sb(st, "nat", [128, 3072], BF16)
                qTd = self.sb(st, "qTd", [128, NT], BF16)
                kTd = self.sb(st, "kTd", [128, 3072], BF16)
                vTd = self.sb(st, "vTd", [128, 3072], BF16)
                Vt = self.sb(st, "Vt", [128, 32, 128], BF16)
                Ob = self.sb(st, "Ob", [128, 3, NT], BF16)
                den = self.sb(st, "den", [128, NT], F32)
                rd = self.sb(st, "rd", [128, NT], F32)
                dtab = self.sb(st, "dtab", [128, 4, 128], F32)
                zz = [self.sb(st, "zz", [128, 2, 128], F32) for _ in range(2)]
                pp = [self.sb(st, "pp", [128, 2, 128], BF16) for _ in range(2)]
                exm = self.sb(st, "exm", [128, 2, 512], BF16)
                pT = self.ps(st, "pT", [128, 8, 128], BF16)
                pa = [self.ps(st, "pa", [128, 512], F32) for _ in range(2)]
                pss = [self.ps(st, "pss", [128, 512], F32) for _ in range(2)]
                poo = [self.ps(st, "poo", [128, 512], F32) for _ in range(2)]
                self.load(dtab, dtab[:, :, :], I["attn_c"], I["attn_c"][:, :, :])
                self.stage_xT(st, I["x_prev"], 16, xT, 0, xs, pT)
                self.stage_xT(st, I["x_own"], 8, xT, 2048, xs, pT)
                npa = 0
                nq = 0
                nh = 0
                stop = self.cfg.get("stop", 99)
                for j in range(4 if stop > 0 else 0):
                    for g in range(self.cfg.get("gmax", 3)):
                        hh = 4 * g + j
                        d = DIL[g]
                        Tp = TPREV[g]
                        Q = 128 if g < 2 else 64
                        ch = slopes[hh] * d
                        wq_, wk_, wv_ = wq[0], wk[0], wv[0]
                        nh += 1
                        with nc.allow_non_contiguous_dma(reason="512B runs"):
                            for w_, c0 in ((wq_, hh * 128), (wk_, W + hh * 128), (wv_, 2 * W + hh * 128)):
                                self.load(w_, w_[:, :, :], w_in, w_in.h.ap()[:, c0:c0 + 128].rearrange("(c p) n -> p c n", p=128), q="pool", cls="w")
                        Lq = NT // d
                        Lk = 128 + Lq
                        tot = Tp + NT
                        def proj(w_, c0tok, ntok, dstd, L_, eng):
                            nonlocal npa
                            s0 = 0
                            while s0 < ntok:
                                nb = min(512, ntok - s0)
                                a_ = pa[npa % 2]; npa += 1
                                for c in range(16):
                                    P.op("pe", lambda e, a_=a_, c=c, w_=w_, s0=s0, nb=nb: e.matmul(
                                        out=a_[:, 0:nb], lhsT=w_[:, c, :], rhs=xT[:, c, c0tok + s0:c0tok + s0 + nb], start=(c == 0), stop=(c == 15)),
                                        reads=[(w_, None), (xT, None)], writes=[(a_, None)], inc=(c == 15))
                                P.op("act", lambda e, a_=a_, s0=s0, nb=nb: e.activation(out=nat[:, s0:s0 + nb], in_=a_[:, 0:nb], func=AF.Copy),
                                     reads=[(a_, None)], writes=[(nat, s0 // 512)])
                                s0 += nb
                            src = nat.h.ap()[:, 0:ntok].rearrange("p (l r) -> p r l", r=d)
                            dst = dstd.h.ap()[:, 0:ntok].rearrange("p (r l) -> p r l", r=d)
                            P.op(eng, lambda e, src=src, dst=dst: e.tensor_copy(out=dst, in_=src), reads=[(nat, None)], writes=[(dstd, None)])
                        proj(wq_, 2048, NT, qTd, Lq, "dve")
                        proj(wk_, 2048 - Tp, tot, kTd, Lk, "pool")
                        proj(wv_, 2048 - Tp, tot, vTd, Lk, "dve")
                        nbk = (Lk + 127) // 128
                        blocks = [(r, b, r * Lk + b * 128, min(128, Lk - b * 128)) for r in range(d) for b in range(nbk)]
                        for b0 in range(0, len(blocks), 8):
                            grp = blocks[b0:b0 + 8]
                            for gi, (r, b, p0, cnt) in enumerate(grp):
                                P.op("pe", lambda e, gi=gi, p0=p0, cnt=cnt: e.transpose(out=pT[0:cnt, gi, :], in_=vTd[:, p0:p0 + cnt], identity=C["ident_b"][:, :]),
                                     reads=[(vTd, None), (C["ident_b"], None)], writes=[(pT, gi)])
                            ng = len(grp)
                            P.op("act", lambda e, b0=b0, ng=ng: e.activation(out=Vt[:, b0:b0 + ng, :], in_=pT[:, 0:ng, :], func=AF.Copy),
                                 reads=[(pT, None)], writes=[(Vt, ("v", b0))], accum="v%d" % hh)
                        nto = Lq // Q
                        for r in range(d if stop > 1 else 0):
                            for n in range(nto):
                                z_, p_ = zz[nq % 2], pp[nq % 2]
                                s_, o_ = pss[nq % 2], poo[nq % 2]
                                nq += 1
                                q0 = r + d * n * Q
                                qd0 = r * Lq + n * Q
                                kp0 = r * Lk + n * Q
                                kc0 = r * Lk + 128 + n * Q
                                vprev = r * nbk + (n * Q) // 128
                                vcur = r * nbk + (128 + n * Q) // 128
                                tbl = (2 if g < 2 else 3) if n == 0 else 0
                                P.op("pe", lambda e, s_=s_, kp0=kp0, qd0=qd0, Q=Q: e.matmul(
                                    out=s_[:, 0:Q], lhsT=kTd[:, kp0:kp0 + 128], rhs=qTd[:, qd0:qd0 + Q], start=True, stop=True),
                                    reads=[(kTd, None), (qTd, None)], writes=[(s_, "p")])
                                P.op("pe", lambda e, s_=s_, kc0=kc0, qd0=qd0, Q=Q: e.matmul(
                                    out=s_[0:Q, 128:128 + Q], lhsT=kTd[:, kc0:kc0 + Q], rhs=qTd[:, qd0:qd0 + Q], start=True, stop=True),
                                    reads=[(kTd, None), (qTd, None)], writes=[(s_, "c")])
                                P.op("dve", lambda e, s_=s_, z_=z_, tbl=tbl, Q=Q, ch=ch: e.scalar_tensor_tensor(
                                    out=z_[:, 0, 0:Q], in0=dtab[:, tbl, 0:Q], scalar=-ch / SCALE, in1=s_[:, 0:Q], op0=ALU.mult, op1=ALU.add),
                                    reads=[(dtab, None), (s_, "p")], writes=[(z_, "p")])
                                P.op("dve", lambda e, s_=s_, z_=z_, Q=Q, ch=ch: e.scalar_tensor_tensor(
                                    out=z_[0:Q, 1, 0:Q], in0=dtab[0:Q, 1, 0:Q], scalar=-ch / SCALE, in1=s_[0:Q, 128:128 + Q], op0=ALU.mult, op1=ALU.add),
                                    reads=[(dtab, None), (s_, "c")], writes=[(z_, "c")])
                                P.op("act", lambda e, z_=z_, p_=p_, Q=Q: e.activation(out=p_[:, 0, 0:Q], in_=z_[:, 0, 0:Q], func=AF.Exp, scale=SCALE),
                                     reads=[(z_, "p")], writes=[(p_, "p")])
                                P.op("act", lambda e, z_=z_, p_=p_, Q=Q: e.activation(out=p_[0:Q, 1, 0:Q], in_=z_[0:Q, 1, 0:Q], func=AF.Exp, scale=SCALE),
                                     reads=[(z_, "c")], writes=[(p_, "c")])
                                P.op("pe", lambda e, o_=o_, p_=p_, vprev=vprev, Q=Q: e.matmul(
                                    out=o_[:, 0:Q], lhsT=Vt[:, vprev, :], rhs=p_[:, 0, 0:Q], start=True, stop=False),
                                    reads=[(Vt, None), (p_, "p")], writes=[(o_, "o")], inc=False)
                                P.op("pe", lambda e, o_=o_, p_=p_, vcur=vcur, Q=Q: e.matmul(
                                    out=o_[:, 0:Q], lhsT=Vt[0:Q, vcur, :], rhs=p_[0:Q, 1, 0:Q], start=False, stop=True),
                                    reads=[(Vt, None), (p_, "c")], writes=[(o_, "o")])
                                P.op("pe", lambda e, o_=o_, p_=p_, Q=Q: e.matmul(
                                    out=o_[:, 128:128 + Q], lhsT=C["ones_b"][:, :], rhs=p_[:, 0, 0:Q], start=True, stop=False),
                                    reads=[(C["ones_b"], None), (p_, "p")], writes=[(o_, "s")], inc=False)
                                P.op("pe", lambda e, o_=o_, p_=p_, Q=Q: e.matmul(
                                    out=o_[:, 128:128 + Q], lhsT=C["ones_b"][0:Q, :], rhs=p_[0:Q, 1, 0:Q], start=False, stop=True),
                                    reads=[(C["ones_b"], None), (p_, "c")], writes=[(o_, "s")])
                                P.op("act", lambda e, o_=o_, g=g, q0=q0, d=d, Q=Q: e.activation(out=Ob[:, g, sl(q0, Q, d)], in_=o_[:, 0:Q], func=AF.Copy),
                                     reads=[(o_, "o")], writes=[(Ob, g)], accum="ob%d" % hh)
                                if g == 0:
                                    P.op("dve", lambda e, o_=o_, q0=q0, d=d, Q=Q: e.tensor_copy(out=den[:, sl(q0, Q, d)], in_=o_[:, 128:128 + Q]),
                                         reads=[(o_, "s")], writes=[(den, None)], accum="den%d" % hh)
                                else:
                                    P.op("dve", lambda e, o_=o_, q0=q0, d=d, Q=Q: e.tensor_tensor(out=den[:, sl(q0, Q, d)], in0=den[:, sl(q0, Q, d)],
                                                                                                in1=o_[:, 128:128 + Q], op=ALU.add),
                                         reads=[(o_, "s"), (den, None)], writes=[(den, None)], accum="den%d" % hh)
                    if stop > 1:
                        P.op("dve", lambda e: e.reciprocal(out=rd[:, :], in_=den[:, :]), reads=[(den, None)], writes=[(rd, None)])
                    for g in range(3 if stop > 1 else 0):
                        eng = "dve" if g != 1 else "pool"
                        P.op(eng, lambda e, g=g, j=j: e.tensor_tensor(out=catT[:, 4 * g + j, :], in0=Ob[:, g, :], in1=rd[:, :], op=ALU.mult),
                             reads=[(Ob, g), (rd, None)], writes=[(catT, ("s", 4 * g + j))])
                if stop > 2:
                    self.mem_attn(st, w_in, 3 * W, xT, 2048, memK, memV, catT, wq, qTd, pa, pss, poo, exm, rd)
                if "cat" in self.cfg.get("debug", ()):
                    self.debug_out("dbg_cat", catT, [128, 16, NT], BF16)
                P.barrier()
            self.mix_out(catT, I["w_mix"], I["x_own"], I["ln1_g"], I["ln1_b"], self.h1o)


    def mixer_ret(self):
        nc, P, I, C = self.nc, self.P, self.I, self.C
        I["h_prev"] = self.inp("h_prev", [3 * NT, D])
        I["memb"] = self.inp("memb", [256, D])
        I["w_in"] = self.inp("w_in", [D, 4 * W + MW])
        I["w_mem"] = self.inp("w_mem", [D, 2 * MW])
        I["w_mix"] = self.inp("w_mix", [D, D])
        I["ln1_g"] = self.inp("ln1_g", [1, D])
        I["ln1_b"] = self.inp("ln1_b", [1, D])
        I["gn_g"] = self.inp("gn_g", [128, 12])
        I["decT"] = self.inp("decT", [128, 12, 128])
        I["qdtab"] = self.inp("qdtab", [128, 12, 128])
        I["kdtab"] = self.inp("kdtab", [128, 12 * 128])
        w_in = I["w_in"]
        gam = [1.0 - 2.0 ** (-(5.0 + h)) for h in range(12)]
        cdec = [g_ ** 128 for g_ in gam]
        NCH = 32
        with ExitStack() as outer:
            memK, memV = self.mem_kv(outer, I["w_mem"], I["memb"])
            Vown = self.sb(outer, "Vown", [128, 8, W], BF16)
            Sb = self.sb(outer, "Sb", [128, 8, 12, 128], BF16)
            with ExitStack() as st:
                wk = self.sb(st, "wk_all", [128, 16, W], BF16)
                wv = self.sb(st, "wv_all", [128, 16, W], BF16)
                hT = self.sb(st, "hTb", [128, 16, 512], BF16)
                xs = [self.sb(st, "xs", [128, D], BF16) for _ in range(2)]
                kdt = self.sb(st, "kdt", [128, W], F32)
                Kd = [self.sb(st, "Kd", [128, W], BF16) for _ in range(2)]
                Vb = [self.sb(st, "Vb", [128, W], BF16) for _ in range(2)]
                S = self.sb(st, "S", [128, 12, 128], F32)
                pT = self.ps(st, "pT", [128, 8, 128], BF16)
                pj = [self.ps(st, "pj", [128, 512], F32) for _ in range(4)]
                pkv = [self.ps(st, "pkv", [128, 4, 128], F32) for _ in range(2)]
                for n in range(3):
                    with nc.allow_non_contiguous_dma(reason="2KB runs"):
                        self.load(wk, wk[:, :, n * 512:(n + 1) * 512], w_in, w_in.h.ap()[:, W + n * 512:W + (n + 1) * 512].rearrange("(c p) n -> p c n", p=128),
                                  q="pool", cls="w", dkey=n)
                        self.load(wv, wv[:, :, n * 512:(n + 1) * 512], w_in, w_in.h.ap()[:, 2 * W + n * 512:2 * W + (n + 1) * 512].rearrange("(c p) n -> p c n", p=128),
                                  q="pool", cls="w", dkey=n)
                self.load(kdt, kdt[:, :], I["kdtab"], I["kdtab"][:, :])
                P.op("dve", lambda e: e.memset(S[:, :, :], 0.0), writes=[(S, None)])
                npj = 0
                nkv = 0
                for blk in range(8):
                    if blk < 6:
                        src = Tile(I["h_prev"].h.ap()[blk * 512:(blk + 1) * 512, :], "hp")
                        src.deps = I["h_prev"].deps
                    else:
                        src = Tile(I["x_own"].h.ap()[(blk - 6) * 512:(blk - 5) * 512, :], "ho")
                        src.deps = I["x_own"].deps
                    self.stage_xT(st, src, 4, hT, 0, xs, pT)
                    for ci in range(4):
                        n = blk * 4 + ci
                        kd_, vb_ = Kd[n % 2], Vb[n % 2]
                        for which, w_, dst in (("k", wk, kd_), ("v", wv, vb_)):
                            for gq in range(3):
                                a_ = pj[npj % 4]; npj += 1
                                for c in range(16):
                                    P.op("pe", lambda e, a_=a_, c=c, w_=w_, gq=gq, ci=ci: e.matmul(
                                        out=a_[:, :], lhsT=hT[:, c, ci * 128:(ci + 1) * 128], rhs=w_[:, c, gq * 512:(gq + 1) * 512], start=(c == 0), stop=(c == 15)),
                                        reads=[(hT, None), (w_, gq)], writes=[(a_, None)], inc=(c == 15))
                                if which == "k":
                                    P.op("dve", lambda e, a_=a_, dst=dst, gq=gq: e.tensor_tensor(out=dst[:, gq * 512:(gq + 1) * 512], in0=a_[:, :],
                                                                                                 in1=kdt[:, gq * 512:(gq + 1) * 512], op=ALU.mult),
                                         reads=[(a_, None), (kdt, None)], writes=[(dst, gq)])
                                else:
                                    P.op("act", lambda e, a_=a_, dst=dst, gq=gq: e.activation(out=dst[:, gq * 512:(gq + 1) * 512], in_=a_[:, :], func=AF.Copy),
                                         reads=[(a_, None)], writes=[(dst, gq)])
                                    if n >= 24:
                                        P.op("pool", lambda e, dst=dst, gq=gq, n=n: e.tensor_copy(out=Vown[:, n - 24, gq * 512:(gq + 1) * 512], in_=dst[:, gq * 512:(gq + 1) * 512]),
                                             reads=[(dst, gq)], writes=[(Vown, (n - 24, gq))])
                        if n >= 24:
                            P.op("act", lambda e, n=n: e.activation(out=Sb[:, n - 24, :, :], in_=S[:, :, :], func=AF.Copy),
                                 reads=[(S, None)], writes=[(Sb, n - 24)])
                        if n == NCH - 1:
                            break
                        for hg in range(3):
                            kv_ = pkv[nkv % 2]; nkv += 1
                            for hh in range(4):
                                h = hg * 4 + hh
                                P.op("pe", lambda e, kv_=kv_, hh=hh, h=h, kd_=kd_, vb_=vb_: e.matmul(
                                    out=kv_[:, hh, :], lhsT=kd_[:, h * 128:(h + 1) * 128], rhs=vb_[:, h * 128:(h + 1) * 128], start=True, stop=True),
                                    reads=[(kd_, None), (vb_, None)], writes=[(kv_, None)])
                            for hh in range(4):
                                h = hg * 4 + hh
                                P.op("dve", lambda e, kv_=kv_, hh=hh, h=h: e.scalar_tensor_tensor(
                                    out=S[:, h, :], in0=S[:, h, :], scalar=cdec[h], in1=kv_[:, hh, :], op0=ALU.mult, op1=ALU.add),
                                    reads=[(kv_, None), (S, h)], writes=[(S, h)])
                P.barrier()
            catT = self.sb(outer, "catT", [128, 16, NT], BF16)
            with ExitStack() as st:
                hT = self.sb(st, "hTo", [128, 16, NT], BF16)
                xs = [self.sb(st, "xs", [128, D], BF16) for _ in range(2)]
                wq = [self.sb(st, "wq", [128, 16, 128], BF16) for _ in range(2)]
                wkf2 = [self.sb(st, "wkf", [128, 16, 128], BF16) for _ in range(2)]
                wg2 = [self.sb(st, "wg", [128, 16, 128], BF16) for _ in range(2)]
                qT = self.sb(st, "qT", [128, NT], BF16)
                kT = self.sb(st, "kT", [128, NT], BF16)
                gT = self.sb(st, "gT", [128, NT], F32)
                sgm = self.sb(st, "sgm", [128, NT], F32)
                qTd = self.sb(st, "qTd", [128, NT], BF16)
                decT = self.sb(st, "decT", [128, 12, 128], F32)
                qdt = self.sb(st, "qdt", [128, 12, 128], F32)
                gng = self.sb(st, "gng", [128, 12], F32)
                onesd = self.sb(st, "onesd", [128, 128], F32)
                r32 = self.sb(st, "r32", [128, NT], F32)
                r2 = self.sb(st, "r2", [128, NT], F32)
                t1 = self.sb(st, "t1", [128, NT], F32)
                t2 = self.sb(st, "t2", [128, NT], F32)
                pp = [self.sb(st, "pp", [128, 128], BF16) for _ in range(2)]
                exm = self.sb(st, "exm", [128, 2, 512], BF16)
                rd = self.sb(st, "rd", [128, NT], F32)
                pT = self.ps(st, "pT", [128, 8, 128], BF16)
                pa = [self.ps(st, "pa", [128, 512], F32) for _ in range(2)]
                pss = [self.ps(st, "pss", [128, 512], F32) for _ in range(2)]
                poo = [self.ps(st, "poo", [128, 512], F32) for _ in range(2)]
                self.load(decT, decT[:, :, :], I["decT"], I["decT"][:, :, :])
                self.load(qdt, qdt[:, :, :], I["qdtab"], I["qdtab"][:, :, :])
                self.load(gng, gng[:, :], I["gn_g"], I["gn_g"][:, :])
                P.op("dve", lambda e: e.memset(onesd[:, :], 1.0 / 128.0), writes=[(onesd, None)])
                self.stage_xT(st, I["x_own"], 8, hT, 0, xs, pT)
                npa = 0
                nq = 0
                for h in range(12):
                    wq_h, wkf, wg = wq[h % 2], wkf2[h % 2], wg2[h % 2]
                    with nc.allow_non_contiguous_dma(reason="512B runs"):
                        for w_, c0 in ((wq_h, h * 128), (wkf, W + h * 128), (wg, 3 * W + h * 128)):
                            self.load(w_, w_[:, :, :], w_in, w_in.h.ap()[:, c0:c0 + 128].rearrange("(c p) n -> p c n", p=128), q="pool", cls="w")
                    for w_, dst, eng in ((wq_h, qT, "act"), (wkf, kT, "dve"), (wg, gT, "act")):
                        for hf_ in range(2):
                            a_ = pa[npa % 2]; npa += 1
                            for c in range(16):
                                P.op("pe", lambda e, a_=a_, c=c, w_=w_, hf_=hf_: e.matmul(out=a_[:, :], lhsT=w_[:, c, :], rhs=hT[:, c, hf_ * 512:(hf_ + 1) * 512],
                                                                                         start=(c == 0), stop=(c == 15)),
                                     reads=[(w_, None), (hT, None)], writes=[(a_, None)], inc=(c == 15))
                            if eng == "act":
                                P.op("act", lambda e, a_=a_, dst=dst, hf_=hf_: e.activation(out=dst[:, hf_ * 512:(hf_ + 1) * 512], in_=a_[:, :], func=AF.Copy),
                                     reads=[(a_, None)], writes=[(dst, hf_)])
                            else:
                                P.op("dve", lambda e, a_=a_, dst=dst, hf_=hf_: e.tensor_copy(out=dst[:, hf_ * 512:(hf_ + 1) * 512], in_=a_[:, :]),
                                     reads=[(a_, None)], writes=[(dst, hf_)])
                    P.op("dve", lambda e, h=h: e.tensor_tensor(out=qTd.h.ap().rearrange("p (n q) -> p n q", q=128), in0=qT.h.ap().rearrange("p (n q) -> p n q", q=128),
                                                                in1=qdt[:, h:h + 1, :].to_broadcast([128, 8, 128]), op=ALU.mult),
                         reads=[(qT, None), (qdt, None)], writes=[(qTd, None)])
                    P.op("act", lambda e: e.activation(out=sgm[:, :], in_=gT[:, :], func=AF.Sigmoid), reads=[(gT, None)], writes=[(sgm, None)])
                    P.op("pool", lambda e: e.tensor_tensor(out=sgm[:, :], in0=sgm[:, :], in1=gT[:, :], op=ALU.mult), reads=[(sgm, None), (gT, None)], writes=[(sgm, None)])
                    for n in range(8):
                        s_, o_, p_ = pss[nq % 2], poo[nq % 2], pp[nq % 2]
                        nq += 1
                        P.op("pe", lambda e, s_=s_, n=n: e.matmul(out=s_[:, 0:128], lhsT=kT[:, n * 128:(n + 1) * 128], rhs=qT[:, n * 128:(n + 1) * 128], start=True, stop=True),
                             reads=[(kT, None), (qT, None)], writes=[(s_, None)])
                        P.op("dve", lambda e, s_=s_, p_=p_, h=h: e.tensor_tensor(out=p_[:, :], in0=s_[:, 0:128], in1=decT[:, h, :], op=ALU.mult),
                             reads=[(s_, None), (decT, None)], writes=[(p_, None)])
                        P.op("pe", lambda e, o_=o_, p_=p_, n=n, h=h: e.matmul(out=o_[:, 0:128], lhsT=Vown[:, n, h * 128:(h + 1) * 128], rhs=p_[:, :], start=True, stop=False),
                             reads=[(Vown, None), (p_, None)], writes=[(o_, None)], inc=False)
                        P.op("pe", lambda e, o_=o_, n=n, h=h: e.matmul(out=o_[:, 0:128], lhsT=Sb[:, n, h, :], rhs=qTd[:, n * 128:(n + 1) * 128], start=False, stop=True),
                             reads=[(Sb, None), (qTd, None)], writes=[(o_, None)])
                        P.op("act", lambda e, o_=o_, n=n: e.activation(out=r32[:, n * 128:(n + 1) * 128], in_=o_[:, 0:128], func=AF.Copy),
                             reads=[(o_, None)], writes=[(r32, n)])
                        P.op("act", lambda e, o_=o_, n=n: e.activation(out=r2[:, n * 128:(n + 1) * 128], in_=o_[:, 0:128], func=AF.Square),
                             reads=[(o_, None)], writes=[(r2, n)])
                    for hf_ in range(2):
                        cs = slice(hf_ * 512, (hf_ + 1) * 512)
                        m_, e_ = pa[0], pa[1]
                        P.op("pe", lambda e, m_=m_, cs=cs: e.matmul(out=m_[:, :], lhsT=onesd[:, :], rhs=r32[:, cs], start=True, stop=True),
                             reads=[(onesd, None), (r32, None)], writes=[(m_, None)])
                        P.op("pe", lambda e, e_=e_, cs=cs: e.matmul(out=e_[:, :], lhsT=onesd[:, :], rhs=r2[:, cs], start=True, stop=True),
                             reads=[(onesd, None), (r2, None)], writes=[(e_, None)])
                        P.op("act", lambda e, m_=m_, cs=cs: e.activation(out=t1[:, cs], in_=m_[:, :], func=AF.Square),
                             reads=[(m_, None)], writes=[(t1, hf_)])
                        P.op("dve", lambda e, e_=e_, cs=cs: e.tensor_tensor(out=t1[:, cs], in0=e_[:, :], in1=t1[:, cs], op=ALU.subtract),
                             reads=[(e_, None), (t1, hf_)], writes=[(t1, hf_)])
                        P.op("dve", lambda e, cs=cs: e.tensor_scalar(out=t1[:, cs], in0=t1[:, cs], scalar1=LN_EPS, scalar2=None, op0=ALU.add),
                             reads=[(t1, hf_)], writes=[(t1, hf_)])
                        P.op("act", lambda e, cs=cs: e.activation(out=t1[:, cs], in_=t1[:, cs], func=AF.Sqrt), reads=[(t1, hf_)], writes=[(t1, hf_)])
                        P.op("dve", lambda e, cs=cs: e.reciprocal(out=t1[:, cs], in_=t1[:, cs]), reads=[(t1, hf_)], writes=[(t1, hf_)])
                        P.op("dve", lambda e, m_=m_, cs=cs: e.tensor_tensor(out=t2[:, cs], in0=r32[:, cs], in1=m_[:, :], op=ALU.subtract),
                             reads=[(m_, None), (r32, None)], writes=[(t2, hf_)])
                        P.op("pool", lambda e, cs=cs: e.tensor_tensor(out=t2[:, cs], in0=t2[:, cs], in1=t1[:, cs], op=ALU.mult),
                             reads=[(t1, hf_), (t2, hf_)], writes=[(t2, hf_)])
                        P.op("dve", lambda e, cs=cs, h=h: e.scalar_tensor_tensor(out=catT[:, h, cs], in0=t2[:, cs], scalar=gng[:, h:h + 1], in1=sgm[:, cs],
                                                                                 op0=ALU.mult, op1=ALU.mult),
                             reads=[(t2, hf_), (gng, None), (sgm, None)], writes=[(catT, ("s", h, hf_))])
                self.mem_attn(st, w_in, 4 * W, hT, 0, memK, memV, catT, wq, qT, pa, pss, poo, exm, rd)
                if "cat" in self.cfg.get("debug", ()):
                    self.debug_out("dbg_cat", catT, [128, 16, NT], BF16)
                P.barrier()
            self.mix_out(catT, I["w_mix"], I["x_own"], I["ln1_g"], I["ln1_b"], self.h1o)


def host_consts(c, nel):
    out = {}
    out["ident"] = np.eye(128, dtype=np.float32)
    out["tri"] = (np.arange(128)[:, None] < np.arange(128)[None, :]).astype(np.float32)
    p = np.arange(128)
    ps = np.zeros((128, nel, NE), np.float32)
    for j in range(nel):
        ps[:, j, c * nel + j] = 1.0
    out["psel"] = ps
    pi = np.zeros((128, NSL, 2), np.float32)
    pi[:, :, 0] = NTOK + p[:, None]
    out["padinit"] = pi
    out["tokidf"] = (np.arange(64)[None, :] * 128 + p[:, None]).astype(np.float32)
    return out


def host_attn_consts(c):
    q = c % 4
    BIGD = 1.0e6
    k = np.arange(128)[:, None].astype(np.float32)
    qq = np.arange(128)[None, :].astype(np.float32)
    t0 = np.where(k >= qq, 128.0 + qq - k, BIGD).astype(np.float32)
    t1 = np.where(k <= qq, qq - k, BIGD).astype(np.float32)
    t2 = t0.copy() if q > 0 else np.full_like(t0, BIGD)
    if q == 0:
        t3 = np.full_like(t0, BIGD)
    elif q == 1:
        t3 = t0.copy()
        t3[:64, :] = BIGD
    else:
        t3 = t0.copy()
    return np.ascontiguousarray(np.stack([t0, t1, t2, t3], 1))


def host_ret_consts(c):
    gam = np.array([1.0 - 2.0 ** (-(5.0 + h)) for h in range(12)], np.float64)
    k = np.arange(128)[:, None, None].astype(np.float64)
    q = np.arange(128)[None, None, :].astype(np.float64)
    g3 = gam[None, :, None]
    decT = np.where(q >= k, SCALE * g3 ** np.maximum(q - k, 0.0), 0.0).astype(np.float32)
    qd = np.broadcast_to((g3 ** (q + 1.0)), (128, 12, 128)).astype(np.float32)
    kd = (SCALE * gam[None, :] ** (127.0 - np.arange(128)[:, None])).astype(np.float32)
    kdtab = np.ascontiguousarray(np.repeat(kd[:, :, None], 128, axis=2).reshape(128, 12 * 128))
    return {"decT": np.ascontiguousarray(decT), "qdtab": np.ascontiguousarray(qd), "kdtab": kdtab}


def host_moe_weights(inputs, L, c, nel):
    es = [c * nel + j for j in range(nel)]
    wi = np.asarray(inputs["moe_w_in"][L])[es]
    wi = wi.reshape(nel, 16, 128, 2, 8, 256)
    wi = np.ascontiguousarray(wi.transpose(0, 4, 2, 1, 3, 5)).reshape(nel, 8, 128, 16, 512)
    wo = np.asarray(inputs["moe_w_out"][L])[es]
    wo = wo.reshape(nel, 16, 128, 4, 512)
    wo = np.ascontiguousarray(wo.transpose(0, 3, 2, 1, 4))
    bi = np.asarray(inputs["moe_b_in"][L])[es].reshape(nel, 32, 128)
    bi = np.ascontiguousarray(bi.transpose(2, 0, 1))
    bo = np.ascontiguousarray(np.asarray(inputs["moe_b_out"][L])[es]).reshape(1, nel, D)
    return {"moe_win": wi, "moe_wout": wo, "moe_bin": bi, "moe_bout": bo}


def host_A_inputs(inputs, L, c, h_own, h_all_batch):
    b, q = c // 4, c % 4
    m = {"x_own": h_own}
    m["router_w"] = np.asarray(inputs["router_w"][L])
    m["router_b"] = np.asarray(inputs["router_b"][L]).reshape(1, NE)
    m["memb"] = np.asarray(inputs["mem"][b])
    m["w_mem"] = np.asarray(inputs["w_mem_kv"][L])
    m["w_mix"] = np.asarray(inputs["w_mix_out"][L])
    m["ln1_g"] = np.asarray(inputs["ln_mix_g"][L]).reshape(1, D)
    m["ln1_b"] = np.asarray(inputs["ln_mix_b"][L]).reshape(1, D)
    if L == 0:
        xb = np.asarray(inputs["x"][b])
        xp = np.zeros((2048, D), np.float32)
        n = min(q * NT, 2048)
        if n:
            xp[2048 - n:] = xb[q * NT - n:q * NT]
        m["x_prev"] = xp
        m["w_in"] = np.asarray(inputs["w_in_dil"][0])
        m["attn_c"] = host_attn_consts(c)
    else:
        hp = np.zeros((3 * NT, D), np.float32)
        if q:
            hp[(3 - q) * NT:] = h_all_batch[:q * NT]
        m["h_prev"] = hp
        m["w_in"] = np.asarray(inputs["w_in_ret"][0])
        m["gn_g"] = np.ascontiguousarray(np.asarray(inputs["ret_gn_g"][0]).reshape(12, 128).T)
        m.update(host_ret_consts(c))
    return m


TRACE = {"on": False, "last": None}


def launch(cfg, per_core):
    bld = Builder(cfg)
    nc = bld.build()
    in_maps = []
    for c in range(NCORES):
        m = per_core(c)
        hc = host_consts(c, cfg.get("nel", NEL))
        for name in bld.ins:
            if name not in m and name in hc:
                m[name] = hc[name]
        mm = {}
        for name, (shape, dt) in bld.ins.items():
            if name not in m:
                raise KeyError(name)
            a = np.ascontiguousarray(m[name])
            assert tuple(a.shape) == tuple(shape), (name, a.shape, shape)
            mm[name] = a
        in_maps.append(mm)
    if TRACE["on"]:
        res = run_bass_kernel_spmd(nc, in_maps, core_ids=list(range(NCORES)), trace=True)
        TRACE["last"] = res
    else:
        res = run_bass_kernel_spmd(nc, in_maps, core_ids=list(range(NCORES)))
    return res.results


def moe_layer(inputs, L, h1_own, h1b_own, g_own, nel=NEL):
    import ml_dtypes
    h1all = np.concatenate(list(h1b_own) + [np.zeros((128, D), ml_dtypes.bfloat16)], 0)
    gall = np.concatenate(list(g_own), 0)
    resB = launch({"kind": "B", "L": L, "nel": nel},
                  lambda c: dict(h1all=h1all, gall=gall, **host_moe_weights(inputs, L, c, nel)))
    yp = [resB[c]["ypart"].reshape(NROW, D) for c in range(NCORES)]
    lg = np.asarray(inputs["ln_ffn_g"][L]).reshape(1, D)
    lb = np.asarray(inputs["ln_ffn_b"][L]).reshape(1, D)
    resC = launch({"kind": "C", "L": L},
                  lambda c: dict(parts=np.stack([yp[r][c * NT:(c + 1) * NT] for r in range(NCORES)], 0), h1=h1_own[c], ln_g=lg, ln_b=lb))
    return [resC[c]["h2_out"] for c in range(NCORES)]


def kernel(**inputs):
    x = np.asarray(inputs["x"])
    h_own = [np.ascontiguousarray(x[c // 4, (c % 4) * NT:(c % 4 + 1) * NT]) for c in range(NCORES)]
    h_batch = [x[0], x[1]]
    for L in (0, 1):
        resA = launch({"kind": "A", "L": L, "mixer": True},
                      lambda c: host_A_inputs(inputs, L, c, h_own[c], h_batch[c // 4]))
        h_own = moe_layer(inputs, L, [resA[c]["h1_out"] for c in range(NCORES)], [resA[c]["h1b_out"] for c in range(NCORES)],
                          [resA[c]["g_out"] for c in range(NCORES)])
        h_batch = [np.concatenate(h_own[0:4], 0), np.concatenate(h_own[4:8], 0)]
    return np.stack(h_batch, 0).astype(np.float32)
```
